# Optimizing a Trainium2 kernel written in Bass

```python
import math
import jax, jax.numpy as jnp
from jax import lax
import numpy as np

D_MODEL = 1024
BATCH = 2
SEQ = 16384
DEPTH = 2

N_HEADS = 8
QK_NOPE = 128
QK_ROPE = 64
V_HEAD = 128
Q_LORA = 384
KV_LORA = 256
ROPE_THETA = 10000.0
Q_BLOCK = 128

POOL_WINDOWS = (2, 4, 8, 16)
POOL_GROUPS = 4
POOL_GROUP_IN = 128
POOL_WIDTH = POOL_GROUPS * POOL_GROUP_IN
POOL_GROUP_OUT = D_MODEL // POOL_GROUPS

IN_COLS = Q_LORA + KV_LORA + QK_ROPE + POOL_WIDTH + 2 * D_MODEL

N_EXPERTS = 32
TOP_K = 4
D_FF = 1024
SWIGLU_LIMIT = 7.0
SWIGLU_ALPHA = 1.702
EXPERT_BLOCK = 128

EPS = 1e-6
N_MOD = 6

kernel_name = "hybrid_mla_pool_moe_adaln"


def rmsnorm(x, g):
    xf = x.astype(jnp.float32)
    inv = lax.rsqrt(jnp.mean(xf * xf, axis=-1, keepdims=True) + EPS)
    return (xf * inv).astype(x.dtype) * g


def rope_tables(positions):
    half = QK_ROPE // 2
    inv_freq = ROPE_THETA ** (-jnp.arange(half, dtype=jnp.float32) / half)
    ang = positions.astype(jnp.float32)[..., None] * inv_freq
    return jnp.cos(ang), jnp.sin(ang)


def apply_rope(x, cos, sin):
    x1, x2 = jnp.split(x, 2, axis=-1)
    cos = cos.astype(x.dtype)
    sin = sin.astype(x.dtype)
    return jnp.concatenate([x1 * cos - x2 * sin, x2 * cos + x1 * sin], axis=-1)


def causal_block_attention(q, k, v):
    B, S, H, Dqk = q.shape
    n_blk = S // Q_BLOCK
    scale = 1.0 / math.sqrt(QK_NOPE + QK_ROPE)
    qb = q.reshape(B, n_blk, Q_BLOCK, H, Dqk).transpose(1, 0, 2, 3, 4)
    kpos = jnp.arange(S)

    def one_block(args):
        i, qi = args
        s = jnp.einsum('bqhd,bkhd->bhqk', qi, k).astype(jnp.float32) * scale
        qpos = i * Q_BLOCK + jnp.arange(Q_BLOCK)
        mask = kpos[None, :] <= qpos[:, None]
        s = jnp.where(mask[None, None], s, jnp.finfo(jnp.float32).min)
        p = jax.nn.softmax(s, axis=-1).astype(v.dtype)
        return jnp.einsum('bhqk,bkhd->bqhd', p, v)

    o = lax.map(one_block, (jnp.arange(n_blk), qb))
    return o.transpose(1, 0, 2, 3, 4).reshape(B, S, H * v.shape[-1])


def mla_branch(c_q, c_kv, k_rope_raw, cos, sin, q_norm, w_uq, kv_norm, w_ukv):
    B, S, _ = c_q.shape
    q = rmsnorm(c_q, q_norm) @ w_uq
    q = q.reshape(B, S, N_HEADS, QK_NOPE + QK_ROPE)
    q_nope, q_rope = q[..., :QK_NOPE], q[..., QK_NOPE:]
    q_rope = apply_rope(q_rope, cos[:, :, None, :], sin[:, :, None, :])
    kv = (rmsnorm(c_kv, kv_norm) @ w_ukv).reshape(B, S, N_HEADS, QK_NOPE + V_HEAD)
    k_nope, v = kv[..., :QK_NOPE], kv[..., QK_NOPE:]
    k_rope = apply_rope(k_rope_raw, cos, sin)[:, :, None, :]
    k = jnp.concatenate([k_nope, jnp.broadcast_to(k_rope, (B, S, N_HEADS, QK_ROPE))], axis=-1)
    qf = jnp.concatenate([q_nope, q_rope], axis=-1)
    return causal_block_attention(qf, k, v)


def pool_branch(u, w_pool, pool_scale):
    B, S, _ = u.shape
    ug = u.reshape(B, S, POOL_GROUPS, POOL_GROUP_IN)
    uf = ug.astype(jnp.float32)
    cs = jnp.cumsum(uf, axis=1)
    cs_pad = jnp.concatenate([jnp.zeros((B, 1, POOL_GROUPS, POOL_GROUP_IN), jnp.float32), cs], axis=1)
    t = jnp.arange(S, dtype=jnp.float32)
    diffs = []
    for g, w in enumerate(POOL_WINDOWS):
        upper = cs_pad[:, 1:, g]
        lower = jnp.pad(cs_pad[:, :S + 1 - w, g], ((0, 0), (w - 1, 0), (0, 0)))
        count = jnp.minimum(t + 1.0, float(w))[None, :, None]
        diffs.append((upper - lower) / count - uf[:, :, g])
    p = jnp.stack(diffs, axis=2).astype(u.dtype)
    out = jnp.einsum('bsgc,gcd->bsgd', p, w_pool) * pool_scale
    return out.reshape(B, S, D_MODEL)


def moe_ffn(h, w_router, b_router, w_gu, b_gu, w_down, b_down):
    B, S, D = h.shape
    T = B * S
    xt = h.reshape(T, D)
    logits = (xt @ w_router + b_router).astype(jnp.float32)
    top_val, top_idx = lax.top_k(logits, TOP_K)
    top_w = jax.nn.softmax(top_val, axis=-1).astype(h.dtype)

    n_assign = T * TOP_K
    flat_e = top_idx.reshape(-1)
    flat_tok = jnp.repeat(jnp.arange(T, dtype=jnp.int32), TOP_K)
    flat_w = top_w.reshape(-1)
    order = jnp.argsort(flat_e, stable=True)
    e_sorted, tok_sorted, w_sorted = flat_e[order], flat_tok[order], flat_w[order]

    counts = jnp.bincount(flat_e, length=N_EXPERTS)
    starts = jnp.cumsum(counts) - counts
    padded = (counts + EXPERT_BLOCK - 1) // EXPERT_BLOCK * EXPERT_BLOCK
    pad_ends = jnp.cumsum(padded)
    pad_starts = pad_ends - padded
    dest = pad_starts[e_sorted] + (jnp.arange(n_assign) - starts[e_sorted])

    n_blocks = (n_assign + EXPERT_BLOCK - 1) // EXPERT_BLOCK + N_EXPERTS
    n_slots = n_blocks * EXPERT_BLOCK
    slot_tok = jnp.full((n_slots,), T, jnp.int32).at[dest].set(tok_sorted)
    slot_w = jnp.zeros((n_slots,), h.dtype).at[dest].set(w_sorted)
    block_e = jnp.minimum(
        jnp.searchsorted(pad_ends, jnp.arange(n_blocks) * EXPERT_BLOCK, side='right'), N_EXPERTS - 1)
    x_pad = jnp.concatenate([xt, jnp.zeros((1, D), xt.dtype)], axis=0)

    def expert_block(args):
        tok, wt, e = args
        xb = x_pad[tok]
        gu = xb @ w_gu[e] + b_gu[e]
        glu, lin = gu[:, :D_FF], gu[:, D_FF:]
        glu = jnp.minimum(glu, SWIGLU_LIMIT)
        lin = jnp.clip(lin, -SWIGLU_LIMIT, SWIGLU_LIMIT)
        act = glu * jax.nn.sigmoid(SWIGLU_ALPHA * glu) * (lin + 1.0)
        return (act @ w_down[e] + b_down[e]) * wt[:, None]

    y = lax.map(expert_block, (slot_tok.reshape(n_blocks, EXPERT_BLOCK),
                               slot_w.reshape(n_blocks, EXPERT_BLOCK), block_e))
    out = jax.ops.segment_sum(y.reshape(n_slots, D), slot_tok, num_segments=T + 1)[:T]
    return out.reshape(B, S, D)


def setup_inputs(seed: int = 0) -> dict:
    key = jax.random.key(seed)
    ks = jax.random.split(key, 24)
    f32 = jnp.float32
    nrm = lambda k, shape, s: (jax.random.normal(k, shape, f32) * s)
    L, D, E = DEPTH, D_MODEL, N_EXPERTS
    x = jax.random.normal(ks[0], (BATCH, SEQ, D), f32)
    c = jax.random.normal(ks[1], (BATCH, D), f32)
    offset = jax.random.randint(ks[2], (BATCH, 1), 0, 4096, dtype=jnp.int32)
    positions = offset + jnp.arange(SEQ, dtype=jnp.int32)[None, :]
    return {
        "x": x,
        "c": c,
        "positions": positions,
        "ada_w": nrm(ks[3], (L, D, N_MOD * D), 0.5 * D ** -0.5),
        "ada_b": nrm(ks[4], (L, N_MOD * D), 0.02),
        "norm_mix": 1.0 + nrm(ks[5], (L, D), 0.05),
        "norm_ffn": 1.0 + nrm(ks[6], (L, D), 0.05),
        "w_in": nrm(ks[7], (L, D, IN_COLS), D ** -0.5),
        "q_norm": 1.0 + nrm(ks[8], (L, Q_LORA), 0.05),
        "w_uq": nrm(ks[9], (L, Q_LORA, N_HEADS * (QK_NOPE + QK_ROPE)), Q_LORA ** -0.5),
        "kv_norm": 1.0 + nrm(ks[10], (L, KV_LORA), 0.05),
        "w_ukv": nrm(ks[11], (L, KV_LORA, N_HEADS * (QK_NOPE + V_HEAD)), KV_LORA ** -0.5),
        "w_pool": nrm(ks[12], (L, POOL_GROUPS, POOL_GROUP_IN, POOL_GROUP_OUT), POOL_GROUP_IN ** -0.5),
        "pool_scale": 1.0 + nrm(ks[13], (L, POOL_GROUPS, POOL_GROUP_OUT), 0.1),
        "w_out": nrm(ks[14], (L, D, D), D ** -0.5),
        "w_router": nrm(ks[15], (L, D, E), D ** -0.5),
        "b_router": nrm(ks[16], (L, E), 0.01),
        "w_gu": nrm(ks[17], (L, E, D, 2 * D_FF), D ** -0.5),
        "b_gu": nrm(ks[18], (L, E, 2 * D_FF), 0.02),
        "w_down": nrm(ks[19], (L, E, D_FF, D), D_FF ** -0.5),
        "b_down": nrm(ks[20], (L, E, D), 0.02),
        "norm_final": 1.0 + nrm(ks[21], (D,), 0.05),
    }


def reference(x, c, positions, ada_w, ada_b, norm_mix, norm_ffn, w_in, q_norm, w_uq,
              kv_norm, w_ukv, w_pool, pool_scale, w_out, w_router, b_router,
              w_gu, b_gu, w_down, b_down, norm_final):
    cos, sin = rope_tables(positions)
    c_act = jax.nn.silu(c)
    o1 = Q_LORA
    o2 = o1 + KV_LORA
    o3 = o2 + QK_ROPE
    o4 = o3 + POOL_WIDTH
    o5 = o4 + D_MODEL
    for l in range(DEPTH):
        mod = c_act @ ada_w[l] + ada_b[l]
        sh_m, sc_m, g_m, sh_f, sc_f, g_f = [m[:, None, :] for m in jnp.split(mod, N_MOD, axis=-1)]

        h = rmsnorm(x, norm_mix[l]) * (1.0 + sc_m) + sh_m
        z = h @ w_in[l]
        attn = mla_branch(z[..., :o1], z[..., o1:o2], z[..., o2:o3], cos, sin,
                          q_norm[l], w_uq[l], kv_norm[l], w_ukv[l])
        pool = pool_branch(z[..., o3:o4], w_pool[l], pool_scale[l])
        mixed = jax.nn.sigmoid(z[..., o4:o5]) * attn + jax.nn.sigmoid(z[..., o5:]) * pool
        x = x + g_m * (mixed @ w_out[l])

        h = rmsnorm(x, norm_ffn[l]) * (1.0 + sc_f) + sh_f
        x = x + g_f * moe_ffn(h, w_router[l], b_router[l], w_gu[l], b_gu[l], w_down[l], b_down[l])
    return rmsnorm(x, norm_final)
```

```python
import math
import numpy as np
import ml_dtypes
import concourse.bass as bass
import concourse.mybir as mybir
from concourse.bass_utils import run_bass_kernel_spmd

F32 = mybir.dt.float32
BF16 = mybir.dt.bfloat16
I32 = mybir.dt.int32
ALU = mybir.AluOpType
AF = mybir.ActivationFunctionType
AX = mybir.AxisListType

D = 1024
S = 16384
NH = 8
QLORA = 384
KVLORA = 256
ROPE = 64
INC = 3264
NE = 32
DFF = 1024
EPS = 1e-6
TL = 4096
NG = 8
SCALE = 1.0 / math.sqrt(192.0)
SAME_SYNC = True
MOE_DBG = 0
NE_RUN = 32
FUSE_DBG = 0


class Buf:
    def __init__(self, t, name):
        self.t = t
        self.name = name
        self.w = None
        self.rs = []
        self.dsems = {}

    def __getitem__(self, idx):
        return self.t[idx]


class Op:
    __slots__ = ("stream", "fn", "reads", "writes", "dma", "dbuf", "deps", "sig", "signal", "barrier", "idx", "inc", "release")

    def __init__(self, stream, fn, reads, writes, dma=False, dbuf=None, barrier=False):
        self.stream = stream
        self.fn = fn
        self.reads = reads
        self.writes = writes
        self.dma = dma
        self.dbuf = dbuf
        self.deps = []
        self.sig = False
        self.signal = None
        self.barrier = barrier
        self.inc = 16
        self.release = None


class KB:
    STREAMS = ("pe", "act", "dve", "pool", "sp")

    def __init__(self, nc):
        self.nc = nc
        self.ops = []
        self.eng = dict(pe=nc.tensor, act=nc.scalar, dve=nc.vector, pool=nc.gpsimd, sp=nc.sync)
        self.bufs = []
        self.guards = []
        self.gbufs = []
        self.n = 0

    def sb(self, shape, dt, name=None):
        self.n += 1
        name = (name or "t") + "_%d" % self.n
        g = self.nc.sbuf_tensor(name, list(shape), dt)
        b = Buf(g.__enter__(), name)
        self.guards.append(g)
        self.gbufs.append((g, b))
        self.bufs.append(b)
        return b

    def scope_begin(self):
        return len(self.guards)

    def scope_end(self, mark):
        self.barrier()
        rel = Op(None, None, [], [])
        rel.release = [b for (_, b) in self.gbufs[mark:]]
        self.ops.append(rel)
        while len(self.guards) > mark:
            self.guards.pop().__exit__(None, None, None)
            self.gbufs.pop()

    def coll(self, kind, in_ap, out_ap, groups):
        b = Buf(None, "cc%d" % self.n)
        self.n += 1
        fn = lambda e: e.collective_compute(kind, ALU.bypass, replica_groups=groups, ins=[in_ap], outs=[out_ap])
        o = Op("pool", fn, [], [b], dma=True, dbuf=b)
        o.inc = 1
        self.ops.append(o)

    def ps(self, shape, dt=F32, name=None):
        self.n += 1
        name = (name or "p") + "_%d" % self.n
        b = Buf(self.nc.alloc_psum_tensor(name, list(shape), dt), name)
        self.bufs.append(b)
        return b

    def op(self, stream, fn, reads=(), writes=()):
        self.ops.append(Op(stream, fn, list(reads), list(writes)))

    def dma(self, q, out_ap, in_ap, buf, load, extra_reads=(), **kw):
        fn = lambda e: e.dma_start(out=out_ap, in_=in_ap, **kw)
        if load:
            o = Op(q, fn, list(extra_reads), [buf], dma=True, dbuf=buf)
        else:
            o = Op(q, fn, [buf] + list(extra_reads), [], dma=True, dbuf=buf)
        self.ops.append(o)

    def barrier(self):
        self.ops.append(Op(None, None, [], [], barrier=True))

    def finish(self):
        nc = self.nc
        last = {s: None for s in self.STREAMS}
        dma_last = {}
        expanded = []
        for o in self.ops:
            if o.release is not None:
                for b in o.release:
                    for kk_ in ("sp", "pool", "cc"):
                        dma_last.pop((id(b), kk_), None)
                expanded.append(o)
                continue
            if o.barrier:
                for s in self.STREAMS:
                    p = Op(s, None, [], [])
                    p.idx = len(expanded)
                    deps = [last[s2] for s2 in self.STREAMS if s2 != s and last[s2] is not None and not last[s2].dma]
                    deps += list(dma_last.values())
                    p.deps = deps
                    for d in deps:
                        if not d.dma:
                            d.sig = True
                    expanded.append(p)
                continue
            deps = set()
            for b in o.reads:
                if b.w is not None:
                    deps.add(b.w)
            for b in o.writes:
                if b.w is not None:
                    deps.add(b.w)
                for r in b.rs:
                    deps.add(r)
            deps.discard(o)
            for b in o.writes:
                b.w = o
                b.rs = []
            for b in o.reads:
                if b not in o.writes:
                    b.rs.append(o)
            fin = []
            best = {}
            for d in deps:
                if d.dma:
                    fin.append(d)
                    continue
                if d.stream == o.stream and (o.stream == "pe" or not SAME_SYNC):
                    continue
                if d.stream not in best or best[d.stream].idx < d.idx:
                    best[d.stream] = d
            for d in best.values():
                d.sig = True
                fin.append(d)
            o.deps = fin
            o.idx = len(expanded)
            last[o.stream] = o
            if o.dma:
                dma_last[(id(o.dbuf), "cc" if o.inc == 1 else o.stream)] = o
            expanded.append(o)
        sems = {s: nc.alloc_semaphore("s_" + s) for s in self.STREAMS}
        cnt = {s: 0 for s in self.STREAMS}
        seen = {s: {} for s in self.STREAMS}
        free_sems = {}
        for o in expanded:
            if o.release is not None:
                for b in o.release:
                    for kind, (sem_, cnt_) in b.dsems.items():
                        free_sems.setdefault(kind, []).append((sem_, cnt_))
                    b.dsems = {}
                continue
            e = self.eng[o.stream]
            need = {}
            for d in o.deps:
                sem, val = d.signal
                k = id(sem)
                if k not in need or need[k][1] < val:
                    need[k] = (sem, val)
            for k, (sem, val) in need.items():
                if seen[o.stream].get(k, 0) < val:
                    e.wait_ge(sem, val)
                    seen[o.stream][k] = val
            if o.fn is None:
                continue
            ins = o.fn(e)
            if o.dma:
                b = o.dbuf
                kind = "cc" if o.inc == 1 else o.stream
                if kind not in b.dsems:
                    fl = free_sems.get(kind)
                    if fl:
                        b.dsems[kind] = fl.pop()
                    else:
                        b.dsems[kind] = (nc.alloc_semaphore("d_%s_%s" % (kind, b.name)), 0)
                sem_, cnt_ = b.dsems[kind]
                cnt_ += o.inc
                b.dsems[kind] = (sem_, cnt_)
                ins.then_inc(sem_, o.inc)
                o.signal = (sem_, cnt_)
            elif o.sig:
                cnt[o.stream] += 1
                ins.then_inc(sems[o.stream], 1)
                o.signal = (sems[o.stream], cnt[o.stream])
        return cnt


def mm(k, out_buf, out_ap, lhsT_buf, lhsT_ap, rhs_buf, rhs_ap, start, stop):
    k.op("pe", lambda e: e.matmul(out_ap, lhsT_ap, rhs_ap, start=start, stop=stop),
         reads=[lhsT_buf, rhs_buf], writes=[out_buf])


def tr(k, out_buf, out_ap, in_buf, in_ap, ident_buf, ident_ap):
    k.op("pe", lambda e: e.transpose(out_ap, in_ap, ident_ap), reads=[in_buf, ident_buf], writes=[out_buf])


class Ctx:
    pass


def AP_(x):
    try:
        return x.ap()
    except TypeError:
        return x


def dram_in(nc, name, shape, dt):
    return nc.dram_tensor(name, list(shape), dt, kind="ExternalInput")


def dram_out(nc, name, shape, dt):
    return nc.dram_tensor(name, list(shape), dt, kind="ExternalOutput")


def dram_tmp(nc, name, shape, dt):
    return nc.dram_tensor(name, list(shape), dt, kind="Internal")


def setup_common(k, c, dr):
    nc = k.nc
    c.ps = [k.ps([128, 512], F32, "bank%d" % i) for i in range(8)]
    c.ident_f = k.sb([128, 128], F32, "identf")
    c.ident_b = k.sb([128, 128], BF16, "identb")
    k.dma("sp", c.ident_f[:, :], AP_(dr["ident"])[:, :], c.ident_f, True)
    k.op("dve", lambda e: e.tensor_copy(c.ident_b[:, :], c.ident_f[:, :]), [c.ident_f], [c.ident_b])
    c.neghalf = k.sb([128, 1], F32, "neghalf")
    k.op("pool", lambda e: e.memset(c.neghalf[:, :], -0.5), [], [c.neghalf])
    c.ones_b = k.sb([128, 128], BF16, "onesb")
    k.op("pool", lambda e: e.memset(c.ones_b[:, :], 1.0), [], [c.ones_b])
    c.ones_f = k.sb([128, 128], F32, "onesf")
    k.op("pool", lambda e: e.memset(c.ones_f[:, :], 1.0), [], [c.ones_f])


def compute_mod(k, c, dr):
    if not hasattr(c, "mod"):
        c.mod = k.sb([128, 6 * D], F32, "modrep")
    k.dma("sp", c.mod[:, :], AP_(dr["ada_b_rep"])[:, :], c.mod, True)
    mark = k.scope_begin()
    cT = k.sb([128, 8], F32, "cT")
    k.dma("sp", cT[:, :], AP_(dr["cT"])[:, :], cT, True)
    cact = k.sb([128, 8], F32, "cact")
    k.op("act", lambda e: e.activation(cact[:, :], cT[:, :], AF.Silu), [cT], [cact])
    cb = k.sb([128, 8, 128], F32, "cactb")
    k.op("pool", lambda e: e.memset(cb[:, :, :], 1.0), [], [cb])
    for kc in range(8):
        k.op("dve", lambda e, kc=kc: e.tensor_scalar(cb[:, kc, :], cb[:, kc, :], cact[:, kc:kc + 1], None, ALU.mult),
             [cb, cact], [cb])
    wbufs = [k.sb([128, 8, 256], F32, "adaw%d" % i) for i in range(2)]
    aw = AP_(dr["ada_w"]).rearrange("(kc p) n -> p kc n", p=128)
    for cg in range(24):
        wb = wbufs[cg % 2]
        k.dma("sp", wb[:, :, :], aw[:, :, cg * 256:(cg + 1) * 256], wb, True)
        pb = c.ps[cg % 2]
        for kc in range(8):
            mm(k, pb, pb[:, 0:256], cb, cb[:, kc, :], wb, wb[:, kc, :], kc == 0, kc == 7)
        k.op("dve", lambda e, pb=pb, cg=cg: e.tensor_tensor(c.mod[:, cg * 256:(cg + 1) * 256], pb[:, 0:256],
                                                           c.mod[:, cg * 256:(cg + 1) * 256], ALU.add),
             [pb, c.mod], [c.mod])
    k.scope_end(mark)


def rms_rstd(k, c, x_buf, x_ap, n, junk, ss, rstd):
    k.op("pool", lambda e: e.memset(ss[:, :], 0.0), [], [ss])
    k.op("dve", lambda e: e.scalar_tensor_tensor(out=junk, in0=x_ap, scalar=1.0, in1=x_ap, op0=ALU.mult,
                                                 op1=ALU.mult, accum_out=ss[:, :]), [x_buf, ss], [ss, c.junkb])
    k.op("dve", lambda e: e.tensor_scalar(ss[:, :], ss[:, :], 1.0 / n, EPS, ALU.mult, ALU.add), [ss], [ss])
    k.op("pool", lambda e: e.tensor_tensor(rstd[:, :], ss[:, :], c.neghalf[:, :], ALU.pow), [ss, c.neghalf], [rstd])


def phase_pre(k, c, dr):
    nc = k.nc
    win = k.sb([128, 8, INC], BF16, "win")
    winap = AP_(dr["w_in"]).rearrange("(kc p) n -> p kc n", p=128)
    for kc in range(8):
        for c0 in range(0, INC, 1632):
            k.dma("pool", win[:, kc, c0:c0 + 1632], winap[:, kc, c0:c0 + 1632], win, True)
    winrot = k.sb([128, 8, 64], BF16, "winrot")
    for kc in range(8):
        k.dma("pool", winrot[:, kc, 0:32], winap[:, kc, 640 + 32:640 + 64], winrot, True)
        k.dma("pool", winrot[:, kc, 32:64], winap[:, kc, 640:640 + 32], winrot, True)
    k.op("dve", lambda e: e.tensor_scalar(winrot[:, :, 0:32], winrot[:, :, 0:32], -1.0, None, ALU.mult), [winrot], [winrot])
    wuq = k.sb([128, 3, 1536], BF16, "wuq")
    wuqrot = k.sb([128, 3, NH, 64], BF16, "wuqrot")
    mark = k.scope_begin()
    wuq_f = k.sb([128, 3, 1536], F32, "wuqf")
    wuqap = AP_(dr["w_uq"]).rearrange("(kc p) n -> p kc n", p=128)
    k.dma("sp", wuq_f[:, :, :], wuqap, wuq_f, True)
    k.op("dve", lambda e: e.tensor_scalar(wuq[:, :, :], wuq_f[:, :, :], SCALE, None, ALU.mult), [wuq_f], [wuq])
    wv = wuq_f[:, :, :].rearrange("p k (h d) -> p k h d", h=NH)
    k.op("dve", lambda e: e.tensor_scalar(wuqrot[:, :, :, 0:32], wv[:, :, :, 160:192], -SCALE, None, ALU.mult), [wuq_f], [wuqrot])
    k.op("dve", lambda e: e.tensor_scalar(wuqrot[:, :, :, 32:64], wv[:, :, :, 128:160], SCALE, None, ALU.mult), [wuq_f], [wuqrot])
    k.scope_end(mark)
    A = k.sb([128, D], F32, "Arep")
    k.dma("sp", A[:, :], AP_(dr["norm_mix_rep"])[:, :], A, True)
    qn = k.sb([128, QLORA], F32, "qnrep")
    k.dma("sp", qn[:, :], AP_(dr["q_norm_rep"])[:, :], qn, True)
    kvn = k.sb([128, KVLORA], F32, "kvnrep")
    k.dma("sp", kvn[:, :], AP_(dr["kv_norm_rep"])[:, :], kvn, True)
    k.op("dve", lambda e: e.scalar_tensor_tensor(out=A[:, :], in0=c.mod[:, D:2 * D], scalar=1.0, in1=A[:, :],
                                                 op0=ALU.add, op1=ALU.mult), [c.mod, A], [A])
    cosT = k.sb([64, TL], F32, "cosT")
    sinT = k.sb([64, TL], F32, "sinT")
    mark = k.scope_begin()
    invf = k.sb([64, 1], F32, "invf")
    k.dma("sp", invf[:, :], AP_(dr["inv_freq"])[:, :], invf, True)
    CH = 1024
    posi = k.sb([64, CH], I32, "posi")
    ang = k.sb([64, CH], F32, "ang")
    tq = k.sb([64, CH], F32, "tq")
    ti = k.sb([64, CH], I32, "ti")
    C1 = 6.28125
    C2 = 2.0 * math.pi - C1
    for ch in range(TL // CH):
        sl = slice(ch * CH, (ch + 1) * CH)
        k.dma("sp", posi[:, :], AP_(dr["pos_rep"])[:, sl], posi, True)
        k.op("dve", lambda e: e.tensor_copy(ang[:, :], posi[:, :]), [posi], [ang])
        k.op("dve", lambda e: e.tensor_scalar(ang[:, :], ang[:, :], invf[:, 0:1], None, ALU.mult), [ang, invf], [ang])
        for (dstb, shift) in ((sinT, 0.0), (cosT, math.pi / 2)):
            dst = dstb[:, sl]
            k.op("dve", lambda e, shift=shift: e.tensor_scalar(tq[:, :], ang[:, :], shift, 1.0 / (2 * math.pi), ALU.add, ALU.mult), [ang], [tq])
            k.op("dve", lambda e: e.tensor_copy(ti[:, :], tq[:, :]), [tq], [ti])
            k.op("dve", lambda e: e.tensor_copy(tq[:, :], ti[:, :]), [ti], [tq])
            k.op("dve", lambda e, dst=dst, shift=shift: e.tensor_scalar(dst, ang[:, :], shift, None, ALU.add), [ang], [dstb])
            k.op("dve", lambda e, dst=dst: e.scalar_tensor_tensor(out=dst, in0=tq[:, :], scalar=-C1, in1=dst, op0=ALU.mult, op1=ALU.add), [tq, dstb], [dstb])
            k.op("dve", lambda e, dst=dst: e.scalar_tensor_tensor(out=dst, in0=tq[:, :], scalar=-C2, in1=dst, op0=ALU.mult, op1=ALU.add), [tq, dstb], [dstb])
            k.op("dve", lambda e, dst=dst: e.tensor_scalar(tq[:, :], dst, math.pi, -2 * math.pi, ALU.is_gt, ALU.mult), [dstb], [tq])
            k.op("dve", lambda e, dst=dst: e.tensor_tensor(dst, dst, tq[:, :], ALU.add), [dstb, tq], [dstb])
            k.op("dve", lambda e, dst=dst: e.tensor_scalar(tq[:, :], dst, -math.pi, 2 * math.pi, ALU.is_lt, ALU.mult), [dstb], [tq])
            k.op("dve", lambda e, dst=dst: e.tensor_tensor(dst, dst, tq[:, :], ALU.add), [dstb, tq], [dstb])
            k.op("dve", lambda e, dst=dst: e.tensor_scalar(dst, dst, math.pi, -math.pi, ALU.min, ALU.max), [dstb], [dstb])
            k.op("act", lambda e, dst=dst: e.activation(dst, dst, AF.Sin), [dstb], [dstb])
    k.scope_end(mark)

    xt = [k.sb([128, D], F32, "xt%d" % i) for i in range(2)]
    h32 = k.sb([128, D], F32, "h32")
    hb = [k.sb([128, D], BF16, "hb%d" % i) for i in range(2)]
    c.junkb = k.sb([128, D], BF16, "junk")
    ss = [k.sb([128, 1], F32, "ss%d" % i) for i in range(2)]
    rstd = [k.sb([128, 1], F32, "rstd%d" % i) for i in range(2)]
    hT = [k.sb([128, 8, 512], BF16, "hT%d" % i) for i in range(2)]
    zs = k.sb([128, 640], F32, "zs")
    ssq = k.sb([128, 1], F32, "ssq")
    rsq = k.sb([128, 1], F32, "rsq")
    ssk = k.sb([128, 1], F32, "ssk")
    rsk = k.sb([128, 1], F32, "rsk")
    cqb = k.sb([128, 640], BF16, "cqb")
    cqT = k.sb([128, 3, 512], BF16, "cqT")
    latT = k.sb([128, 2, 512], BF16, "latT")
    ub = [k.sb([128, 512], BF16, "ub%d" % i) for i in range(2)]
    sg = [k.sb([128, 512], BF16, "sg%d" % i) for i in range(3)]
    kr1 = k.sb([64, 512], F32, "kr1")
    kr2 = k.sb([64, 512], F32, "kr2")
    krb = [k.sb([64, 512], BF16, "krb%d" % i) for i in range(2)]
    qnb = [k.sb([128, 512], BF16, "qnb%d" % i) for i in range(2)]
    qrb = [k.sb([64, 512], BF16, "qrb%d" % i) for i in range(2)]
    x_d = AP_(dr["x"])
    u_d = AP_(dr["u_local"])
    sig_d = AP_(dr["sigT"])
    lat_d = AP_(dr["latT_local"]) if "latT_local" in dr else None
    q_d = AP_(dr["qT"])
    ps = c.ps
    psb = [p[:, :].bitcast(BF16) for p in ps]
    nst = 0
    for g in range(NG):
        hTg = hT[g % 2]
        for tt in range(4):
            t = g * 4 + tt
            xb = xt[t % 2]
            k.dma("sp", xb[:, :], x_d[t * 128:(t + 1) * 128, :], xb, True)
            s_, r_ = ss[t % 2], rstd[t % 2]
            rms_rstd(k, c, xb, xb[:, :], D, c.junkb[:, :], s_, r_)
            k.op("dve", lambda e, xb=xb, r_=r_: e.scalar_tensor_tensor(out=h32[:, :], in0=xb[:, :], scalar=r_[:, 0:1], in1=A[:, :],
                                                                     op0=ALU.mult, op1=ALU.mult), [xb, r_, A], [h32])
            hbb = hb[t % 2]
            k.op("pool", lambda e, hbb=hbb: e.tensor_tensor(hbb[:, :], h32[:, :], c.mod[:, 0:D], ALU.add), [h32, c.mod], [hbb])
            pt = ps[7]
            for kc in range(8):
                tr(k, pt, psb[7][:, kc * 128:(kc + 1) * 128], hbb, hbb[:, kc * 128:(kc + 1) * 128], c.ident_b, c.ident_b[:, :])
            k.op("act", lambda e, hTg=hTg, tt=tt: e.copy(hTg[:, :, tt * 128:(tt + 1) * 128],
                                                        psb[7][:, :].rearrange("p (k t) -> p k t", k=8)), [pt], [hTg])
        for tt in range(4):
            t = g * 4 + tt
            cols = ((0, 512), (512, 1024), (1024, 1216))
            for bi, (c0, c1) in enumerate(cols):
                for kc in range(8):
                    mm(k, ps[bi], ps[bi][:, 0:c1 - c0], hTg, hTg[:, kc, tt * 128:(tt + 1) * 128], win, win[:, kc, c0:c1], kc == 0, kc == 7)
            k.op("act", lambda e: e.copy(zs[:, 0:512], ps[0][:, 0:512]), [ps[0]], [zs])
            k.op("act", lambda e: e.copy(zs[:, 512:640], ps[1][:, 0:128]), [ps[1]], [zs])
            ubb = ub[t % 2]
            k.op("act", lambda e, ubb=ubb: e.copy(ubb[:, 0:320], ps[1][:, 192:512]), [ps[1]], [ubb])
            k.op("act", lambda e, ubb=ubb: e.copy(ubb[:, 320:512], ps[2][:, 0:192]), [ps[2]], [ubb])
            k.dma("pool", u_d[t * 128:(t + 1) * 128, :], ubb[:, :], ubb, False)
            if tt == 3 and "uh_local" in dr:
                k.dma("pool", AP_(dr["uh_local"])[g * 16:(g + 1) * 16, :], ubb[112:128, :], ubb, False)
            rms_rstd(k, c, zs, zs[:, 0:QLORA], QLORA, c.junkb[:, 0:QLORA], ssq, rsq)
            rms_rstd(k, c, zs, zs[:, QLORA:640], KVLORA, c.junkb[:, 0:KVLORA], ssk, rsk)
            k.op("dve", lambda e: e.scalar_tensor_tensor(out=cqb[:, 0:QLORA], in0=zs[:, 0:QLORA], scalar=rsq[:, 0:1], in1=qn[:, :],
                                                         op0=ALU.mult, op1=ALU.mult), [zs, rsq, qn], [cqb])
            k.op("dve", lambda e: e.scalar_tensor_tensor(out=cqb[:, QLORA:640], in0=zs[:, QLORA:640], scalar=rsk[:, 0:1], in1=kvn[:, :],
                                                         op0=ALU.mult, op1=ALU.mult), [zs, rsk, kvn], [cqb])
            pt = ps[7]
            for kc in range(5):
                tr(k, pt, psb[7][:, kc * 128:(kc + 1) * 128], cqb, cqb[:, kc * 128:(kc + 1) * 128], c.ident_b, c.ident_b[:, :])
            k.op("act", lambda e, tt=tt: e.copy(cqT[:, :, tt * 128:(tt + 1) * 128],
                                               psb[7][:, 0:384].rearrange("p (k t) -> p k t", k=3)), [pt], [cqT])
            k.op("act", lambda e, tt=tt: e.copy(latT[:, :, tt * 128:(tt + 1) * 128],
                                               psb[7][:, 384:640].rearrange("p (k t) -> p k t", k=2)), [pt], [latT])
        tok = slice(g * 512, (g + 1) * 512)
        for kc in range(2):
            dst = dr["lat_parts"][kc][:, tok] if "lat_parts" in dr else lat_d[kc * 128:(kc + 1) * 128, tok]
            k.dma("pool", dst, latT[:, kc, :], latT, False)
        for f in range(16):
            pb = ps[3 + (f % 3)]
            for kc in range(8):
                mm(k, pb, pb[:, :], win, win[:, kc, 1216 + f * 128:1216 + (f + 1) * 128], hTg, hTg[:, kc, :], kc == 0, kc == 7)
            sgb = sg[nst % 3]
            nst += 1
            k.op("act", lambda e, sgb=sgb, pb=pb: e.activation(sgb[:, :], pb[:, :], AF.Sigmoid), [pb], [sgb])
            k.dma("pool", sig_d[f * 128:(f + 1) * 128, tok], sgb[:, :], sgb, False)
        for kc in range(8):
            mm(k, ps[6], ps[6][0:64, :], win, win[:, kc, 640:704], hTg, hTg[:, kc, :], kc == 0, kc == 7)
        for kc in range(8):
            mm(k, ps[3], ps[3][0:64, :], winrot, winrot[:, kc, :], hTg, hTg[:, kc, :], kc == 0, kc == 7)
        krbb = krb[g % 2]
        k.op("dve", lambda e, tok=tok: e.tensor_tensor(kr1[:, :], ps[6][0:64, :], cosT[:, tok], ALU.mult), [ps[6], cosT], [kr1])
        k.op("dve", lambda e, tok=tok: e.tensor_tensor(kr2[:, :], ps[3][0:64, :], sinT[:, tok], ALU.mult), [ps[3], sinT], [kr2])
        k.op("pool", lambda e, krbb=krbb: e.tensor_tensor(krbb[:, :], kr1[:, :], kr2[:, :], ALU.add), [kr1, kr2], [krbb])
        k.dma("pool", dr["lat_parts"][2][:, tok] if "lat_parts" in dr else lat_d[256:320, tok], krbb[:, :], krbb, False)
        for h in range(NH):
            pb = ps[4 + (h % 2)]
            for kc in range(3):
                mm(k, pb, pb[:, :], wuq, wuq[:, kc, h * 192:h * 192 + 128], cqT, cqT[:, kc, :], kc == 0, kc == 2)
            qb = qnb[h % 2]
            k.op("act", lambda e, qb=qb, pb=pb: e.copy(qb[:, :], pb[:, :]), [pb], [qb])
            k.dma("pool", q_d[h, 0:128, tok], qb[:, :], qb, False)
            for kc in range(3):
                mm(k, ps[6], ps[6][0:64, :], wuq, wuq[:, kc, h * 192 + 128:h * 192 + 192], cqT, cqT[:, kc, :], kc == 0, kc == 2)
            for kc in range(3):
                mm(k, ps[3], ps[3][0:64, :], wuqrot, wuqrot[:, kc, h, :], cqT, cqT[:, kc, :], kc == 0, kc == 2)
            kb_ = qrb[h % 2]
            k.op("dve", lambda e, tok=tok: e.tensor_tensor(kr1[:, :], ps[6][0:64, :], cosT[:, tok], ALU.mult), [ps[6], cosT], [kr1])
            k.op("dve", lambda e, tok=tok: e.tensor_tensor(kr2[:, :], ps[3][0:64, :], sinT[:, tok], ALU.mult), [ps[3], sinT], [kr2])
            k.op("pool", lambda e, kb_=kb_: e.tensor_tensor(kb_[:, :], kr1[:, :], kr2[:, :], ALU.add), [kr1, kr2], [kb_])
            k.dma("pool", q_d[h, 128:192, tok], kb_[:, :], kb_, False)


def k_q_rope_bufs(k, c):
    if not hasattr(c, "_qrb"):
        c._qrb = [k.sb([64, 512], BF16, "qrb%d" % i) for i in range(2)]
    return c._qrb


def TT(k, eng, ob, o, ab, a, bb, b, op):
    k.op(eng, lambda e: e.tensor_tensor(o, a, b, op), [ab, bb], [ob])


def TS(k, eng, ob, o, ab, a, s1, s2, op0, op1=None, rd=()):
    if op1 is None:
        k.op(eng, lambda e: e.tensor_scalar(o, a, s1, None, op0), [ab] + list(rd), [ob])
    else:
        k.op(eng, lambda e: e.tensor_scalar(o, a, s1, s2, op0, op1), [ab] + list(rd), [ob])


def STT(k, eng, ob, o, ab, a, sc, bb, b, op0, op1, rd=()):
    k.op(eng, lambda e: e.scalar_tensor_tensor(out=o, in0=a, scalar=sc, in1=b, op0=op0, op1=op1), [ab, bb] + list(rd), [ob])


def ACTF(k, ob, o, ib, i, func, scale=1.0, bias=0.0, rd=()):
    k.op("act", lambda e: e.activation(o, i, func, bias=bias, scale=scale), [ib] + list(rd), [ob])


def CP(k, eng, ob, o, ib, i):
    if eng == "act":
        k.op("act", lambda e: e.copy(o, i), [ib], [ob])
    else:
        k.op(eng, lambda e: e.tensor_copy(o, i), [ib], [ob])


def seq_group_owner(G):
    kk, m = G // 8, G % 8
    if m < 4:
        return m, 2 * kk
    return 7 - m, 2 * kk + 1


def phase_attn(k, c, dr):
    ps = c.ps
    psb = [p[:, :].bitcast(BF16) for p in ps]
    mark = k.scope_begin()
    KT = k.sb([128, S], BF16, "KT")
    krT = k.sb([128, S], BF16, "krT")
    V = k.sb([128, 128, 130], BF16, "V")
    masks = k.sb([128, 32, 512], BF16, "masks")
    qn = k.sb([128, TL], BF16, "qn")
    qr = k.sb([128, TL], BF16, "qr")
    latb = [k.sb([128, 2, 512], BF16, "latb%d" % i) for i in range(2)]
    wkv = [k.sb([128, 2, 256], BF16, "wkv%d" % i) for i in range(2)]
    PT = [k.sb([128, 512], BF16, "PT%d" % i) for i in range(8)]
    pacc = [k.sb([128, 512], F32, "pacc%d" % i) for i in range(4)]
    rls = [k.sb([128, 512], F32, "rls%d" % i) for i in range(2)]
    sga = [k.sb([128, 512], BF16, "sga%d" % i) for i in range(4)]
    mxo = [k.sb([128, 512], BF16, "mxo%d" % i) for i in range(2)]
    lat_all = AP_(dr["latT_all"]) if "latT_all" in dr else None
    q_d = AP_(dr["qT"])
    sig_d = AP_(dr["sigT"])
    mix_d = AP_(dr["mixT"])
    wukv = AP_(dr["w_ukv"]).rearrange("(kc p) n -> p kc n", p=128)
    mk_d = AP_(dr["maskbank"])
    for i in range(4):
        k.dma("sp", masks[:, i * 8:(i + 1) * 8, :], mk_d[:, i * 8:(i + 1) * 8, :], masks, True)
    k.op("pool", lambda e: e.memset(krT[64:128, :], 0.0), [], [krT])
    k.op("pool", lambda e: e.memset(qr[64:128, :], 0.0), [], [qr])
    for G in range(32):
        jp, gi = seq_group_owner(G)
        src = dr["lat_all_parts"][2][jp, :, gi * 512:(gi + 1) * 512] if "lat_all_parts" in dr else lat_all[jp, 256:320, gi * 512:(gi + 1) * 512]
        k.dma("sp", krT[0:64, G * 512:(G + 1) * 512], src, krT, True)
    nld = 0
    npt = 0
    nfin = 0
    for h in range(NH):
        wb = wkv[h % 2]
        k.dma("pool", wb[:, :, :], wukv[:, :, h * 256:(h + 1) * 256], wb, True)
        k.dma("sp", qn[:, :], q_d[h, 0:128, :], qn, True)
        k.dma("sp", qr[0:64, :], q_d[h, 128:192, :], qr, True)
        for G in range(32):
            jp, gi = seq_group_owner(G)
            lb = latb[nld % 2]
            nld += 1
            if "lat_all_parts" in dr:
                for kc in range(2):
                    k.dma("sp", lb[:, kc, :], dr["lat_all_parts"][kc][jp, :, gi * 512:(gi + 1) * 512], lb, True)
            else:
                k.dma("sp", lb[:, :, :], lat_all[jp, 0:256, gi * 512:(gi + 1) * 512].rearrange("(kc p) t -> p kc t", p=128), lb, True)
            pk = ps[G % 2]
            for kc in range(2):
                mm(k, pk, pk[:, :], wb, wb[:, kc, 0:128], lb, lb[:, kc, :], kc == 0, kc == 1)
            CP(k, "act" if G % 2 == 0 else "dve", KT, KT[:, G * 512:(G + 1) * 512], pk, pk[:, :])
            pv = ps[2 + G % 2]
            for a in range(4):
                for kc in range(2):
                    mm(k, pv, pv[:, a * 128:(a + 1) * 128], lb, lb[:, kc, a * 128:(a + 1) * 128], wb, wb[:, kc, 128:256], kc == 0, kc == 1)
            CP(k, "dve" if G % 2 == 0 else "act", V, V[:, 4 * G:4 * G + 4, 0:128], pv, pv[:, :].rearrange("p (a d) -> p a d", a=4))
        for SL in ((4, 5, 6, 7), (0, 1, 2, 3)):
            nst = {s_: 32 * (s_ // 2) + 16 + 16 * (s_ % 2) for s_ in SL}
            maxlen = max(nst.values())
            for idx, s_ in enumerate(SL):
                sgb = sga[idx]
                k.dma("sp", sgb[:, :], sig_d[h * 128:(h + 1) * 128, s_ * 512:(s_ + 1) * 512], sgb, True)
            for st in range(maxlen + 1):
                if st < maxlen:
                    act_ = [(idx, s_) for idx, s_ in enumerate(SL) if st < nst[s_]]
                    for idx, s_ in act_:
                        pS = ps[idx]
                        mm(k, pS, pS[:, :], KT, KT[:, st * 128:(st + 1) * 128], qn, qn[:, s_ * 512:(s_ + 1) * 512], True, False)
                    for idx, s_ in act_:
                        pS = ps[idx]
                        mm(k, pS, pS[:, :], krT, krT[:, st * 128:(st + 1) * 128], qr, qr[:, s_ * 512:(s_ + 1) * 512], False, True)
                    for idx, s_ in act_:
                        pS = ps[idx]
                        pt = PT[idx * 2 + st % 2]
                        ACTF(k, pt, pt[:, :], pS, pS[:, :], AF.Exp)
                        sp_ = st - (nst[s_] - 16)
                        if sp_ >= 0:
                            TT(k, "dve", pt, pt[:, :], pt, pt[:, :], masks, masks[:, (s_ % 2) * 16 + sp_, :], ALU.mult)
                if st >= 1:
                    stp = st - 1
                    act_ = [(idx, s_) for idx, s_ in enumerate(SL) if stp < nst[s_]]
                    for idx, s_ in act_:
                        po = ps[4 + idx]
                        pt = PT[idx * 2 + stp % 2]
                        mm(k, po, po[:, :], V, V[:, stp, 0:128], pt, pt[:, :], stp == 0, stp == nst[s_] - 1)
                    for idx, s_ in act_:
                        pt = PT[idx * 2 + stp % 2]
                        pa = pacc[idx]
                        if stp == 0:
                            CP(k, "dve", pa, pa[:, :], pt, pt[:, :])
                        else:
                            TT(k, "dve", pa, pa[:, :], pa, pa[:, :], pt, pt[:, :], ALU.add)
                    for idx, s_ in act_:
                        if stp != nst[s_] - 1:
                            continue
                        po = ps[4 + idx]
                        pa = pacc[idx]
                        sgb = sga[idx]
                        qc = slice(s_ * 512, (s_ + 1) * 512)
                        pl_ = ps[idx]
                        mm(k, pl_, pl_[:, :], c.ones_f, c.ones_f[:, :], pa, pa[:, :], True, True)
                        rl = rls[idx % 2]
                        k.op("dve", lambda e, rl=rl, pl_=pl_: e.reciprocal(rl[:, :], pl_[:, :]), [pl_], [rl])
                        TT(k, "pool", rl, rl[:, :], rl, rl[:, :], sgb, sgb[:, :], ALU.mult)
                        mo = mxo[idx % 2]
                        TT(k, "dve", mo, mo[:, :], po, po[:, :], rl, rl[:, :], ALU.mult)
                        k.dma("pool", mix_d[h * 128:(h + 1) * 128, qc], mo[:, :], mo, False)
    k.scope_end(mark)


def phase_mix(k, c, dr):
    ps = c.ps
    mark = k.scope_begin()
    wpool = k.sb([128, 4, 256], BF16, "wpool")
    k.dma("pool", wpool[:, :, :], AP_(dr["w_pool"]).rearrange("g c d -> c g d"), wpool, True)
    pscale = k.sb([128, 8], F32, "pscale")
    k.dma("sp", pscale[:, :], AP_(dr["pscaleT"])[:, :], pscale, True)
    wout = k.sb([128, 8, D], BF16, "wout")
    woap = AP_(dr["w_out"]).rearrange("(kc p) n -> p kc n", p=128)
    for kc in range(8):
        k.dma("pool", wout[:, kc, :], woap[:, kc, :], wout, True)
    band = k.sb([128, 4, 4, 128], BF16, "band")
    k.dma("sp", band[:, :, :, :], AP_(dr["bands"])[:, :, :, :], band, True)
    sel = k.sb([128, 4, 8, 32], BF16, "sel")
    k.dma("sp", sel[:, :, :, :], AP_(dr["sel"])[:, :, :, :], sel, True)
    ucand = k.sb([128, 4, 512], BF16, "ucand")
    k.dma("sp", ucand[:, :, :], AP_(dr["uhalo_all"]).rearrange("(kt p) c -> p kt c", p=128), ucand, True)
    ug = [k.sb([128, 4, 512], BF16, "ug%d" % i) for i in range(2)]
    halo = k.sb([32, 512], BF16, "halo")
    pTb = [k.sb([128, 512], BF16, "pTb%d" % i) for i in range(2)]
    sgbt = [k.sb([128, 512], BF16, "sgbt%d" % i) for i in range(2)]
    attp = [k.sb([128, 512], BF16, "attp%d" % i) for i in range(2)]
    tmp = [k.sb([128, 512], F32, "ptmp%d" % i) for i in range(2)]
    mixT = [k.sb([128, 8, 512], BF16, "mixT%d" % i) for i in range(2)]
    xt = [k.sb([128, D], F32, "xmt%d" % i) for i in range(2)]
    xo = [k.sb([128, D], F32, "xmo%d" % i) for i in range(2)]
    tmp2 = k.sb([128, D], F32, "tmp2")
    u_d = AP_(dr["u_local"])
    sig_d = AP_(dr["sigT"])
    mix_d = AP_(dr["mixT"])
    x_d = AP_(dr["x"])
    x1_d = AP_(dr["x1"])
    nf = 0
    for gi in range(NG):
        qc = slice(gi * 512, (gi + 1) * 512)
        ugb = ug[gi % 2]
        k.dma("sp", ugb[:, :, :], u_d[qc, :].rearrange("(t p) c -> p t c", p=128), ugb, True)
        ph = ps[0]
        for kt in range(4):
            mm(k, ph, ph[0:32, :], sel, sel[:, kt, gi, :], ucand, ucand[:, kt, :], kt == 0, kt == 3)
        CP(k, "act", halo, halo[:, :], ph, ph[0:32, :])
        mxb = mixT[gi % 2]
        for g4 in range(4):
            pp = ps[1 + g4 % 2]
            for tt in range(4):
                bk = 1 if (gi == 0 and tt == 0) else 0
                mm(k, pp, pp[:, tt * 128:(tt + 1) * 128], ugb, ugb[:, tt, g4 * 128:(g4 + 1) * 128], band, band[:, bk, g4, :], True, False)
                if tt == 0:
                    mm(k, pp, pp[:, 0:128], halo, halo[0:32, g4 * 128:(g4 + 1) * 128], band, band[0:32, 3, g4, :], False, True)
                else:
                    mm(k, pp, pp[:, tt * 128:(tt + 1) * 128], ugb, ugb[64:128, tt - 1, g4 * 128:(g4 + 1) * 128], band, band[64:128, 2, g4, :], False, True)
            pb_ = pTb[g4 % 2]
            CP(k, "act", pb_, pb_[:, :], pp, pp[:, :])
            for half in range(2):
                f = 2 * g4 + half
                po = ps[3 + half]
                mm(k, po, po[:, :], wpool, wpool[:, g4, half * 128:(half + 1) * 128], pb_, pb_[:, :], True, True)
                sb_ = sgbt[nf % 2]
                ap_ = attp[nf % 2]
                tp_ = tmp[nf % 2]
                nf += 1
                k.dma("sp", sb_[:, :], sig_d[1024 + f * 128:1024 + (f + 1) * 128, qc], sb_, True)
                k.dma("sp", ap_[:, :], mix_d[f * 128:(f + 1) * 128, qc], ap_, True)
                STT(k, "dve", tp_, tp_[:, :], po, po[:, :], pscale[:, f:f + 1], sb_, sb_[:, :], ALU.mult, ALU.mult, rd=[pscale])
                TT(k, "pool", mxb, mxb[:, f, :], tp_, tp_[:, :], ap_, ap_[:, :], ALU.add)
        for tt in range(4):
            t = gi * 4 + tt
            xb = xt[t % 2]
            xob = xo[t % 2]
            k.dma("sp", xb[:, :], x_d[t * 128:(t + 1) * 128, :], xb, True)
            for cg in range(2):
                po = ps[5 + cg]
                for f in range(8):
                    mm(k, po, po[:, :], mxb, mxb[:, f, tt * 128:(tt + 1) * 128], wout, wout[:, f, cg * 512:(cg + 1) * 512], f == 0, f == 7)
                TT(k, "dve", tmp2, tmp2[:, cg * 512:(cg + 1) * 512], po, po[:, :], c.mod, c.mod[:, 2 * D + cg * 512:2 * D + (cg + 1) * 512], ALU.mult)
            TT(k, "pool", xob, xob[:, :], tmp2, tmp2[:, :], xb, xb[:, :], ALU.add)
            k.dma("pool", x1_d[t * 128:(t + 1) * 128, :], xob[:, :], xob, False)
    k.scope_end(mark)


def phase_moe(k, c, dr, final):
    ps = c.ps
    mark = k.scope_begin()
    A2 = k.sb([128, D], F32, "A2")
    k.dma("sp", A2[:, :], AP_(dr["norm_ffn_rep"])[:, :], A2, True)
    STT(k, "dve", A2, A2[:, :], c.mod, c.mod[:, 4 * D:5 * D], 1.0, A2, A2[:, :], ALU.add, ALU.mult)
    wr = k.sb([128, 8, NE], F32, "wr")
    k.dma("sp", wr[:, :, :], AP_(dr["w_router"]).rearrange("(kc p) n -> p kc n", p=128), wr, True)
    brep = k.sb([128, NE], F32, "brep")
    k.dma("sp", brep[:, :], AP_(dr["b_router_rep"])[:, :], brep, True)
    bgu = k.sb([128, NE, 16], F32, "bgu")
    k.dma("sp", bgu[:, :, :], AP_(dr["b_guT"])[:, :, :], bgu, True)
    TS(k, "dve", bgu, bgu[:, :, 8:16], bgu, bgu[:, :, 8:16], 1.0, None, ALU.add)
    if final:
        nfr = k.sb([128, D], F32, "nfr")
        k.dma("sp", nfr[:, :], AP_(dr["norm_final_rep"])[:, :], nfr, True)
    NT = 8
    NSG = NT // 4
    acc = k.sb([128, NT, D], F32, "acc")
    h2T = k.sb([128, 8, NT * 128], BF16, "h2T")
    Gq = k.sb([128, NT, NE], F32, "Gq")
    wg = k.sb([128, 8, 2 * DFF], BF16, "wgu")
    wd = k.sb([128, 8, D], BF16, "wdn")
    bdf = k.sb([1, D], F32, "bdf")
    bd = k.sb([1, D], BF16, "bdn")
    actT = [k.sb([128, 8, 512], BF16, "actT%d" % i) for i in range(NSG)]
    g1s = [k.sb([128, 512], F32, "g1_%d" % i) for i in range(2)]
    sgms = [k.sb([128, 512], F32, "sgm_%d" % i) for i in range(2)]
    l1s = [k.sb([128, 512], F32, "l1_%d" % i) for i in range(2)]
    nch = 0
    ss = k.sb([128, 1], F32, "mss")
    rstd = k.sb([128, 1], F32, "mrstd")
    x1_d = AP_(dr["x1"])
    xo_d = AP_(dr["x_out"])
    wgu_d = AP_(dr["w_gu"])
    wdn_d = AP_(dr["w_down"])
    bdn_d = AP_(dr["b_down"])
    NQ = TL // (NT * 128)
    ncast = 0

    def load_wgu(ei, stg):
        nonlocal ncast
        wga = wgu_d[ei].rearrange("(kc p) n -> p kc n", p=128)
        for kc in range(8):
            sb_ = stg[ncast % len(stg)]
            k.dma("sp", sb_[:, :], wga[:, kc, :], sb_, True)
            CP(k, "act" if ncast % 3 != 2 else "dve", wg, wg[:, kc, :], sb_, sb_[:, :])
            ncast += 1

    def load_wdn(ei, stg):
        nonlocal ncast
        wda = wdn_d[ei].rearrange("(kc p) n -> p kc n", p=128)
        for kc in range(0, 8, 2):
            sb_ = stg[ncast % len(stg)]
            k.dma("sp", sb_[:, :].rearrange("p (a n) -> p a n", a=2), wda[:, kc:kc + 2, :], sb_, True)
            CP(k, "act" if ncast % 3 != 2 else "dve", wd, wd[:, kc:kc + 2, :], sb_, sb_[:, :].rearrange("p (a n) -> p a n", a=2))
            ncast += 1
        k.dma("sp", bdf[:, :], bdn_d[ei:ei + 1, :], bdf, True)
        CP(k, "pool", bd, bd[:, :], bdf, bdf[:, :])

    for q in range(NQ):
        m2 = k.scope_begin()
        xt = [k.sb([128, D], F32, "xq%d" % i) for i in range(2)]
        h2 = k.sb([128, D], F32, "h2")
        h2Tf = k.sb([128, 8, 128], F32, "h2Tf")
        c.junkb = k.sb([128, D], BF16, "junk2")
        lg = k.sb([128, NE], F32, "lg")
        top8 = k.sb([128, 8], F32, "top8")
        negm = k.sb([128, 1], F32, "negm")
        msk = k.sb([128, NE], F32, "msk")
        ex = k.sb([128, NE], F32, "ex")
        sm = k.sb([128, 1], F32, "sm")
        for T in range(NT):
            t = q * NT + T
            xb = xt[t % 2]
            k.dma("sp", xb[:, :], x1_d[t * 128:(t + 1) * 128, :], xb, True)
            rms_rstd(k, c, xb, xb[:, :], D, c.junkb[:, :], ss, rstd)
            STT(k, "dve", h2, h2[:, :], xb, xb[:, :], rstd[:, 0:1], A2, A2[:, :], ALU.mult, ALU.mult, rd=[rstd])
            TT(k, "pool", h2, h2[:, :], h2, h2[:, :], c.mod, c.mod[:, 3 * D:4 * D], ALU.add)
            for kc in range(8):
                pb = ps[kc // 4]
                tr(k, pb, pb[:, (kc % 4) * 128:(kc % 4 + 1) * 128], h2, h2[:, kc * 128:(kc + 1) * 128], c.ident_f, c.ident_f[:, :])
            for hf in range(2):
                CP(k, "act", h2Tf, h2Tf[:, hf * 4:(hf + 1) * 4, :], ps[hf], ps[hf][:, :].rearrange("p (k t) -> p k t", k=4))
                CP(k, "dve", h2T, h2T[:, hf * 4:(hf + 1) * 4, T * 128:(T + 1) * 128], h2Tf, h2Tf[:, hf * 4:(hf + 1) * 4, :])
            pr = ps[2]
            for kc in range(8):
                mm(k, pr, pr[:, 0:NE], h2Tf, h2Tf[:, kc, :], wr, wr[:, kc, :], kc == 0, kc == 7)
            TT(k, "dve", lg, lg[:, :], pr, pr[:, 0:NE], brep, brep[:, :], ALU.add)
            k.op("dve", lambda e, top8=top8, lg=lg: e.max(top8[:, :], lg[:, :]), [lg], [top8])
            TS(k, "dve", negm, negm[:, :], top8, top8[:, 0:1], -1.0, None, ALU.mult)
            TS(k, "dve", msk, msk[:, :], lg, lg[:, :], top8[:, 3:4], None, ALU.is_ge, rd=[top8])
            ACTF(k, ex, ex[:, :], lg, lg[:, :], AF.Exp, bias=negm[:, 0:1], rd=[negm])
            TT(k, "dve", ex, ex[:, :], ex, ex[:, :], msk, msk[:, :], ALU.mult)
            k.op("dve", lambda e, sm=sm, ex=ex: e.reduce_sum(sm[:, :], ex[:, :], axis=AX.X), [ex], [sm])
            k.op("dve", lambda e, sm=sm: e.reciprocal(sm[:, :], sm[:, :]), [sm], [sm])
            TS(k, "dve", Gq, Gq[:, T, :], ex, ex[:, :], sm[:, 0:1], None, ALU.mult, rd=[sm])
        k.op("pool", lambda e: e.memset(acc[:, :, :], 0.0), [], [acc])
        k.scope_end(m2)
        m2 = k.scope_begin()
        stg = [k.sb([128, 2 * DFF], F32, "stg%d" % i) for i in range(5)]
        ne_run = NE_RUN if MOE_DBG == 0 else 0
        if ne_run:
            load_wgu(0, stg)
            load_wdn(0, stg)
        for ei in range(ne_run):
            for f in range(8):
                base = 4 * (f % 2)
                for half in range(2):
                    for kc in range(8):
                        for sg_ in range(NSG):
                            pb = ps[base + 2 * half + sg_]
                            mm(k, pb, pb[:, :], wg, wg[:, kc, half * DFF + f * 128:half * DFF + (f + 1) * 128],
                               h2T, h2T[:, kc, sg_ * 512:(sg_ + 1) * 512], kc == 0, kc == 7)
                for sg_ in range(NSG):
                    pg = ps[base + sg_]
                    pl = ps[base + 2 + sg_]
                    aT = actT[sg_]
                    g1 = g1s[nch % 2]
                    sgm = sgms[nch % 2]
                    l1 = l1s[nch % 2]
                    nch += 1
                    TS(k, "dve", g1, g1[:, :], pg, pg[:, :], bgu[:, ei, f:f + 1], 7.0, ALU.add, ALU.min, rd=[bgu])
                    ACTF(k, sgm, sgm[:, :], g1, g1[:, :], AF.Sigmoid, scale=1.702)
                    TS(k, "dve", l1, l1[:, :], pl, pl[:, :], bgu[:, ei, 8 + f:9 + f], 8.0, ALU.add, ALU.min, rd=[bgu])
                    TT(k, "pool", sgm, sgm[:, :], g1, g1[:, :], sgm, sgm[:, :], ALU.mult)
                    STT(k, "dve", aT, aT[:, f, :], l1, l1[:, :], -6.0, sgm, sgm[:, :], ALU.max, ALU.mult)
            if ei + 1 < ne_run:
                load_wgu(ei + 1, stg)
            for sg_ in range(NSG):
                aT = actT[sg_]
                for tt in range(4):
                    T = sg_ * 4 + tt
                    pyb = 2 * (T % 2)
                    for cg in range(2):
                        py = ps[pyb + cg]
                        mm(k, py, py[:, :], c.ones_b, c.ones_b[0:1, :], bd, bd[0:1, cg * 512:(cg + 1) * 512], True, False)
                    for f in range(8):
                        for cg in range(2):
                            py = ps[pyb + cg]
                            mm(k, py, py[:, :], aT, aT[:, f, tt * 128:(tt + 1) * 128], wd, wd[:, f, cg * 512:(cg + 1) * 512], False, f == 7)
                    for cg in range(2):
                        py = ps[pyb + cg]
                        STT(k, "dve", acc, acc[:, T, cg * 512:(cg + 1) * 512], py, py[:, :], Gq[:, T, ei:ei + 1], acc,
                            acc[:, T, cg * 512:(cg + 1) * 512], ALU.mult, ALU.add, rd=[Gq])
            if ei + 1 < ne_run:
                load_wdn(ei + 1, stg)
        k.scope_end(m2)
        m2 = k.scope_begin()
        xt = [k.sb([128, D], F32, "xr%d" % i) for i in range(2)]
        ho = [k.sb([128, D], F32, "ho%d" % i) for i in range(2)]
        c.junkb = k.sb([128, D], BF16, "junk3")
        for T in range(NT):
            t = q * NT + T
            xb = xt[t % 2]
            h2 = ho[t % 2]
            k.dma("sp", xb[:, :], x1_d[t * 128:(t + 1) * 128, :], xb, True)
            TT(k, "dve", h2, h2[:, :], acc, acc[:, T, :], c.mod, c.mod[:, 5 * D:6 * D], ALU.mult)
            TT(k, "pool", h2, h2[:, :], h2, h2[:, :], xb, xb[:, :], ALU.add)
            if final:
                rms_rstd(k, c, h2, h2[:, :], D, c.junkb[:, :], ss, rstd)
                STT(k, "dve", h2, h2[:, :], h2, h2[:, :], rstd[:, 0:1], nfr, nfr[:, :], ALU.mult, ALU.mult, rd=[rstd])
            k.dma("pool", xo_d[t * 128:(t + 1) * 128, :], h2[:, :], h2, False)
        k.scope_end(m2)
    k.scope_end(mark)


def core_groups(j):
    out = []
    for kk in range(4):
        out.append(8 * kk + j)
        out.append(8 * kk + 7 - j)
    return out


def local_token_index(j):
    idx = np.concatenate([np.arange(G * 512, (G + 1) * 512) for G in core_groups(j)])
    return idx


def rep(v, n=128):
    return np.ascontiguousarray(np.broadcast_to(np.asarray(v, np.float32).reshape(1, -1), (n, v.size)))


def build_pre(layer_has=None):
    nc = bass.Bass("TRN2", target_bir_lowering=False)
    dr = {}
    dr["ident"] = dram_in(nc, "ident", [128, 128], F32)
    dr["cT"] = dram_in(nc, "cT", [128, 8], F32)
    dr["ada_b_rep"] = dram_in(nc, "ada_b_rep", [128, 6 * D], F32)
    dr["ada_w"] = dram_in(nc, "ada_w", [D, 6 * D], F32)
    dr["w_in"] = dram_in(nc, "w_in", [D, INC], F32)
    dr["w_uq"] = dram_in(nc, "w_uq", [QLORA, 1536], F32)
    dr["norm_mix_rep"] = dram_in(nc, "norm_mix_rep", [128, D], F32)
    dr["q_norm_rep"] = dram_in(nc, "q_norm_rep", [128, QLORA], F32)
    dr["kv_norm_rep"] = dram_in(nc, "kv_norm_rep", [128, KVLORA], F32)
    dr["pos_rep"] = dram_in(nc, "pos_rep", [64, TL], I32)
    dr["inv_freq"] = dram_in(nc, "inv_freq", [64, 1], F32)
    dr["x"] = dram_in(nc, "x", [TL, D], F32)
    dr["u_local"] = dram_out(nc, "u_local", [TL, 512], BF16)
    dr["sigT"] = dram_out(nc, "sigT", [2048, TL], BF16)
    dr["latT_local"] = dram_out(nc, "latT_local", [320, TL], BF16)
    dr["qT"] = dram_out(nc, "qT", [NH, 192, TL], BF16)
    k = KB(nc)
    c = Ctx()
    setup_common(k, c, dr)
    compute_mod(k, c, dr)
    phase_pre(k, c, dr)
    k.barrier()
    cnt = k.finish()
    return nc, cnt


def build_post(final, phases=(1, 2, 3), debug=False):
    nc = bass.Bass("TRN2", target_bir_lowering=False)
    dr = {}
    def din(name, shape, dt=F32):
        dr[name] = dram_in(nc, name, shape, dt)
    din("ident", [128, 128])
    din("cT", [128, 8])
    din("ada_b_rep", [128, 6 * D])
    din("ada_w", [D, 6 * D])
    din("latT_all", [4, 320, TL], BF16)
    din("qT", [NH, 192, TL], BF16)
    din("sigT", [2048, TL], BF16)
    din("u_local", [TL, 512], BF16)
    din("uhalo_all", [512, 512], BF16)
    din("maskbank", [128, 32, 512], BF16)
    din("bands", [128, 4, 4, 128], BF16)
    din("sel", [128, 4, 8, 32], BF16)
    din("x", [TL, D])
    din("w_ukv", [KVLORA, 2048])
    din("w_pool", [4, 128, 256])
    din("pscaleT", [128, 8])
    din("w_out", [D, D])
    din("norm_ffn_rep", [128, D])
    din("w_router", [D, NE])
    din("b_router_rep", [128, NE])
    din("b_guT", [128, NE, 16])
    din("w_gu", [NE, D, 2 * DFF])
    din("w_down", [NE, DFF, D])
    din("b_down", [NE, D])
    if final:
        din("norm_final_rep", [128, D])
    mk = dram_out if debug else dram_tmp
    dr["mixT"] = mk(nc, "mixT", [D, TL], BF16)
    dr["x1"] = mk(nc, "x1", [TL, D], F32)
    dr["x_out"] = dram_out(nc, "x_out", [TL, D], F32)
    k = KB(nc)
    c = Ctx()
    setup_common(k, c, dr)
    compute_mod(k, c, dr)
    if 1 in phases:
        phase_attn(k, c, dr)
    if 2 in phases:
        phase_mix(k, c, dr)
    if 3 in phases:
        phase_moe(k, c, dr, final)
    k.barrier()
    cnt = k.finish()
    return nc, cnt


POOL_W = (2, 4, 8, 16)
_bf = ml_dtypes.bfloat16


def host_consts(j):
    bands = np.zeros((128, 4, 4, 128), np.float32)
    tp = np.arange(128)[:, None]
    t = np.arange(128)[None, :]
    for g, w in enumerate(POOL_W):
        inwin = (tp <= t) & (tp > t - w)
        bands[:, 0, g, :] = inwin / float(w) - (tp == t)
        if j == 0:
            cntv = np.minimum(t + 1, w).astype(np.float32)
            bands[:, 1, g, :] = inwin / cntv - (tp == t)
        else:
            bands[:, 1, g, :] = bands[:, 0, g, :]
        tprev = tp - 128
        bands[:, 2, g, :] = ((tprev > t - w) & (tp >= 96)) / float(w)
        th = tp - 32
        bands[:, 3, g, :] = ((th > t - w) & (tp >= 16) & (tp < 32)) / float(w)
    sel = np.zeros((4, 128, 8, 32), np.float32)
    groups = core_groups(j)
    for gi, G in enumerate(groups):
        if G == 0:
            continue
        jp, gip = seq_group_owner(G - 1)
        base = (jp * 8 + gip) * 16
        for r in range(16):
            R = base + r
            sel[R // 128, R % 128, gi, 16 + r] = 1.0
    sel = np.ascontiguousarray(sel.transpose(1, 0, 2, 3))
    mb = np.zeros((128, 32, 512), np.float32)
    p = np.arange(128)[:, None]
    cc = np.arange(512)[None, :]
    for side in range(2):
        for sp in range(16):
            rel = sp - 4 * j if side == 0 else sp - 12 + 4 * j
            mb[:, side * 16 + sp, :] = (rel * 128 + p <= cc)
    return dict(bands=bands.astype(_bf), sel=sel.astype(_bf), maskbank=mb.astype(_bf))


def pre_inputs(inp, L, r, xloc):
    b, j = r // 4, r % 4
    idx = local_token_index(j)
    half = ROPE // 2
    inv_freq = (10000.0 ** (-np.arange(half, dtype=np.float32) / half)).astype(np.float32)
    return dict(
        ident=np.eye(128, dtype=np.float32),
        cT=np.ascontiguousarray(inp["c"][b].reshape(8, 128).T),
        ada_b_rep=rep(inp["ada_b"][L]),
        ada_w=np.ascontiguousarray(inp["ada_w"][L]),
        w_in=np.ascontiguousarray(inp["w_in"][L]),
        w_uq=np.ascontiguousarray(inp["w_uq"][L]),
        norm_mix_rep=rep(inp["norm_mix"][L]),
        q_norm_rep=rep(inp["q_norm"][L]),
        kv_norm_rep=rep(inp["kv_norm"][L]),
        pos_rep=np.ascontiguousarray(np.broadcast_to(inp["positions"][b][idx][None, :], (64, TL))).astype(np.int32),
        inv_freq=np.concatenate([inv_freq, inv_freq]).reshape(64, 1).astype(np.float32),
        x=xloc,
    )


def post_inputs(inp, L, r, xloc, pre_out, final, consts):
    b, j = r // 4, r % 4
    ranks = [b * 4 + i for i in range(4)]
    lat_all = np.ascontiguousarray(np.stack([pre_out[q]["latT_local"] for q in ranks], 0))
    uh = np.concatenate([pre_out[q]["u_local"].reshape(8, 512, 512)[:, 496:512, :].reshape(128, 512) for q in ranks], 0)
    d = dict(
        ident=np.eye(128, dtype=np.float32),
        cT=np.ascontiguousarray(inp["c"][b].reshape(8, 128).T),
        ada_b_rep=rep(inp["ada_b"][L]),
        ada_w=np.ascontiguousarray(inp["ada_w"][L]),
        latT_all=lat_all,
        qT=pre_out[r]["qT"],
        sigT=pre_out[r]["sigT"],
        u_local=pre_out[r]["u_local"],
        uhalo_all=np.ascontiguousarray(uh),
        maskbank=consts[j]["maskbank"],
        bands=consts[j]["bands"],
        sel=consts[j]["sel"],
        x=xloc,
        w_ukv=np.ascontiguousarray(inp["w_ukv"][L]),
        w_pool=np.ascontiguousarray(inp["w_pool"][L]),
        pscaleT=np.ascontiguousarray(inp["pool_scale"][L].reshape(8, 128).T),
        w_out=np.ascontiguousarray(inp["w_out"][L]),
        norm_ffn_rep=rep(inp["norm_ffn"][L]),
        w_router=np.ascontiguousarray(inp["w_router"][L]),
        b_router_rep=rep(inp["b_router"][L]),
        b_guT=np.ascontiguousarray(inp["b_gu"][L].reshape(NE, 16, 128).transpose(2, 0, 1)),
        w_gu=np.ascontiguousarray(inp["w_gu"][L]),
        w_down=np.ascontiguousarray(inp["w_down"][L]),
        b_down=np.ascontiguousarray(inp["b_down"][L]),
    )
    if final:
        d["norm_final_rep"] = rep(inp["norm_final"])
    return d


LAYERED = dict(
    ada_b_rep=[128, 6 * D], ada_w=[D, 6 * D], w_in=[D, INC], w_uq=[QLORA, 1536], norm_mix_rep=[128, D],
    q_norm_rep=[128, QLORA], kv_norm_rep=[128, KVLORA], w_ukv=[KVLORA, 2048], w_pool=[4, 128, 256],
    pscaleT=[128, 8], w_out=[D, D], norm_ffn_rep=[128, D], w_router=[D, NE], b_router_rep=[128, NE],
    b_guT=[128, NE, 16], w_gu=[NE, D, 2 * DFF], w_down=[NE, DFF, D], b_down=[NE, D])
SHARED = dict(ident=([128, 128], F32), cT=([128, 8], F32), pos_rep=([64, TL], I32), inv_freq=([64, 1], F32),
              maskbank=([128, 32, 512], BF16), bands=([128, 4, 4, 128], BF16), sel=([128, 4, 8, 32], BF16),
              norm_final_rep=([128, D], F32), x=([TL, D], F32))
GROUPS = [[0, 1, 2, 3], [4, 5, 6, 7]]
BIG = ("w_gu", "w_down")


def build_fused():
    nc = bass.Bass("TRN2", target_bir_lowering=False)
    hin = {}
    for n, sh in LAYERED.items():
        if n in BIG:
            hin[n] = [dram_in(nc, n + "%d" % L, sh, F32) for L in range(2)]
        else:
            hin[n] = dram_in(nc, n, [2] + sh, F32)
    for n, (sh, dt) in SHARED.items():
        hin[n] = dram_in(nc, n, sh, dt)
    out = dram_out(nc, "out", [TL, D], F32)
    tmp = dict(
        u_local=dram_tmp(nc, "u_local", [TL, 512], BF16), sigT=dram_tmp(nc, "sigT", [2048, TL], BF16),
        qT=dram_tmp(nc, "qT", [NH, 192, TL], BF16),
        uh_local=dram_tmp(nc, "uh_local", [128, 512], BF16),
        uhalo_all=dram_tmp(nc, "uhalo_all", [512, 512], BF16), mixT=dram_tmp(nc, "mixT", [D, TL], BF16),
        x1=dram_tmp(nc, "x1", [TL, D], F32), xbuf=dram_tmp(nc, "xbuf", [TL, D], F32))
    PR = (128, 128, 64)
    lat_loc = [dram_tmp(nc, "lat%d" % i, [PR[i], TL], BF16) for i in range(3)]
    lat_all_t = [dram_tmp(nc, "lata%d" % i, [4 * PR[i], TL], BF16) for i in range(3)]
    k = KB(nc)
    c = Ctx()
    setup_common(k, c, hin)
    for L in range(2):
        d = {n: (hin[n][L].ap() if n in BIG else hin[n].ap()[L]) for n in LAYERED}
        for n in SHARED:
            d[n] = hin[n].ap()
        for n in tmp:
            d[n] = tmp[n].ap()
        d["lat_parts"] = [t_.ap() for t_ in lat_loc]
        d["lat_all_parts"] = [t_.ap().rearrange("(r f) t -> r f t", r=4) for t_ in lat_all_t]
        d["x"] = hin["x"].ap() if L == 0 else tmp["xbuf"].ap()
        d["x_out"] = tmp["xbuf"].ap() if L == 0 else out.ap()
        compute_mod(k, c, d)
        m = k.scope_begin()
        phase_pre(k, c, d)
        k.scope_end(m)
        if FUSE_DBG != 1:
            for t_i, t_o in zip(lat_loc, lat_all_t):
                k.coll("AllGather", t_i.ap(), t_o.ap(), GROUPS)
            k.coll("AllGather", tmp["uh_local"].ap(), tmp["uhalo_all"].ap(), GROUPS)
        k.barrier()
        phase_attn(k, c, d)
        phase_mix(k, c, d)
        phase_moe(k, c, d, L == 1)
    k.barrier()
    cnt = k.finish()
    return nc, cnt


def fused_inputs(inp, r, consts):
    b, j = r // 4, r % 4
    idx = local_token_index(j)
    half = ROPE // 2
    inv_freq = (10000.0 ** (-np.arange(half, dtype=np.float32) / half)).astype(np.float32)
    st = lambda f: np.ascontiguousarray(np.stack([f(L) for L in range(2)], 0))
    d = dict(
        ada_b_rep=st(lambda L: rep(inp["ada_b"][L])), ada_w=np.ascontiguousarray(inp["ada_w"]),
        w_in=np.ascontiguousarray(inp["w_in"]), w_uq=np.ascontiguousarray(inp["w_uq"]),
        norm_mix_rep=st(lambda L: rep(inp["norm_mix"][L])), q_norm_rep=st(lambda L: rep(inp["q_norm"][L])),
        kv_norm_rep=st(lambda L: rep(inp["kv_norm"][L])), w_ukv=np.ascontiguousarray(inp["w_ukv"]),
        w_pool=np.ascontiguousarray(inp["w_pool"]),
        pscaleT=st(lambda L: inp["pool_scale"][L].reshape(8, 128).T),
        w_out=np.ascontiguousarray(inp["w_out"]), norm_ffn_rep=st(lambda L: rep(inp["norm_ffn"][L])),
        w_router=np.ascontiguousarray(inp["w_router"]), b_router_rep=st(lambda L: rep(inp["b_router"][L])),
        b_guT=st(lambda L: inp["b_gu"][L].reshape(NE, 16, 128).transpose(2, 0, 1)),
        w_gu0=np.ascontiguousarray(inp["w_gu"][0]), w_gu1=np.ascontiguousarray(inp["w_gu"][1]),
        w_down0=np.ascontiguousarray(inp["w_down"][0]), w_down1=np.ascontiguousarray(inp["w_down"][1]),
        b_down=np.ascontiguousarray(inp["b_down"]),
        ident=np.eye(128, dtype=np.float32),
        cT=np.ascontiguousarray(inp["c"][b].reshape(8, 128).T),
        pos_rep=np.ascontiguousarray(np.broadcast_to(inp["positions"][b][idx][None, :], (64, TL))).astype(np.int32),
        inv_freq=np.concatenate([inv_freq, inv_freq]).reshape(64, 1).astype(np.float32),
        maskbank=consts[j]["maskbank"], bands=consts[j]["bands"], sel=consts[j]["sel"],
        norm_final_rep=rep(inp["norm_final"]),
        x=np.ascontiguousarray(inp["x"][b][idx]),
    )
    return d


_CACHE = {}


def kernel(**inputs):
    inp = {k_: np.asarray(v) for k_, v in inputs.items()}
    if "fused" not in _CACHE:
        _CACHE["fused"] = build_fused()[0]
    consts = [host_consts(j) for j in range(4)]
    cores = list(range(8))
    maps = [fused_inputs(inp, r, consts) for r in cores]
    res = run_bass_kernel_spmd(_CACHE["fused"], maps, core_ids=cores)
    out = np.empty((2, S, D), np.float32)
    for r in cores:
        out[r // 4][local_token_index(r % 4)] = res.results[r]["out"]
    return out
```

```python
import math
import numpy as np
import ml_dtypes
import concourse.bass as bass
import concourse.mybir as mybir
from concourse.bass_utils import run_bass_kernel_spmd

F32 = mybir.dt.float32
BF16 = mybir.dt.bfloat16
I32 = mybir.dt.int32
ALU = mybir.AluOpType
AF = mybir.ActivationFunctionType
AX = mybir.AxisListType

D = 1024
S = 16384
NH = 8
QLORA = 384
KVLORA = 256
ROPE = 64
INC = 3264
NE = 32
DFF = 1024
EPS = 1e-6
TL = 4096
NG = 8
SCALE = 1.0 / math.sqrt(192.0)
SAME_SYNC = True
MOE_DBG = 0
NE_RUN = 32
FUSE_DBG = 0


class Buf:
    def __init__(self, t, name):
        self.t = t
        self.name = name
        self.w = None
        self.rs = []
        self.dsems = {}

    def __getitem__(self, idx):
        return self.t[idx]


class Op:
    __slots__ = ("stream", "fn", "reads", "writes", "dma", "dbuf", "deps", "sig", "signal", "barrier", "idx", "inc", "release")

    def __init__(self, stream, fn, reads, writes, dma=False, dbuf=None, barrier=False):
        self.stream = stream
        self.fn = fn
        self.reads = reads
        self.writes = writes
        self.dma = dma
        self.dbuf = dbuf
        self.deps = []
        self.sig = False
        self.signal = None
        self.barrier = barrier
        self.inc = 16
        self.release = None


class KB:
    STREAMS = ("pe", "act", "dve", "pool", "sp")

    def __init__(self, nc):
        self.nc = nc
        self.ops = []
        self.eng = dict(pe=nc.tensor, act=nc.scalar, dve=nc.vector, pool=nc.gpsimd, sp=nc.sync)
        self.bufs = []
        self.guards = []
        self.gbufs = []
        self.n = 0

    def sb(self, shape, dt, name=None):
        self.n += 1
        name = (name or "t") + "_%d" % self.n
        g = self.nc.sbuf_tensor(name, list(shape), dt)
        b = Buf(g.__enter__(), name)
        self.guards.append(g)
        self.gbufs.append((g, b))
        self.bufs.append(b)
        return b

    def scope_begin(self):
        return len(self.guards)

    def scope_end(self, mark):
        self.barrier()
        rel = Op(None, None, [], [])
        rel.release = [b for (_, b) in self.gbufs[mark:]]
        self.ops.append(rel)
        while len(self.guards) > mark:
            self.guards.pop().__exit__(None, None, None)
            self.gbufs.pop()

    def coll(self, kind, in_ap, out_ap, groups):
        b = Buf(None, "cc%d" % self.n)
        self.n += 1
        fn = lambda e: e.collective_compute(kind, ALU.bypass, replica_groups=groups, ins=[in_ap], outs=[out_ap])
        o = Op("pool", fn, [], [b], dma=True, dbuf=b)
        o.inc = 1
        self.ops.append(o)

    def ps(self, shape, dt=F32, name=None):
        self.n += 1
        name = (name or "p") + "_%d" % self.n
        b = Buf(self.nc.alloc_psum_tensor(name, list(shape), dt), name)
        self.bufs.append(b)
        return b

    def op(self, stream, fn, reads=(), writes=()):
        self.ops.append(Op(stream, fn, list(reads), list(writes)))

    def dma(self, q, out_ap, in_ap, buf, load, extra_reads=(), **kw):
        fn = lambda e: e.dma_start(out=out_ap, in_=in_ap, **kw)
        if load:
            o = Op(q, fn, list(extra_reads), [buf], dma=True, dbuf=buf)
        else:
            o = Op(q, fn, [buf] + list(extra_reads), [], dma=True, dbuf=buf)
        self.ops.append(o)

    def barrier(self):
        self.ops.append(Op(None, None, [], [], barrier=True))

    def finish(self):
        nc = self.nc
        last = {s: None for s in self.STREAMS}
        dma_last = {}
        expanded = []
        for o in self.ops:
            if o.release is not None:
                for b in o.release:
                    for kk_ in ("sp", "pool", "cc"):
                        dma_last.pop((id(b), kk_), None)
                expanded.append(o)
                continue
            if o.barrier:
                for s in self.STREAMS:
                    p = Op(s, None, [], [])
                    p.idx = len(expanded)
                    deps = [last[s2] for s2 in self.STREAMS if s2 != s and last[s2] is not None and not last[s2].dma]
                    deps += list(dma_last.values())
                    p.deps = deps
                    for d in deps:
                        if not d.dma:
                            d.sig = True
                    expanded.append(p)
                continue
            deps = set()
            for b in o.reads:
                if b.w is not None:
                    deps.add(b.w)
            for b in o.writes:
                if b.w is not None:
                    deps.add(b.w)
                for r in b.rs:
                    deps.add(r)
            deps.discard(o)
            for b in o.writes:
                b.w = o
                b.rs = []
            for b in o.reads:
                if b not in o.writes:
                    b.rs.append(o)
            fin = []
            best = {}
            for d in deps:
                if d.dma:
                    fin.append(d)
                    continue
                if d.stream == o.stream and (o.stream == "pe" or not SAME_SYNC):
                    continue
                if d.stream not in best or best[d.stream].idx < d.idx:
                    best[d.stream] = d
            for d in best.values():
                d.sig = True
                fin.append(d)
            o.deps = fin
            o.idx = len(expanded)
            last[o.stream] = o
            if o.dma:
                dma_last[(id(o.dbuf), "cc" if o.inc == 1 else o.stream)] = o
            expanded.append(o)
        sems = {s: nc.alloc_semaphore("s_" + s) for s in self.STREAMS}
        cnt = {s: 0 for s in self.STREAMS}
        seen = {s: {} for s in self.STREAMS}
        free_sems = {}
        for o in expanded:
            if o.release is not None:
                for b in o.release:
                    for kind, (sem_, cnt_) in b.dsems.items():
                        free_sems.setdefault(kind, []).append((sem_, cnt_))
                    b.dsems = {}
                continue
            e = self.eng[o.stream]
            need = {}
            for d in o.deps:
                sem, val = d.signal
                k = id(sem)
                if k not in need or need[k][1] < val:
                    need[k] = (sem, val)
            for k, (sem, val) in need.items():
                if seen[o.stream].get(k, 0) < val:
                    e.wait_ge(sem, val)
                    seen[o.stream][k] = val
            if o.fn is None:
                continue
            ins = o.fn(e)
            if o.dma:
                b = o.dbuf
                kind = "cc" if o.inc == 1 else o.stream
                if kind not in b.dsems:
                    fl = free_sems.get(kind)
                    if fl:
                        b.dsems[kind] = fl.pop()
                    else:
                        b.dsems[kind] = (nc.alloc_semaphore("d_%s_%s" % (kind, b.name)), 0)
                sem_, cnt_ = b.dsems[kind]
                cnt_ += o.inc
                b.dsems[kind] = (sem_, cnt_)
                ins.then_inc(sem_, o.inc)
                o.signal = (sem_, cnt_)
            elif o.sig:
                cnt[o.stream] += 1
                ins.then_inc(sems[o.stream], 1)
                o.signal = (sems[o.stream], cnt[o.stream])
        return cnt


def mm(k, out_buf, out_ap, lhsT_buf, lhsT_ap, rhs_buf, rhs_ap, start, stop):
    k.op("pe", lambda e: e.matmul(out_ap, lhsT_ap, rhs_ap, start=start, stop=stop),
         reads=[lhsT_buf, rhs_buf], writes=[out_buf])


def tr(k, out_buf, out_ap, in_buf, in_ap, ident_buf, ident_ap):
    k.op("pe", lambda e: e.transpose(out_ap, in_ap, ident_ap), reads=[in_buf, ident_buf], writes=[out_buf])


class Ctx:
    pass


def AP_(x):
    try:
        return x.ap()
    except TypeError:
        return x


def dram_in(nc, name, shape, dt):
    return nc.dram_tensor(name, list(shape), dt, kind="ExternalInput")


def dram_out(nc, name, shape, dt):
    return nc.dram_tensor(name, list(shape), dt, kind="ExternalOutput")


def dram_tmp(nc, name, shape, dt):
    return nc.dram_tensor(name, list(shape), dt, kind="Internal")


def setup_common(k, c, dr):
    nc = k.nc
    c.ps = [k.ps([128, 512], F32, "bank%d" % i) for i in range(8)]
    c.ident_f = k.sb([128, 128], F32, "identf")
    c.ident_b = k.sb([128, 128], BF16, "identb")
    k.dma("sp", c.ident_f[:, :], AP_(dr["ident"])[:, :], c.ident_f, True)
    k.op("dve", lambda e: e.tensor_copy(c.ident_b[:, :], c.ident_f[:, :]), [c.ident_f], [c.ident_b])
    c.neghalf = k.sb([128, 1], F32, "neghalf")
    k.op("pool", lambda e: e.memset(c.neghalf[:, :], -0.5), [], [c.neghalf])
    c.ones_b = k.sb([128, 128], BF16, "onesb")
    k.op("pool", lambda e: e.memset(c.ones_b[:, :], 1.0), [], [c.ones_b])
    c.e0 = k.sb([128, 128], BF16, "e0row")
    k.op("pool", lambda e: e.memset(c.e0[:, :], 0.0), [], [c.e0])
    k.op("pool", lambda e: e.memset(c.e0[0:1, :], 1.0), [c.e0], [c.e0])
    c.ones_f = k.sb([128, 128], F32, "onesf")
    k.op("pool", lambda e: e.memset(c.ones_f[:, :], 1.0), [], [c.ones_f])


def compute_mod(k, c, dr):
    if not hasattr(c, "mod"):
        c.mod = k.sb([128, 6 * D], F32, "modrep")
    k.dma("sp", c.mod[:, :], AP_(dr["ada_b_rep"])[:, :], c.mod, True)
    mark = k.scope_begin()
    cT = k.sb([128, 8], F32, "cT")
    k.dma("sp", cT[:, :], AP_(dr["cT"])[:, :], cT, True)
    cact = k.sb([128, 8], F32, "cact")
    k.op("act", lambda e: e.activation(cact[:, :], cT[:, :], AF.Silu), [cT], [cact])
    cb = k.sb([128, 8, 128], F32, "cactb")
    k.op("pool", lambda e: e.memset(cb[:, :, :], 1.0), [], [cb])
    for kc in range(8):
        k.op("dve", lambda e, kc=kc: e.tensor_scalar(cb[:, kc, :], cb[:, kc, :], cact[:, kc:kc + 1], None, ALU.mult),
             [cb, cact], [cb])
    wbufs = [k.sb([128, 8, 256], F32, "adaw%d" % i) for i in range(2)]
    aw = AP_(dr["ada_w"]).rearrange("(kc p) n -> p kc n", p=128)
    for cg in range(24):
        wb = wbufs[cg % 2]
        k.dma("sp", wb[:, :, :], aw[:, :, cg * 256:(cg + 1) * 256], wb, True)
        pb = c.ps[cg % 2]
        for kc in range(8):
            mm(k, pb, pb[:, 0:256], cb, cb[:, kc, :], wb, wb[:, kc, :], kc == 0, kc == 7)
        k.op("dve", lambda e, pb=pb, cg=cg: e.tensor_tensor(c.mod[:, cg * 256:(cg + 1) * 256], pb[:, 0:256],
                                                           c.mod[:, cg * 256:(cg + 1) * 256], ALU.add),
             [pb, c.mod], [c.mod])
    k.scope_end(mark)


def rms_rstd(k, c, x_buf, x_ap, n, junk, ss, rstd):
    k.op("pool", lambda e: e.memset(ss[:, :], 0.0), [], [ss])
    k.op("dve", lambda e: e.scalar_tensor_tensor(out=junk, in0=x_ap, scalar=1.0, in1=x_ap, op0=ALU.mult,
                                                 op1=ALU.mult, accum_out=ss[:, :]), [x_buf, ss], [ss, c.junkb])
    k.op("dve", lambda e: e.tensor_scalar(ss[:, :], ss[:, :], 1.0 / n, EPS, ALU.mult, ALU.add), [ss], [ss])
    k.op("pool", lambda e: e.tensor_tensor(rstd[:, :], ss[:, :], c.neghalf[:, :], ALU.pow), [ss, c.neghalf], [rstd])


def phase_pre(k, c, dr):
    nc = k.nc
    win = k.sb([128, 8, INC], BF16, "win")
    winap = AP_(dr["w_in"]).rearrange("(kc p) n -> p kc n", p=128)
    for kc in range(8):
        for c0 in range(0, INC, 1632):
            k.dma("pool", win[:, kc, c0:c0 + 1632], winap[:, kc, c0:c0 + 1632], win, True)
    winrot = k.sb([128, 8, 64], BF16, "winrot")
    for kc in range(8):
        k.dma("pool", winrot[:, kc, 0:32], winap[:, kc, 640 + 32:640 + 64], winrot, True)
        k.dma("pool", winrot[:, kc, 32:64], winap[:, kc, 640:640 + 32], winrot, True)
    k.op("dve", lambda e: e.tensor_scalar(winrot[:, :, 0:32], winrot[:, :, 0:32], -1.0, None, ALU.mult), [winrot], [winrot])
    wuq = k.sb([128, 3, 1536], BF16, "wuq")
    wuqrot = k.sb([128, 3, NH, 64], BF16, "wuqrot")
    mark = k.scope_begin()
    wuq_f = k.sb([128, 3, 1536], F32, "wuqf")
    wuqap = AP_(dr["w_uq"]).rearrange("(kc p) n -> p kc n", p=128)
    k.dma("sp", wuq_f[:, :, :], wuqap, wuq_f, True)
    k.op("dve", lambda e: e.tensor_scalar(wuq[:, :, :], wuq_f[:, :, :], SCALE, None, ALU.mult), [wuq_f], [wuq])
    wv = wuq_f[:, :, :].rearrange("p k (h d) -> p k h d", h=NH)
    k.op("dve", lambda e: e.tensor_scalar(wuqrot[:, :, :, 0:32], wv[:, :, :, 160:192], -SCALE, None, ALU.mult), [wuq_f], [wuqrot])
    k.op("dve", lambda e: e.tensor_scalar(wuqrot[:, :, :, 32:64], wv[:, :, :, 128:160], SCALE, None, ALU.mult), [wuq_f], [wuqrot])
    k.scope_end(mark)
    A = k.sb([128, D], F32, "Arep")
    k.dma("sp", A[:, :], AP_(dr["norm_mix_rep"])[:, :], A, True)
    qn = k.sb([128, QLORA], F32, "qnrep")
    k.dma("sp", qn[:, :], AP_(dr["q_norm_rep"])[:, :], qn, True)
    kvn = k.sb([128, KVLORA], F32, "kvnrep")
    k.dma("sp", kvn[:, :], AP_(dr["kv_norm_rep"])[:, :], kvn, True)
    k.op("dve", lambda e: e.scalar_tensor_tensor(out=A[:, :], in0=c.mod[:, D:2 * D], scalar=1.0, in1=A[:, :],
                                                 op0=ALU.add, op1=ALU.mult), [c.mod, A], [A])
    cosT = k.sb([64, TL], F32, "cosT")
    sinT = k.sb([64, TL], F32, "sinT")
    mark = k.scope_begin()
    invf = k.sb([64, 1], F32, "invf")
    k.dma("sp", invf[:, :], AP_(dr["inv_freq"])[:, :], invf, True)
    CH = 1024
    posi = k.sb([64, CH], I32, "posi")
    ang = k.sb([64, CH], F32, "ang")
    tq = k.sb([64, CH], F32, "tq")
    ti = k.sb([64, CH], I32, "ti")
    C1 = 6.28125
    C2 = 2.0 * math.pi - C1
    for ch in range(TL // CH):
        sl = slice(ch * CH, (ch + 1) * CH)
        k.dma("sp", posi[:, :], AP_(dr["pos_rep"])[:, sl], posi, True)
        k.op("dve", lambda e: e.tensor_copy(ang[:, :], posi[:, :]), [posi], [ang])
        k.op("dve", lambda e: e.tensor_scalar(ang[:, :], ang[:, :], invf[:, 0:1], None, ALU.mult), [ang, invf], [ang])
        for (dstb, shift) in ((sinT, 0.0), (cosT, math.pi / 2)):
            dst = dstb[:, sl]
            k.op("dve", lambda e, shift=shift: e.tensor_scalar(tq[:, :], ang[:, :], shift, 1.0 / (2 * math.pi), ALU.add, ALU.mult), [ang], [tq])
            k.op("dve", lambda e: e.tensor_copy(ti[:, :], tq[:, :]), [tq], [ti])
            k.op("dve", lambda e: e.tensor_copy(tq[:, :], ti[:, :]), [ti], [tq])
            k.op("dve", lambda e, dst=dst, shift=shift: e.tensor_scalar(dst, ang[:, :], shift, None, ALU.add), [ang], [dstb])
            k.op("dve", lambda e, dst=dst: e.scalar_tensor_tensor(out=dst, in0=tq[:, :], scalar=-C1, in1=dst, op0=ALU.mult, op1=ALU.add), [tq, dstb], [dstb])
            k.op("dve", lambda e, dst=dst: e.scalar_tensor_tensor(out=dst, in0=tq[:, :], scalar=-C2, in1=dst, op0=ALU.mult, op1=ALU.add), [tq, dstb], [dstb])
            k.op("dve", lambda e, dst=dst: e.tensor_scalar(tq[:, :], dst, math.pi, -2 * math.pi, ALU.is_gt, ALU.mult), [dstb], [tq])
            k.op("dve", lambda e, dst=dst: e.tensor_tensor(dst, dst, tq[:, :], ALU.add), [dstb, tq], [dstb])
            k.op("dve", lambda e, dst=dst: e.tensor_scalar(tq[:, :], dst, -math.pi, 2 * math.pi, ALU.is_lt, ALU.mult), [dstb], [tq])
            k.op("dve", lambda e, dst=dst: e.tensor_tensor(dst, dst, tq[:, :], ALU.add), [dstb, tq], [dstb])
            k.op("dve", lambda e, dst=dst: e.tensor_scalar(dst, dst, math.pi, -math.pi, ALU.min, ALU.max), [dstb], [dstb])
            k.op("act", lambda e, dst=dst: e.activation(dst, dst, AF.Sin), [dstb], [dstb])
    k.scope_end(mark)

    xt = [k.sb([128, D], F32, "xt%d" % i) for i in range(2)]
    h32 = k.sb([128, D], F32, "h32")
    hb = [k.sb([128, D], BF16, "hb%d" % i) for i in range(2)]
    c.junkb = k.sb([128, D], BF16, "junk")
    ss = [k.sb([128, 1], F32, "ss%d" % i) for i in range(2)]
    rstd = [k.sb([128, 1], F32, "rstd%d" % i) for i in range(2)]
    hT = [k.sb([128, 8, 512], BF16, "hT%d" % i) for i in range(2)]
    zs = k.sb([128, 640], F32, "zs")
    ssq = k.sb([128, 1], F32, "ssq")
    rsq = k.sb([128, 1], F32, "rsq")
    ssk = k.sb([128, 1], F32, "ssk")
    rsk = k.sb([128, 1], F32, "rsk")
    cqb = k.sb([128, 640], BF16, "cqb")
    cqT = k.sb([128, 3, 512], BF16, "cqT")
    latT = k.sb([128, 2, 512], BF16, "latT")
    ub = [k.sb([128, 512], BF16, "ub%d" % i) for i in range(2)]
    sg = [k.sb([128, 512], BF16, "sg%d" % i) for i in range(3)]
    kr1 = k.sb([64, 512], F32, "kr1")
    kr2 = k.sb([64, 512], F32, "kr2")
    krb = [k.sb([64, 512], BF16, "krb%d" % i) for i in range(2)]
    qnb = [k.sb([128, 512], BF16, "qnb%d" % i) for i in range(2)]
    qrb = [k.sb([64, 512], BF16, "qrb%d" % i) for i in range(2)]
    x_d = AP_(dr["x"])
    u_d = AP_(dr["u_local"])
    sig_d = AP_(dr["sigT"])
    lat_d = AP_(dr["latT_local"]) if "latT_local" in dr else None
    q_d = AP_(dr["qT"])
    ps = c.ps
    psb = [p[:, :].bitcast(BF16) for p in ps]
    nst = 0
    for g in range(NG):
        hTg = hT[g % 2]
        for tt in range(4):
            t = g * 4 + tt
            xb = xt[t % 2]
            k.dma("sp", xb[:, :], x_d[t * 128:(t + 1) * 128, :], xb, True)
            s_, r_ = ss[t % 2], rstd[t % 2]
            rms_rstd(k, c, xb, xb[:, :], D, c.junkb[:, :], s_, r_)
            k.op("dve", lambda e, xb=xb, r_=r_: e.scalar_tensor_tensor(out=h32[:, :], in0=xb[:, :], scalar=r_[:, 0:1], in1=A[:, :],
                                                                     op0=ALU.mult, op1=ALU.mult), [xb, r_, A], [h32])
            hbb = hb[t % 2]
            k.op("pool", lambda e, hbb=hbb: e.tensor_tensor(hbb[:, :], h32[:, :], c.mod[:, 0:D], ALU.add), [h32, c.mod], [hbb])
            pt = ps[7]
            for kc in range(8):
                tr(k, pt, psb[7][:, kc * 128:(kc + 1) * 128], hbb, hbb[:, kc * 128:(kc + 1) * 128], c.ident_b, c.ident_b[:, :])
            k.op("act", lambda e, hTg=hTg, tt=tt: e.copy(hTg[:, :, tt * 128:(tt + 1) * 128],
                                                        psb[7][:, :].rearrange("p (k t) -> p k t", k=8)), [pt], [hTg])
        for tt in range(4):
            t = g * 4 + tt
            cols = ((0, 512), (512, 1024), (1024, 1216))
            for bi, (c0, c1) in enumerate(cols):
                for kc in range(8):
                    mm(k, ps[bi], ps[bi][:, 0:c1 - c0], hTg, hTg[:, kc, tt * 128:(tt + 1) * 128], win, win[:, kc, c0:c1], kc == 0, kc == 7)
            k.op("act", lambda e: e.copy(zs[:, 0:512], ps[0][:, 0:512]), [ps[0]], [zs])
            k.op("act", lambda e: e.copy(zs[:, 512:640], ps[1][:, 0:128]), [ps[1]], [zs])
            ubb = ub[t % 2]
            k.op("act", lambda e, ubb=ubb: e.copy(ubb[:, 0:320], ps[1][:, 192:512]), [ps[1]], [ubb])
            k.op("act", lambda e, ubb=ubb: e.copy(ubb[:, 320:512], ps[2][:, 0:192]), [ps[2]], [ubb])
            k.dma("pool", u_d[t * 128:(t + 1) * 128, :], ubb[:, :], ubb, False)
            if tt == 3 and "uh_local" in dr:
                k.dma("pool", AP_(dr["uh_local"])[g * 16:(g + 1) * 16, :], ubb[112:128, :], ubb, False)
            rms_rstd(k, c, zs, zs[:, 0:QLORA], QLORA, c.junkb[:, 0:QLORA], ssq, rsq)
            rms_rstd(k, c, zs, zs[:, QLORA:640], KVLORA, c.junkb[:, 0:KVLORA], ssk, rsk)
            k.op("dve", lambda e: e.scalar_tensor_tensor(out=cqb[:, 0:QLORA], in0=zs[:, 0:QLORA], scalar=rsq[:, 0:1], in1=qn[:, :],
                                                         op0=ALU.mult, op1=ALU.mult), [zs, rsq, qn], [cqb])
            k.op("dve", lambda e: e.scalar_tensor_tensor(out=cqb[:, QLORA:640], in0=zs[:, QLORA:640], scalar=rsk[:, 0:1], in1=kvn[:, :],
                                                         op0=ALU.mult, op1=ALU.mult), [zs, rsk, kvn], [cqb])
            pt = ps[7]
            for kc in range(5):
                tr(k, pt, psb[7][:, kc * 128:(kc + 1) * 128], cqb, cqb[:, kc * 128:(kc + 1) * 128], c.ident_b, c.ident_b[:, :])
            k.op("act", lambda e, tt=tt: e.copy(cqT[:, :, tt * 128:(tt + 1) * 128],
                                               psb[7][:, 0:384].rearrange("p (k t) -> p k t", k=3)), [pt], [cqT])
            k.op("act", lambda e, tt=tt: e.copy(latT[:, :, tt * 128:(tt + 1) * 128],
                                               psb[7][:, 384:640].rearrange("p (k t) -> p k t", k=2)), [pt], [latT])
        tok = slice(g * 512, (g + 1) * 512)
        for kc in range(2):
            dst = dr["lat_parts"][kc][:, tok] if "lat_parts" in dr else lat_d[kc * 128:(kc + 1) * 128, tok]
            k.dma("pool", dst, latT[:, kc, :], latT, False)
        for f in range(16):
            pb = ps[3 + (f % 3)]
            for kc in range(8):
                mm(k, pb, pb[:, :], win, win[:, kc, 1216 + f * 128:1216 + (f + 1) * 128], hTg, hTg[:, kc, :], kc == 0, kc == 7)
            sgb = sg[nst % 3]
            nst += 1
            k.op("act", lambda e, sgb=sgb, pb=pb: e.activation(sgb[:, :], pb[:, :], AF.Sigmoid), [pb], [sgb])
            k.dma("pool", sig_d[f * 128:(f + 1) * 128, tok], sgb[:, :], sgb, False)
        for kc in range(8):
            mm(k, ps[6], ps[6][0:64, :], win, win[:, kc, 640:704], hTg, hTg[:, kc, :], kc == 0, kc == 7)
        for kc in range(8):
            mm(k, ps[3], ps[3][0:64, :], winrot, winrot[:, kc, :], hTg, hTg[:, kc, :], kc == 0, kc == 7)
        krbb = krb[g % 2]
        k.op("dve", lambda e, tok=tok: e.tensor_tensor(kr1[:, :], ps[6][0:64, :], cosT[:, tok], ALU.mult), [ps[6], cosT], [kr1])
        k.op("dve", lambda e, tok=tok: e.tensor_tensor(kr2[:, :], ps[3][0:64, :], sinT[:, tok], ALU.mult), [ps[3], sinT], [kr2])
        k.op("pool", lambda e, krbb=krbb: e.tensor_tensor(krbb[:, :], kr1[:, :], kr2[:, :], ALU.add), [kr1, kr2], [krbb])
        k.dma("pool", dr["lat_parts"][2][:, tok] if "lat_parts" in dr else lat_d[256:320, tok], krbb[:, :], krbb, False)
        for h in range(NH):
            pb = ps[4 + (h % 2)]
            for kc in range(3):
                mm(k, pb, pb[:, :], wuq, wuq[:, kc, h * 192:h * 192 + 128], cqT, cqT[:, kc, :], kc == 0, kc == 2)
            qb = qnb[h % 2]
            k.op("act", lambda e, qb=qb, pb=pb: e.copy(qb[:, :], pb[:, :]), [pb], [qb])
            k.dma("pool", q_d[h, 0:128, tok], qb[:, :], qb, False)
            for kc in range(3):
                mm(k, ps[6], ps[6][0:64, :], wuq, wuq[:, kc, h * 192 + 128:h * 192 + 192], cqT, cqT[:, kc, :], kc == 0, kc == 2)
            for kc in range(3):
                mm(k, ps[3], ps[3][0:64, :], wuqrot, wuqrot[:, kc, h, :], cqT, cqT[:, kc, :], kc == 0, kc == 2)
            kb_ = qrb[h % 2]
            k.op("dve", lambda e, tok=tok: e.tensor_tensor(kr1[:, :], ps[6][0:64, :], cosT[:, tok], ALU.mult), [ps[6], cosT], [kr1])
            k.op("dve", lambda e, tok=tok: e.tensor_tensor(kr2[:, :], ps[3][0:64, :], sinT[:, tok], ALU.mult), [ps[3], sinT], [kr2])
            k.op("pool", lambda e, kb_=kb_: e.tensor_tensor(kb_[:, :], kr1[:, :], kr2[:, :], ALU.add), [kr1, kr2], [kb_])
            k.dma("pool", q_d[h, 128:192, tok], kb_[:, :], kb_, False)


def k_q_rope_bufs(k, c):
    if not hasattr(c, "_qrb"):
        c._qrb = [k.sb([64, 512], BF16, "qrb%d" % i) for i in range(2)]
    return c._qrb


def TT(k, eng, ob, o, ab, a, bb, b, op):
    k.op(eng, lambda e: e.tensor_tensor(o, a, b, op), [ab, bb], [ob])


def TS(k, eng, ob, o, ab, a, s1, s2, op0, op1=None, rd=()):
    if op1 is None:
        k.op(eng, lambda e: e.tensor_scalar(o, a, s1, None, op0), [ab] + list(rd), [ob])
    else:
        k.op(eng, lambda e: e.tensor_scalar(o, a, s1, s2, op0, op1), [ab] + list(rd), [ob])


def STT(k, eng, ob, o, ab, a, sc, bb, b, op0, op1, rd=()):
    k.op(eng, lambda e: e.scalar_tensor_tensor(out=o, in0=a, scalar=sc, in1=b, op0=op0, op1=op1), [ab, bb] + list(rd), [ob])


def ACTF(k, ob, o, ib, i, func, scale=1.0, bias=0.0, rd=()):
    k.op("act", lambda e: e.activation(o, i, func, bias=bias, scale=scale), [ib] + list(rd), [ob])


def CP(k, eng, ob, o, ib, i):
    if eng == "act":
        k.op("act", lambda e: e.copy(o, i), [ib], [ob])
    else:
        k.op(eng, lambda e: e.tensor_copy(o, i), [ib], [ob])


def seq_group_owner(G):
    kk, m = G // 8, G % 8
    if m < 4:
        return m, 2 * kk
    return 7 - m, 2 * kk + 1


def phase_attn(k, c, dr):
    ps = c.ps
    psb = [p[:, :].bitcast(BF16) for p in ps]
    mark = k.scope_begin()
    KT = k.sb([128, S], BF16, "KT")
    krT = k.sb([128, S], BF16, "krT")
    V = k.sb([128, 128, 130], BF16, "V")
    masks = k.sb([128, 32, 512], BF16, "masks")
    qn = k.sb([128, TL], BF16, "qn")
    qr = k.sb([128, TL], BF16, "qr")
    latb = [k.sb([128, 2, 512], BF16, "latb%d" % i) for i in range(2)]
    wkv = [k.sb([128, 2, 256], BF16, "wkv%d" % i) for i in range(2)]
    PT = [k.sb([128, 512], BF16, "PT%d" % i) for i in range(8)]
    pacc = [k.sb([128, 512], F32, "pacc%d" % i) for i in range(4)]
    rls = [k.sb([128, 512], F32, "rls%d" % i) for i in range(2)]
    sga = [k.sb([128, 512], BF16, "sga%d" % i) for i in range(4)]
    mxo = [k.sb([128, 512], BF16, "mxo%d" % i) for i in range(2)]
    lat_all = AP_(dr["latT_all"]) if "latT_all" in dr else None
    q_d = AP_(dr["qT"])
    sig_d = AP_(dr["sigT"])
    mix_d = AP_(dr["mixT"])
    wukv = AP_(dr["w_ukv"]).rearrange("(kc p) n -> p kc n", p=128)
    mk_d = AP_(dr["maskbank"])
    for i in range(4):
        k.dma("sp", masks[:, i * 8:(i + 1) * 8, :], mk_d[:, i * 8:(i + 1) * 8, :], masks, True)
    k.op("pool", lambda e: e.memset(krT[64:128, :], 0.0), [], [krT])
    k.op("pool", lambda e: e.memset(qr[64:128, :], 0.0), [], [qr])
    for G in range(32):
        jp, gi = seq_group_owner(G)
        src = dr["lat_all_parts"][2][jp, :, gi * 512:(gi + 1) * 512] if "lat_all_parts" in dr else lat_all[jp, 256:320, gi * 512:(gi + 1) * 512]
        k.dma("sp", krT[0:64, G * 512:(G + 1) * 512], src, krT, True)
    nld = 0
    npt = 0
    nfin = 0
    for h in range(NH):
        wb = wkv[h % 2]
        k.dma("pool", wb[:, :, :], wukv[:, :, h * 256:(h + 1) * 256], wb, True)
        k.dma("sp", qn[:, :], q_d[h, 0:128, :], qn, True)
        k.dma("sp", qr[0:64, :], q_d[h, 128:192, :], qr, True)
        for G in range(32):
            jp, gi = seq_group_owner(G)
            lb = latb[nld % 2]
            nld += 1
            if "lat_all_parts" in dr:
                for kc in range(2):
                    k.dma("sp", lb[:, kc, :], dr["lat_all_parts"][kc][jp, :, gi * 512:(gi + 1) * 512], lb, True)
            else:
                k.dma("sp", lb[:, :, :], lat_all[jp, 0:256, gi * 512:(gi + 1) * 512].rearrange("(kc p) t -> p kc t", p=128), lb, True)
            pk = ps[G % 2]
            for kc in range(2):
                mm(k, pk, pk[:, :], wb, wb[:, kc, 0:128], lb, lb[:, kc, :], kc == 0, kc == 1)
            CP(k, "act" if G % 2 == 0 else "dve", KT, KT[:, G * 512:(G + 1) * 512], pk, pk[:, :])
            pv = ps[2 + G % 2]
            for a in range(4):
                for kc in range(2):
                    mm(k, pv, pv[:, a * 128:(a + 1) * 128], lb, lb[:, kc, a * 128:(a + 1) * 128], wb, wb[:, kc, 128:256], kc == 0, kc == 1)
            CP(k, "dve" if G % 2 == 0 else "act", V, V[:, 4 * G:4 * G + 4, 0:128], pv, pv[:, :].rearrange("p (a d) -> p a d", a=4))
        for SL in ((4, 5, 6, 7), (0, 1, 2, 3)):
            nst = {s_: 32 * (s_ // 2) + 16 + 16 * (s_ % 2) for s_ in SL}
            maxlen = max(nst.values())
            for idx, s_ in enumerate(SL):
                sgb = sga[idx]
                k.dma("sp", sgb[:, :], sig_d[h * 128:(h + 1) * 128, s_ * 512:(s_ + 1) * 512], sgb, True)
            for st in range(maxlen + 1):
                if st < maxlen:
                    act_ = [(idx, s_) for idx, s_ in enumerate(SL) if st < nst[s_]]
                    for idx, s_ in act_:
                        pS = ps[idx]
                        mm(k, pS, pS[:, :], KT, KT[:, st * 128:(st + 1) * 128], qn, qn[:, s_ * 512:(s_ + 1) * 512], True, False)
                    for idx, s_ in act_:
                        pS = ps[idx]
                        mm(k, pS, pS[:, :], krT, krT[:, st * 128:(st + 1) * 128], qr, qr[:, s_ * 512:(s_ + 1) * 512], False, True)
                    for idx, s_ in act_:
                        pS = ps[idx]
                        pt = PT[idx * 2 + st % 2]
                        ACTF(k, pt, pt[:, :], pS, pS[:, :], AF.Exp)
                        sp_ = st - (nst[s_] - 16)
                        if sp_ >= 0:
                            TT(k, "dve", pt, pt[:, :], pt, pt[:, :], masks, masks[:, (s_ % 2) * 16 + sp_, :], ALU.mult)
                if st >= 1:
                    stp = st - 1
                    act_ = [(idx, s_) for idx, s_ in enumerate(SL) if stp < nst[s_]]
                    for idx, s_ in act_:
                        po = ps[4 + idx]
                        pt = PT[idx * 2 + stp % 2]
                        mm(k, po, po[:, :], V, V[:, stp, 0:128], pt, pt[:, :], stp == 0, stp == nst[s_] - 1)
                    for idx, s_ in act_:
                        pt = PT[idx * 2 + stp % 2]
                        pa = pacc[idx]
                        if stp == 0:
                            CP(k, "dve", pa, pa[:, :], pt, pt[:, :])
                        else:
                            TT(k, "dve", pa, pa[:, :], pa, pa[:, :], pt, pt[:, :], ALU.add)
                    for idx, s_ in act_:
                        if stp != nst[s_] - 1:
                            continue
                        po = ps[4 + idx]
                        pa = pacc[idx]
                        sgb = sga[idx]
                        qc = slice(s_ * 512, (s_ + 1) * 512)
                        pl_ = ps[idx]
                        mm(k, pl_, pl_[:, :], c.ones_f, c.ones_f[:, :], pa, pa[:, :], True, True)
                        rl = rls[idx % 2]
                        k.op("dve", lambda e, rl=rl, pl_=pl_: e.reciprocal(rl[:, :], pl_[:, :]), [pl_], [rl])
                        TT(k, "pool", rl, rl[:, :], rl, rl[:, :], sgb, sgb[:, :], ALU.mult)
                        mo = mxo[idx % 2]
                        TT(k, "dve", mo, mo[:, :], po, po[:, :], rl, rl[:, :], ALU.mult)
                        k.dma("pool", mix_d[h * 128:(h + 1) * 128, qc], mo[:, :], mo, False)
    k.scope_end(mark)


def phase_mix(k, c, dr):
    ps = c.ps
    mark = k.scope_begin()
    wpool = k.sb([128, 4, 256], BF16, "wpool")
    k.dma("pool", wpool[:, :, :], AP_(dr["w_pool"]).rearrange("g c d -> c g d"), wpool, True)
    pscale = k.sb([128, 8], F32, "pscale")
    k.dma("sp", pscale[:, :], AP_(dr["pscaleT"])[:, :], pscale, True)
    wout = k.sb([128, 8, D], BF16, "wout")
    woap = AP_(dr["w_out"]).rearrange("(kc p) n -> p kc n", p=128)
    for kc in range(8):
        k.dma("pool", wout[:, kc, :], woap[:, kc, :], wout, True)
    band = k.sb([128, 4, 4, 128], BF16, "band")
    k.dma("sp", band[:, :, :, :], AP_(dr["bands"])[:, :, :, :], band, True)
    sel = k.sb([128, 4, 8, 32], BF16, "sel")
    k.dma("sp", sel[:, :, :, :], AP_(dr["sel"])[:, :, :, :], sel, True)
    ucand = k.sb([128, 4, 512], BF16, "ucand")
    k.dma("sp", ucand[:, :, :], AP_(dr["uhalo_all"]).rearrange("(kt p) c -> p kt c", p=128), ucand, True)
    ug = [k.sb([128, 4, 512], BF16, "ug%d" % i) for i in range(2)]
    halo = k.sb([32, 512], BF16, "halo")
    pTb = [k.sb([128, 512], BF16, "pTb%d" % i) for i in range(2)]
    sgbt = [k.sb([128, 512], BF16, "sgbt%d" % i) for i in range(2)]
    attp = [k.sb([128, 512], BF16, "attp%d" % i) for i in range(2)]
    tmp = [k.sb([128, 512], F32, "ptmp%d" % i) for i in range(2)]
    mixT = [k.sb([128, 8, 512], BF16, "mixT%d" % i) for i in range(2)]
    xt = [k.sb([128, D], F32, "xmt%d" % i) for i in range(2)]
    xo = [k.sb([128, D], F32, "xmo%d" % i) for i in range(2)]
    tmp2 = k.sb([128, D], F32, "tmp2")
    u_d = AP_(dr["u_local"])
    sig_d = AP_(dr["sigT"])
    mix_d = AP_(dr["mixT"])
    x_d = AP_(dr["x"])
    x1_d = AP_(dr["x1"])
    nf = 0
    for gi in range(NG):
        qc = slice(gi * 512, (gi + 1) * 512)
        ugb = ug[gi % 2]
        k.dma("sp", ugb[:, :, :], u_d[qc, :].rearrange("(t p) c -> p t c", p=128), ugb, True)
        ph = ps[0]
        for kt in range(4):
            mm(k, ph, ph[0:32, :], sel, sel[:, kt, gi, :], ucand, ucand[:, kt, :], kt == 0, kt == 3)
        CP(k, "act", halo, halo[:, :], ph, ph[0:32, :])
        mxb = mixT[gi % 2]
        for g4 in range(4):
            pp = ps[1 + g4 % 2]
            for tt in range(4):
                bk = 1 if (gi == 0 and tt == 0) else 0
                mm(k, pp, pp[:, tt * 128:(tt + 1) * 128], ugb, ugb[:, tt, g4 * 128:(g4 + 1) * 128], band, band[:, bk, g4, :], True, False)
                if tt == 0:
                    mm(k, pp, pp[:, 0:128], halo, halo[0:32, g4 * 128:(g4 + 1) * 128], band, band[0:32, 3, g4, :], False, True)
                else:
                    mm(k, pp, pp[:, tt * 128:(tt + 1) * 128], ugb, ugb[64:128, tt - 1, g4 * 128:(g4 + 1) * 128], band, band[64:128, 2, g4, :], False, True)
            pb_ = pTb[g4 % 2]
            CP(k, "act", pb_, pb_[:, :], pp, pp[:, :])
            for half in range(2):
                f = 2 * g4 + half
                po = ps[3 + half]
                mm(k, po, po[:, :], wpool, wpool[:, g4, half * 128:(half + 1) * 128], pb_, pb_[:, :], True, True)
                sb_ = sgbt[nf % 2]
                ap_ = attp[nf % 2]
                tp_ = tmp[nf % 2]
                nf += 1
                k.dma("sp", sb_[:, :], sig_d[1024 + f * 128:1024 + (f + 1) * 128, qc], sb_, True)
                k.dma("sp", ap_[:, :], mix_d[f * 128:(f + 1) * 128, qc], ap_, True)
                STT(k, "dve", tp_, tp_[:, :], po, po[:, :], pscale[:, f:f + 1], sb_, sb_[:, :], ALU.mult, ALU.mult, rd=[pscale])
                TT(k, "pool", mxb, mxb[:, f, :], tp_, tp_[:, :], ap_, ap_[:, :], ALU.add)
        for tt in range(4):
            t = gi * 4 + tt
            xb = xt[t % 2]
            xob = xo[t % 2]
            k.dma("sp", xb[:, :], x_d[t * 128:(t + 1) * 128, :], xb, True)
            for cg in range(2):
                po = ps[5 + cg]
                for f in range(8):
                    mm(k, po, po[:, :], mxb, mxb[:, f, tt * 128:(tt + 1) * 128], wout, wout[:, f, cg * 512:(cg + 1) * 512], f == 0, f == 7)
                TT(k, "dve", tmp2, tmp2[:, cg * 512:(cg + 1) * 512], po, po[:, :], c.mod, c.mod[:, 2 * D + cg * 512:2 * D + (cg + 1) * 512], ALU.mult)
            TT(k, "pool", xob, xob[:, :], tmp2, tmp2[:, :], xb, xb[:, :], ALU.add)
            k.dma("pool", x1_d[t * 128:(t + 1) * 128, :], xob[:, :], xob, False)
    k.scope_end(mark)


def phase_moe(k, c, dr, final):
    ps = c.ps
    mark = k.scope_begin()
    A2 = k.sb([128, D], F32, "A2")
    k.dma("sp", A2[:, :], AP_(dr["norm_ffn_rep"])[:, :], A2, True)
    STT(k, "dve", A2, A2[:, :], c.mod, c.mod[:, 4 * D:5 * D], 1.0, A2, A2[:, :], ALU.add, ALU.mult)
    wr = k.sb([128, 8, NE], F32, "wr")
    k.dma("sp", wr[:, :, :], AP_(dr["w_router"]).rearrange("(kc p) n -> p kc n", p=128), wr, True)
    brep = k.sb([128, NE], F32, "brep")
    k.dma("sp", brep[:, :], AP_(dr["b_router_rep"])[:, :], brep, True)
    bgu = k.sb([128, NE, 16], F32, "bgu")
    k.dma("sp", bgu[:, :, :], AP_(dr["b_guT"])[:, :, :], bgu, True)
    TS(k, "dve", bgu, bgu[:, :, 8:16], bgu, bgu[:, :, 8:16], 1.0, None, ALU.add)
    if final:
        nfr = k.sb([128, D], F32, "nfr")
        k.dma("sp", nfr[:, :], AP_(dr["norm_final_rep"])[:, :], nfr, True)
    NT = 8
    NSG = NT // 4
    acc = k.sb([128, NT, D], F32, "acc")
    h2T = k.sb([128, 8, NT * 128], BF16, "h2T")
    Gq = k.sb([128, NT, NE], F32, "Gq")
    wg = k.sb([128, 8, 2 * DFF], BF16, "wgu")
    wd = k.sb([128, 8, D], BF16, "wdn")
    bdf = k.sb([1, D], F32, "bdf")
    bd = k.sb([128, D], BF16, "bdn")
    k.op("pool", lambda e: e.memset(bd[:, :], 0.0), [], [bd])
    actT = [k.sb([128, 8, 512], BF16, "actT%d" % i) for i in range(NSG)]
    g1s = [k.sb([128, 512], F32, "g1_%d" % i) for i in range(2)]
    sgms = [k.sb([128, 512], F32, "sgm_%d" % i) for i in range(2)]
    l1s = [k.sb([128, 512], F32, "l1_%d" % i) for i in range(2)]
    nch = 0
    ss = k.sb([128, 1], F32, "mss")
    rstd = k.sb([128, 1], F32, "mrstd")
    x1_d = AP_(dr["x1"])
    xo_d = AP_(dr["x_out"])
    wgu_d = AP_(dr["w_gu"])
    wdn_d = AP_(dr["w_down"])
    bdn_d = AP_(dr["b_down"])
    NQ = TL // (NT * 128)
    ncast = 0

    def load_wgu(ei, stg):
        nonlocal ncast
        wga = wgu_d[ei].rearrange("(kc p) n -> p kc n", p=128)
        for kc in range(8):
            sb_ = stg[ncast % len(stg)]
            k.dma("sp", sb_[:, :], wga[:, kc, :], sb_, True)
            CP(k, "act" if ncast % 3 != 2 else "dve", wg, wg[:, kc, :], sb_, sb_[:, :])
            ncast += 1

    def load_wdn(ei, stg):
        nonlocal ncast
        wda = wdn_d[ei].rearrange("(kc p) n -> p kc n", p=128)
        for kc in range(0, 8, 2):
            sb_ = stg[ncast % len(stg)]
            k.dma("sp", sb_[:, :].rearrange("p (a n) -> p a n", a=2), wda[:, kc:kc + 2, :], sb_, True)
            CP(k, "act" if ncast % 3 != 2 else "dve", wd, wd[:, kc:kc + 2, :], sb_, sb_[:, :].rearrange("p (a n) -> p a n", a=2))
            ncast += 1
        k.dma("sp", bdf[:, :], bdn_d[ei:ei + 1, :], bdf, True)
        CP(k, "pool", bd, bd[0:1, :], bdf, bdf[:, :])

    for q in range(NQ):
        m2 = k.scope_begin()
        xt = [k.sb([128, D], F32, "xq%d" % i) for i in range(2)]
        h2 = k.sb([128, D], F32, "h2")
        h2Tf = k.sb([128, 8, 128], F32, "h2Tf")
        c.junkb = k.sb([128, D], BF16, "junk2")
        lg = k.sb([128, NE], F32, "lg")
        top8 = k.sb([128, 8], F32, "top8")
        negm = k.sb([128, 1], F32, "negm")
        msk = k.sb([128, NE], F32, "msk")
        ex = k.sb([128, NE], F32, "ex")
        sm = k.sb([128, 1], F32, "sm")
        for T in range(NT):
            t = q * NT + T
            xb = xt[t % 2]
            k.dma("sp", xb[:, :], x1_d[t * 128:(t + 1) * 128, :], xb, True)
            rms_rstd(k, c, xb, xb[:, :], D, c.junkb[:, :], ss, rstd)
            STT(k, "dve", h2, h2[:, :], xb, xb[:, :], rstd[:, 0:1], A2, A2[:, :], ALU.mult, ALU.mult, rd=[rstd])
            TT(k, "pool", h2, h2[:, :], h2, h2[:, :], c.mod, c.mod[:, 3 * D:4 * D], ALU.add)
            for kc in range(8):
                pb = ps[kc // 4]
                tr(k, pb, pb[:, (kc % 4) * 128:(kc % 4 + 1) * 128], h2, h2[:, kc * 128:(kc + 1) * 128], c.ident_f, c.ident_f[:, :])
            for hf in range(2):
                CP(k, "act", h2Tf, h2Tf[:, hf * 4:(hf + 1) * 4, :], ps[hf], ps[hf][:, :].rearrange("p (k t) -> p k t", k=4))
                CP(k, "dve", h2T, h2T[:, hf * 4:(hf + 1) * 4, T * 128:(T + 1) * 128], h2Tf, h2Tf[:, hf * 4:(hf + 1) * 4, :])
            pr = ps[2]
            for kc in range(8):
                mm(k, pr, pr[:, 0:NE], h2Tf, h2Tf[:, kc, :], wr, wr[:, kc, :], kc == 0, kc == 7)
            TT(k, "dve", lg, lg[:, :], pr, pr[:, 0:NE], brep, brep[:, :], ALU.add)
            k.op("dve", lambda e, top8=top8, lg=lg: e.max(top8[:, :], lg[:, :]), [lg], [top8])
            TS(k, "dve", negm, negm[:, :], top8, top8[:, 0:1], -1.0, None, ALU.mult)
            TS(k, "dve", msk, msk[:, :], lg, lg[:, :], top8[:, 3:4], None, ALU.is_ge, rd=[top8])
            ACTF(k, ex, ex[:, :], lg, lg[:, :], AF.Exp, bias=negm[:, 0:1], rd=[negm])
            TT(k, "dve", ex, ex[:, :], ex, ex[:, :], msk, msk[:, :], ALU.mult)
            k.op("dve", lambda e, sm=sm, ex=ex: e.reduce_sum(sm[:, :], ex[:, :], axis=AX.X), [ex], [sm])
            k.op("dve", lambda e, sm=sm: e.reciprocal(sm[:, :], sm[:, :]), [sm], [sm])
            TS(k, "dve", Gq, Gq[:, T, :], ex, ex[:, :], sm[:, 0:1], None, ALU.mult, rd=[sm])
        k.op("pool", lambda e: e.memset(acc[:, :, :], 0.0), [], [acc])
        k.scope_end(m2)
        m2 = k.scope_begin()
        stg = [k.sb([128, 2 * DFF], F32, "stg%d" % i) for i in range(4 if final else 5)]
        ne_run = NE_RUN if MOE_DBG == 0 else 0
        if ne_run:
            load_wgu(0, stg)
            load_wdn(0, stg)
        for ei in range(ne_run):
            for f in range(8):
                base = 4 * (f % 2)
                for half in range(2):
                    for kc in range(8):
                        for sg_ in range(NSG):
                            pb = ps[base + 2 * half + sg_]
                            mm(k, pb, pb[:, :], wg, wg[:, kc, half * DFF + f * 128:half * DFF + (f + 1) * 128],
                               h2T, h2T[:, kc, sg_ * 512:(sg_ + 1) * 512], kc == 0, kc == 7)
                for sg_ in range(NSG):
                    pg = ps[base + sg_]
                    pl = ps[base + 2 + sg_]
                    aT = actT[sg_]
                    g1 = g1s[nch % 2]
                    sgm = sgms[nch % 2]
                    l1 = l1s[nch % 2]
                    nch += 1
                    TS(k, "dve", g1, g1[:, :], pg, pg[:, :], bgu[:, ei, f:f + 1], 7.0, ALU.add, ALU.min, rd=[bgu])
                    ACTF(k, sgm, sgm[:, :], g1, g1[:, :], AF.Sigmoid, scale=1.702)
                    TS(k, "dve", l1, l1[:, :], pl, pl[:, :], bgu[:, ei, 8 + f:9 + f], 8.0, ALU.add, ALU.min, rd=[bgu])
                    TT(k, "pool", sgm, sgm[:, :], g1, g1[:, :], sgm, sgm[:, :], ALU.mult)
                    STT(k, "dve", aT, aT[:, f, :], l1, l1[:, :], -6.0, sgm, sgm[:, :], ALU.max, ALU.mult)
            if ei + 1 < ne_run:
                load_wgu(ei + 1, stg)
            for sg_ in range(NSG):
                aT = actT[sg_]
                for tt in range(4):
                    T = sg_ * 4 + tt
                    pyb = 2 * (T % 2)
                    for cg in range(2):
                        py = ps[pyb + cg]
                        mm(k, py, py[:, :], c.e0, c.e0[:, :], bd, bd[:, cg * 512:(cg + 1) * 512], True, False)
                    for f in range(8):
                        for cg in range(2):
                            py = ps[pyb + cg]
                            mm(k, py, py[:, :], aT, aT[:, f, tt * 128:(tt + 1) * 128], wd, wd[:, f, cg * 512:(cg + 1) * 512], False, f == 7)
                    for cg in range(2):
                        py = ps[pyb + cg]
                        STT(k, "dve", acc, acc[:, T, cg * 512:(cg + 1) * 512], py, py[:, :], Gq[:, T, ei:ei + 1], acc,
                            acc[:, T, cg * 512:(cg + 1) * 512], ALU.mult, ALU.add, rd=[Gq])
            if ei + 1 < ne_run:
                load_wdn(ei + 1, stg)
        k.scope_end(m2)
        m2 = k.scope_begin()
        xt = [k.sb([128, D], F32, "xr%d" % i) for i in range(2)]
        ho = [k.sb([128, D], F32, "ho%d" % i) for i in range(2)]
        c.junkb = k.sb([128, D], BF16, "junk3")
        for T in range(NT):
            t = q * NT + T
            xb = xt[t % 2]
            h2 = ho[t % 2]
            k.dma("sp", xb[:, :], x1_d[t * 128:(t + 1) * 128, :], xb, True)
            TT(k, "dve", h2, h2[:, :], acc, acc[:, T, :], c.mod, c.mod[:, 5 * D:6 * D], ALU.mult)
            TT(k, "pool", h2, h2[:, :], h2, h2[:, :], xb, xb[:, :], ALU.add)
            if final:
                rms_rstd(k, c, h2, h2[:, :], D, c.junkb[:, :], ss, rstd)
                STT(k, "dve", h2, h2[:, :], h2, h2[:, :], rstd[:, 0:1], nfr, nfr[:, :], ALU.mult, ALU.mult, rd=[rstd])
            k.dma("pool", xo_d[t * 128:(t + 1) * 128, :], h2[:, :], h2, False)
        k.scope_end(m2)
    k.scope_end(mark)


def core_groups(j):
    out = []
    for kk in range(4):
        out.append(8 * kk + j)
        out.append(8 * kk + 7 - j)
    return out


def local_token_index(j):
    idx = np.concatenate([np.arange(G * 512, (G + 1) * 512) for G in core_groups(j)])
    return idx


def rep(v, n=128):
    return np.ascontiguousarray(np.broadcast_to(np.asarray(v, np.float32).reshape(1, -1), (n, v.size)))


def build_pre(layer_has=None):
    nc = bass.Bass("TRN2", target_bir_lowering=False)
    dr = {}
    dr["ident"] = dram_in(nc, "ident", [128, 128], F32)
    dr["cT"] = dram_in(nc, "cT", [128, 8], F32)
    dr["ada_b_rep"] = dram_in(nc, "ada_b_rep", [128, 6 * D], F32)
    dr["ada_w"] = dram_in(nc, "ada_w", [D, 6 * D], F32)
    dr["w_in"] = dram_in(nc, "w_in", [D, INC], F32)
    dr["w_uq"] = dram_in(nc, "w_uq", [QLORA, 1536], F32)
    dr["norm_mix_rep"] = dram_in(nc, "norm_mix_rep", [128, D], F32)
    dr["q_norm_rep"] = dram_in(nc, "q_norm_rep", [128, QLORA], F32)
    dr["kv_norm_rep"] = dram_in(nc, "kv_norm_rep", [128, KVLORA], F32)
    dr["pos_rep"] = dram_in(nc, "pos_rep", [64, TL], I32)
    dr["inv_freq"] = dram_in(nc, "inv_freq", [64, 1], F32)
    dr["x"] = dram_in(nc, "x", [TL, D], F32)
    dr["u_local"] = dram_out(nc, "u_local", [TL, 512], BF16)
    dr["sigT"] = dram_out(nc, "sigT", [2048, TL], BF16)
    dr["latT_local"] = dram_out(nc, "latT_local", [320, TL], BF16)
    dr["qT"] = dram_out(nc, "qT", [NH, 192, TL], BF16)
    k = KB(nc)
    c = Ctx()
    setup_common(k, c, dr)
    compute_mod(k, c, dr)
    phase_pre(k, c, dr)
    k.barrier()
    cnt = k.finish()
    return nc, cnt


def build_post(final, phases=(1, 2, 3), debug=False):
    nc = bass.Bass("TRN2", target_bir_lowering=False)
    dr = {}
    def din(name, shape, dt=F32):
        dr[name] = dram_in(nc, name, shape, dt)
    din("ident", [128, 128])
    din("cT", [128, 8])
    din("ada_b_rep", [128, 6 * D])
    din("ada_w", [D, 6 * D])
    din("latT_all", [4, 320, TL], BF16)
    din("qT", [NH, 192, TL], BF16)
    din("sigT", [2048, TL], BF16)
    din("u_local", [TL, 512], BF16)
    din("uhalo_all", [512, 512], BF16)
    din("maskbank", [128, 32, 512], BF16)
    din("bands", [128, 4, 4, 128], BF16)
    din("sel", [128, 4, 8, 32], BF16)
    din("x", [TL, D])
    din("w_ukv", [KVLORA, 2048])
    din("w_pool", [4, 128, 256])
    din("pscaleT", [128, 8])
    din("w_out", [D, D])
    din("norm_ffn_rep", [128, D])
    din("w_router", [D, NE])
    din("b_router_rep", [128, NE])
    din("b_guT", [128, NE, 16])
    din("w_gu", [NE, D, 2 * DFF])
    din("w_down", [NE, DFF, D])
    din("b_down", [NE, D])
    if final:
        din("norm_final_rep", [128, D])
    mk = dram_out if debug else dram_tmp
    dr["mixT"] = mk(nc, "mixT", [D, TL], BF16)
    dr["x1"] = mk(nc, "x1", [TL, D], F32)
    dr["x_out"] = dram_out(nc, "x_out", [TL, D], F32)
    k = KB(nc)
    c = Ctx()
    setup_common(k, c, dr)
    compute_mod(k, c, dr)
    if 1 in phases:
        phase_attn(k, c, dr)
    if 2 in phases:
        phase_mix(k, c, dr)
    if 3 in phases:
        phase_moe(k, c, dr, final)
    k.barrier()
    cnt = k.finish()
    return nc, cnt


POOL_W = (2, 4, 8, 16)
_bf = ml_dtypes.bfloat16


def host_consts(j):
    bands = np.zeros((128, 4, 4, 128), np.float32)
    tp = np.arange(128)[:, None]
    t = np.arange(128)[None, :]
    for g, w in enumerate(POOL_W):
        inwin = (tp <= t) & (tp > t - w)
        bands[:, 0, g, :] = inwin / float(w) - (tp == t)
        if j == 0:
            cntv = np.minimum(t + 1, w).astype(np.float32)
            bands[:, 1, g, :] = inwin / cntv - (tp == t)
        else:
            bands[:, 1, g, :] = bands[:, 0, g, :]
        tprev = tp - 128
        bands[:, 2, g, :] = ((tprev > t - w) & (tp >= 96)) / float(w)
        th = tp - 32
        bands[:, 3, g, :] = ((th > t - w) & (tp >= 16) & (tp < 32)) / float(w)
    sel = np.zeros((4, 128, 8, 32), np.float32)
    groups = core_groups(j)
    for gi, G in enumerate(groups):
        if G == 0:
            continue
        jp, gip = seq_group_owner(G - 1)
        base = (jp * 8 + gip) * 16
        for r in range(16):
            R = base + r
            sel[R // 128, R % 128, gi, 16 + r] = 1.0
    sel = np.ascontiguousarray(sel.transpose(1, 0, 2, 3))
    mb = np.zeros((128, 32, 512), np.float32)
    p = np.arange(128)[:, None]
    cc = np.arange(512)[None, :]
    for side in range(2):
        for sp in range(16):
            rel = sp - 4 * j if side == 0 else sp - 12 + 4 * j
            mb[:, side * 16 + sp, :] = (rel * 128 + p <= cc)
    return dict(bands=bands.astype(_bf), sel=sel.astype(_bf), maskbank=mb.astype(_bf))


def pre_inputs(inp, L, r, xloc):
    b, j = r // 4, r % 4
    idx = local_token_index(j)
    half = ROPE // 2
    inv_freq = (10000.0 ** (-np.arange(half, dtype=np.float32) / half)).astype(np.float32)
    return dict(
        ident=np.eye(128, dtype=np.float32),
        cT=np.ascontiguousarray(inp["c"][b].reshape(8, 128).T),
        ada_b_rep=rep(inp["ada_b"][L]),
        ada_w=np.ascontiguousarray(inp["ada_w"][L]),
        w_in=np.ascontiguousarray(inp["w_in"][L]),
        w_uq=np.ascontiguousarray(inp["w_uq"][L]),
        norm_mix_rep=rep(inp["norm_mix"][L]),
        q_norm_rep=rep(inp["q_norm"][L]),
        kv_norm_rep=rep(inp["kv_norm"][L]),
        pos_rep=np.ascontiguousarray(np.broadcast_to(inp["positions"][b][idx][None, :], (64, TL))).astype(np.int32),
        inv_freq=np.concatenate([inv_freq, inv_freq]).reshape(64, 1).astype(np.float32),
        x=xloc,
    )


def post_inputs(inp, L, r, xloc, pre_out, final, consts):
    b, j = r // 4, r % 4
    ranks = [b * 4 + i for i in range(4)]
    lat_all = np.ascontiguousarray(np.stack([pre_out[q]["latT_local"] for q in ranks], 0))
    uh = np.concatenate([pre_out[q]["u_local"].reshape(8, 512, 512)[:, 496:512, :].reshape(128, 512) for q in ranks], 0)
    d = dict(
        ident=np.eye(128, dtype=np.float32),
        cT=np.ascontiguousarray(inp["c"][b].reshape(8, 128).T),
        ada_b_rep=rep(inp["ada_b"][L]),
        ada_w=np.ascontiguousarray(inp["ada_w"][L]),
        latT_all=lat_all,
        qT=pre_out[r]["qT"],
        sigT=pre_out[r]["sigT"],
        u_local=pre_out[r]["u_local"],
        uhalo_all=np.ascontiguousarray(uh),
        maskbank=consts[j]["maskbank"],
        bands=consts[j]["bands"],
        sel=consts[j]["sel"],
        x=xloc,
        w_ukv=np.ascontiguousarray(inp["w_ukv"][L]),
        w_pool=np.ascontiguousarray(inp["w_pool"][L]),
        pscaleT=np.ascontiguousarray(inp["pool_scale"][L].reshape(8, 128).T),
        w_out=np.ascontiguousarray(inp["w_out"][L]),
        norm_ffn_rep=rep(inp["norm_ffn"][L]),
        w_router=np.ascontiguousarray(inp["w_router"][L]),
        b_router_rep=rep(inp["b_router"][L]),
        b_guT=np.ascontiguousarray(inp["b_gu"][L].reshape(NE, 16, 128).transpose(2, 0, 1)),
        w_gu=np.ascontiguousarray(inp["w_gu"][L]),
        w_down=np.ascontiguousarray(inp["w_down"][L]),
        b_down=np.ascontiguousarray(inp["b_down"][L]),
    )
    if final:
        d["norm_final_rep"] = rep(inp["norm_final"])
    return d


LAYERED = dict(
    ada_b_rep=[128, 6 * D], ada_w=[D, 6 * D], w_in=[D, INC], w_uq=[QLORA, 1536], norm_mix_rep=[128, D],
    q_norm_rep=[128, QLORA], kv_norm_rep=[128, KVLORA], w_ukv=[KVLORA, 2048], w_pool=[4, 128, 256],
    pscaleT=[128, 8], w_out=[D, D], norm_ffn_rep=[128, D], w_router=[D, NE], b_router_rep=[128, NE],
    b_guT=[128, NE, 16], w_gu=[NE, D, 2 * DFF], w_down=[NE, DFF, D], b_down=[NE, D])
SHARED = dict(ident=([128, 128], F32), cT=([128, 8], F32), pos_rep=([64, TL], I32), inv_freq=([64, 1], F32),
              maskbank=([128, 32, 512], BF16), bands=([128, 4, 4, 128], BF16), sel=([128, 4, 8, 32], BF16),
              norm_final_rep=([128, D], F32), x=([TL, D], F32))
GROUPS = [[0, 1, 2, 3], [4, 5, 6, 7]]
BIG = ("w_gu", "w_down")


def build_fused():
    nc = bass.Bass("TRN2", target_bir_lowering=False)
    hin = {}
    for n, sh in LAYERED.items():
        if n in BIG:
            hin[n] = [dram_in(nc, n + "%d" % L, sh, F32) for L in range(2)]
        else:
            hin[n] = dram_in(nc, n, [2] + sh, F32)
    for n, (sh, dt) in SHARED.items():
        hin[n] = dram_in(nc, n, sh, dt)
    out = dram_out(nc, "out", [TL, D], F32)
    tmp = dict(
        u_local=dram_tmp(nc, "u_local", [TL, 512], BF16), sigT=dram_tmp(nc, "sigT", [2048, TL], BF16),
        qT=dram_tmp(nc, "qT", [NH, 192, TL], BF16),
        uh_local=dram_tmp(nc, "uh_local", [128, 512], BF16),
        uhalo_all=dram_tmp(nc, "uhalo_all", [512, 512], BF16), mixT=dram_tmp(nc, "mixT", [D, TL], BF16),
        x1=dram_tmp(nc, "x1", [TL, D], F32), xbuf=dram_tmp(nc, "xbuf", [TL, D], F32))
    PR = (128, 128, 64)
    lat_loc = [dram_tmp(nc, "lat%d" % i, [PR[i], TL], BF16) for i in range(3)]
    lat_all_t = [dram_tmp(nc, "lata%d" % i, [4 * PR[i], TL], BF16) for i in range(3)]
    k = KB(nc)
    c = Ctx()
    setup_common(k, c, hin)
    for L in range(2):
        d = {n: (hin[n][L].ap() if n in BIG else hin[n].ap()[L]) for n in LAYERED}
        for n in SHARED:
            d[n] = hin[n].ap()
        for n in tmp:
            d[n] = tmp[n].ap()
        d["lat_parts"] = [t_.ap() for t_ in lat_loc]
        d["lat_all_parts"] = [t_.ap().rearrange("(r f) t -> r f t", r=4) for t_ in lat_all_t]
        d["x"] = hin["x"].ap() if L == 0 else tmp["xbuf"].ap()
        d["x_out"] = tmp["xbuf"].ap() if L == 0 else out.ap()
        compute_mod(k, c, d)
        m = k.scope_begin()
        phase_pre(k, c, d)
        k.scope_end(m)
        if FUSE_DBG != 1:
            for t_i, t_o in zip(lat_loc, lat_all_t):
                k.coll("AllGather", t_i.ap(), t_o.ap(), GROUPS)
            k.coll("AllGather", tmp["uh_local"].ap(), tmp["uhalo_all"].ap(), GROUPS)
        k.barrier()
        phase_attn(k, c, d)
        phase_mix(k, c, d)
        phase_moe(k, c, d, L == 1)
    k.barrier()
    cnt = k.finish()
    return nc, cnt


def fused_inputs(inp, r, consts):
    b, j = r // 4, r % 4
    idx = local_token_index(j)
    half = ROPE // 2
    inv_freq = (10000.0 ** (-np.arange(half, dtype=np.float32) / half)).astype(np.float32)
    st = lambda f: np.ascontiguousarray(np.stack([f(L) for L in range(2)], 0))
    d = dict(
        ada_b_rep=st(lambda L: rep(inp["ada_b"][L])), ada_w=np.ascontiguousarray(inp["ada_w"]),
        w_in=np.ascontiguousarray(inp["w_in"]), w_uq=np.ascontiguousarray(inp["w_uq"]),
        norm_mix_rep=st(lambda L: rep(inp["norm_mix"][L])), q_norm_rep=st(lambda L: rep(inp["q_norm"][L])),
        kv_norm_rep=st(lambda L: rep(inp["kv_norm"][L])), w_ukv=np.ascontiguousarray(inp["w_ukv"]),
        w_pool=np.ascontiguousarray(inp["w_pool"]),
        pscaleT=st(lambda L: inp["pool_scale"][L].reshape(8, 128).T),
        w_out=np.ascontiguousarray(inp["w_out"]), norm_ffn_rep=st(lambda L: rep(inp["norm_ffn"][L])),
        w_router=np.ascontiguousarray(inp["w_router"]), b_router_rep=st(lambda L: rep(inp["b_router"][L])),
        b_guT=st(lambda L: inp["b_gu"][L].reshape(NE, 16, 128).transpose(2, 0, 1)),
        w_gu0=np.ascontiguousarray(inp["w_gu"][0]), w_gu1=np.ascontiguousarray(inp["w_gu"][1]),
        w_down0=np.ascontiguousarray(inp["w_down"][0]), w_down1=np.ascontiguousarray(inp["w_down"][1]),
        b_down=np.ascontiguousarray(inp["b_down"]),
        ident=np.eye(128, dtype=np.float32),
        cT=np.ascontiguousarray(inp["c"][b].reshape(8, 128).T),
        pos_rep=np.ascontiguousarray(np.broadcast_to(inp["positions"][b][idx][None, :], (64, TL))).astype(np.int32),
        inv_freq=np.concatenate([inv_freq, inv_freq]).reshape(64, 1).astype(np.float32),
        maskbank=consts[j]["maskbank"], bands=consts[j]["bands"], sel=consts[j]["sel"],
        norm_final_rep=rep(inp["norm_final"]),
        x=np.ascontiguousarray(inp["x"][b][idx]),
    )
    return d


_CACHE = {}


def kernel(**inputs):
    inp = {k_: np.asarray(v) for k_, v in inputs.items()}
    if "fused" not in _CACHE:
        _CACHE["fused"] = build_fused()[0]
    consts = [host_consts(j) for j in range(4)]
    cores = list(range(8))
    maps = [fused_inputs(inp, r, consts) for r in cores]
    res = run_bass_kernel_spmd(_CACHE["fused"], maps, core_ids=cores)
    out = np.empty((2, S, D), np.float32)
    for r in cores:
        out[r // 4][local_token_index(r % 4)] = res.results[r]["out"]
    return out
```

```python
import math
import numpy as np
import ml_dtypes
import concourse.bass as bass
import concourse.mybir as mybir
from concourse.bass_utils import run_bass_kernel_spmd

F32 = mybir.dt.float32
BF16 = mybir.dt.bfloat16
I32 = mybir.dt.int32
ALU = mybir.AluOpType
AF = mybir.ActivationFunctionType
AX = mybir.AxisListType

D = 1024
S = 16384
NH = 8
QLORA = 384
KVLORA = 256
ROPE = 64
INC = 3264
NE = 32
DFF = 1024
EPS = 1e-6
TL = 4096
NG = 8
SCALE = 1.0 / math.sqrt(192.0)
SAME_SYNC = True
MOE_DBG = 0
NE_RUN = 32
FUSE_DBG = 0


class Buf:
    def __init__(self, t, name):
        self.t = t
        self.name = name
        self.w = None
        self.rs = []
        self.dsems = {}

    def __getitem__(self, idx):
        return self.t[idx]


class Op:
    __slots__ = ("stream", "fn", "reads", "writes", "dma", "dbuf", "deps", "sig", "signal", "barrier", "idx", "inc", "release")

    def __init__(self, stream, fn, reads, writes, dma=False, dbuf=None, barrier=False):
        self.stream = stream
        self.fn = fn
        self.reads = reads
        self.writes = writes
        self.dma = dma
        self.dbuf = dbuf
        self.deps = []
        self.sig = False
        self.signal = None
        self.barrier = barrier
        self.inc = 16
        self.release = None


class KB:
    STREAMS = ("pe", "act", "dve", "pool", "sp")

    def __init__(self, nc):
        self.nc = nc
        self.ops = []
        self.eng = dict(pe=nc.tensor, act=nc.scalar, dve=nc.vector, pool=nc.gpsimd, sp=nc.sync)
        self.bufs = []
        self.guards = []
        self.gbufs = []
        self.n = 0

    def sb(self, shape, dt, name=None):
        self.n += 1
        name = (name or "t") + "_%d" % self.n
        g = self.nc.sbuf_tensor(name, list(shape), dt)
        b = Buf(g.__enter__(), name)
        self.guards.append(g)
        self.gbufs.append((g, b))
        self.bufs.append(b)
        return b

    def scope_begin(self):
        return len(self.guards)

    def scope_end(self, mark):
        self.barrier()
        rel = Op(None, None, [], [])
        rel.release = [b for (_, b) in self.gbufs[mark:]]
        self.ops.append(rel)
        while len(self.guards) > mark:
            self.guards.pop().__exit__(None, None, None)
            self.gbufs.pop()

    def coll(self, kind, in_ap, out_ap, groups):
        b = Buf(None, "cc%d" % self.n)
        self.n += 1
        fn = lambda e: e.collective_compute(kind, ALU.bypass, replica_groups=groups, ins=[in_ap], outs=[out_ap])
        o = Op("pool", fn, [], [b], dma=True, dbuf=b)
        o.inc = 1
        self.ops.append(o)

    def ps(self, shape, dt=F32, name=None):
        self.n += 1
        name = (name or "p") + "_%d" % self.n
        b = Buf(self.nc.alloc_psum_tensor(name, list(shape), dt), name)
        self.bufs.append(b)
        return b

    def op(self, stream, fn, reads=(), writes=()):
        self.ops.append(Op(stream, fn, list(reads), list(writes)))

    def dma(self, q, out_ap, in_ap, buf, load, extra_reads=(), **kw):
        fn = lambda e: e.dma_start(out=out_ap, in_=in_ap, **kw)
        if load:
            o = Op(q, fn, list(extra_reads), [buf], dma=True, dbuf=buf)
        else:
            o = Op(q, fn, [buf] + list(extra_reads), [], dma=True, dbuf=buf)
        self.ops.append(o)

    def barrier(self):
        self.ops.append(Op(None, None, [], [], barrier=True))

    def finish(self):
        nc = self.nc
        last = {s: None for s in self.STREAMS}
        dma_last = {}
        expanded = []
        for o in self.ops:
            if o.release is not None:
                for b in o.release:
                    for kk_ in ("sp", "pool", "cc"):
                        dma_last.pop((id(b), kk_), None)
                expanded.append(o)
                continue
            if o.barrier:
                for s in self.STREAMS:
                    p = Op(s, None, [], [])
                    p.idx = len(expanded)
                    deps = [last[s2] for s2 in self.STREAMS if s2 != s and last[s2] is not None and not last[s2].dma]
                    deps += list(dma_last.values())
                    p.deps = deps
                    for d in deps:
                        if not d.dma:
                            d.sig = True
                    expanded.append(p)
                continue
            deps = set()
            for b in o.reads:
                if b.w is not None:
                    deps.add(b.w)
            for b in o.writes:
                if b.w is not None:
                    deps.add(b.w)
                for r in b.rs:
                    deps.add(r)
            deps.discard(o)
            for b in o.writes:
                b.w = o
                b.rs = []
            for b in o.reads:
                if b not in o.writes:
                    b.rs.append(o)
            fin = []
            best = {}
            for d in deps:
                if d.dma:
                    fin.append(d)
                    continue
                if d.stream == o.stream and (o.stream == "pe" or not SAME_SYNC):
                    continue
                if d.stream not in best or best[d.stream].idx < d.idx:
                    best[d.stream] = d
            for d in best.values():
                d.sig = True
                fin.append(d)
            o.deps = fin
            o.idx = len(expanded)
            last[o.stream] = o
            if o.dma:
                dma_last[(id(o.dbuf), "cc" if o.inc == 1 else o.stream)] = o
            expanded.append(o)
        sems = {s: nc.alloc_semaphore("s_" + s) for s in self.STREAMS}
        cnt = {s: 0 for s in self.STREAMS}
        seen = {s: {} for s in self.STREAMS}
        free_sems = {}
        for o in expanded:
            if o.release is not None:
                for b in o.release:
                    for kind, (sem_, cnt_) in b.dsems.items():
                        free_sems.setdefault(kind, []).append((sem_, cnt_))
                    b.dsems = {}
                continue
            e = self.eng[o.stream]
            need = {}
            for d in o.deps:
                sem, val = d.signal
                k = id(sem)
                if k not in need or need[k][1] < val:
                    need[k] = (sem, val)
            for k, (sem, val) in need.items():
                if seen[o.stream].get(k, 0) < val:
                    e.wait_ge(sem, val)
                    seen[o.stream][k] = val
            if o.fn is None:
                continue
            ins = o.fn(e)
            if o.dma:
                b = o.dbuf
                kind = "cc" if o.inc == 1 else o.stream
                if kind not in b.dsems:
                    fl = free_sems.get(kind)
                    if fl:
                        b.dsems[kind] = fl.pop()
                    else:
                        b.dsems[kind] = (nc.alloc_semaphore("d_%s_%s" % (kind, b.name)), 0)
                sem_, cnt_ = b.dsems[kind]
                cnt_ += o.inc
                b.dsems[kind] = (sem_, cnt_)
                ins.then_inc(sem_, o.inc)
                o.signal = (sem_, cnt_)
            elif o.sig:
                cnt[o.stream] += 1
                ins.then_inc(sems[o.stream], 1)
                o.signal = (sems[o.stream], cnt[o.stream])
        return cnt


def mm(k, out_buf, out_ap, lhsT_buf, lhsT_ap, rhs_buf, rhs_ap, start, stop):
    k.op("pe", lambda e: e.matmul(out_ap, lhsT_ap, rhs_ap, start=start, stop=stop),
         reads=[lhsT_buf, rhs_buf], writes=[out_buf])


def tr(k, out_buf, out_ap, in_buf, in_ap, ident_buf, ident_ap):
    k.op("pe", lambda e: e.transpose(out_ap, in_ap, ident_ap), reads=[in_buf, ident_buf], writes=[out_buf])


class Ctx:
    pass


def AP_(x):
    try:
        return x.ap()
    except TypeError:
        return x


def dram_in(nc, name, shape, dt):
    return nc.dram_tensor(name, list(shape), dt, kind="ExternalInput")


def dram_out(nc, name, shape, dt):
    return nc.dram_tensor(name, list(shape), dt, kind="ExternalOutput")


def dram_tmp(nc, name, shape, dt):
    return nc.dram_tensor(name, list(shape), dt, kind="Internal")


def setup_common(k, c, dr):
    nc = k.nc
    c.ps = [k.ps([128, 512], F32, "bank%d" % i) for i in range(8)]
    c.ident_f = k.sb([128, 128], F32, "identf")
    c.ident_b = k.sb([128, 128], BF16, "identb")
    k.dma("sp", c.ident_f[:, :], AP_(dr["ident"])[:, :], c.ident_f, True)
    k.op("dve", lambda e: e.tensor_copy(c.ident_b[:, :], c.ident_f[:, :]), [c.ident_f], [c.ident_b])
    c.neghalf = k.sb([128, 1], F32, "neghalf")
    k.op("pool", lambda e: e.memset(c.neghalf[:, :], -0.5), [], [c.neghalf])
    c.ones_b = k.sb([128, 128], BF16, "onesb")
    k.op("pool", lambda e: e.memset(c.ones_b[:, :], 1.0), [], [c.ones_b])
    c.e0 = k.sb([128, 128], BF16, "e0row")
    k.op("pool", lambda e: e.memset(c.e0[:, :], 0.0), [], [c.e0])
    k.op("pool", lambda e: e.memset(c.e0[0:1, :], 1.0), [c.e0], [c.e0])
    c.ones_f = k.sb([128, 128], F32, "onesf")
    k.op("pool", lambda e: e.memset(c.ones_f[:, :], 1.0), [], [c.ones_f])


def compute_mod(k, c, dr):
    if not hasattr(c, "mod"):
        c.mod = k.sb([128, 6 * D], F32, "modrep")
    k.dma("sp", c.mod[:, :], AP_(dr["ada_b_rep"])[:, :], c.mod, True)
    mark = k.scope_begin()
    cT = k.sb([128, 8], F32, "cT")
    k.dma("sp", cT[:, :], AP_(dr["cT"])[:, :], cT, True)
    cact = k.sb([128, 8], F32, "cact")
    k.op("act", lambda e: e.activation(cact[:, :], cT[:, :], AF.Silu), [cT], [cact])
    cb = k.sb([128, 8, 128], F32, "cactb")
    k.op("pool", lambda e: e.memset(cb[:, :, :], 1.0), [], [cb])
    for kc in range(8):
        k.op("dve", lambda e, kc=kc: e.tensor_scalar(cb[:, kc, :], cb[:, kc, :], cact[:, kc:kc + 1], None, ALU.mult),
             [cb, cact], [cb])
    wbufs = [k.sb([128, 8, 256], F32, "adaw%d" % i) for i in range(2)]
    aw = AP_(dr["ada_w"]).rearrange("(kc p) n -> p kc n", p=128)
    for cg in range(24):
        wb = wbufs[cg % 2]
        k.dma("sp", wb[:, :, :], aw[:, :, cg * 256:(cg + 1) * 256], wb, True)
        pb = c.ps[cg % 2]
        for kc in range(8):
            mm(k, pb, pb[:, 0:256], cb, cb[:, kc, :], wb, wb[:, kc, :], kc == 0, kc == 7)
        k.op("dve", lambda e, pb=pb, cg=cg: e.tensor_tensor(c.mod[:, cg * 256:(cg + 1) * 256], pb[:, 0:256],
                                                           c.mod[:, cg * 256:(cg + 1) * 256], ALU.add),
             [pb, c.mod], [c.mod])
    k.scope_end(mark)


def rms_rstd(k, c, x_buf, x_ap, n, junk, ss, rstd):
    k.op("pool", lambda e: e.memset(ss[:, :], 0.0), [], [ss])
    k.op("dve", lambda e: e.scalar_tensor_tensor(out=junk, in0=x_ap, scalar=1.0, in1=x_ap, op0=ALU.mult,
                                                 op1=ALU.mult, accum_out=ss[:, :]), [x_buf, ss], [ss, c.junkb])
    k.op("dve", lambda e: e.tensor_scalar(ss[:, :], ss[:, :], 1.0 / n, EPS, ALU.mult, ALU.add), [ss], [ss])
    k.op("pool", lambda e: e.tensor_tensor(rstd[:, :], ss[:, :], c.neghalf[:, :], ALU.pow), [ss, c.neghalf], [rstd])


def phase_pre(k, c, dr):
    nc = k.nc
    win = k.sb([128, 8, INC], BF16, "win")
    winap = AP_(dr["w_in"]).rearrange("(kc p) n -> p kc n", p=128)
    for kc in range(8):
        for c0 in range(0, INC, 1632):
            k.dma("pool", win[:, kc, c0:c0 + 1632], winap[:, kc, c0:c0 + 1632], win, True)
    winrot = k.sb([128, 8, 64], BF16, "winrot")
    for kc in range(8):
        k.dma("pool", winrot[:, kc, 0:32], winap[:, kc, 640 + 32:640 + 64], winrot, True)
        k.dma("pool", winrot[:, kc, 32:64], winap[:, kc, 640:640 + 32], winrot, True)
    k.op("dve", lambda e: e.tensor_scalar(winrot[:, :, 0:32], winrot[:, :, 0:32], -1.0, None, ALU.mult), [winrot], [winrot])
    wuq = k.sb([128, 3, 1536], BF16, "wuq")
    wuqrot = k.sb([128, 3, NH, 64], BF16, "wuqrot")
    mark = k.scope_begin()
    wuq_f = k.sb([128, 3, 1536], F32, "wuqf")
    wuqap = AP_(dr["w_uq"]).rearrange("(kc p) n -> p kc n", p=128)
    k.dma("sp", wuq_f[:, :, :], wuqap, wuq_f, True)
    k.op("dve", lambda e: e.tensor_scalar(wuq[:, :, :], wuq_f[:, :, :], SCALE, None, ALU.mult), [wuq_f], [wuq])
    wv = wuq_f[:, :, :].rearrange("p k (h d) -> p k h d", h=NH)
    k.op("dve", lambda e: e.tensor_scalar(wuqrot[:, :, :, 0:32], wv[:, :, :, 160:192], -SCALE, None, ALU.mult), [wuq_f], [wuqrot])
    k.op("dve", lambda e: e.tensor_scalar(wuqrot[:, :, :, 32:64], wv[:, :, :, 128:160], SCALE, None, ALU.mult), [wuq_f], [wuqrot])
    k.scope_end(mark)
    A = k.sb([128, D], F32, "Arep")
    k.dma("sp", A[:, :], AP_(dr["norm_mix_rep"])[:, :], A, True)
    qn = k.sb([128, QLORA], F32, "qnrep")
    k.dma("sp", qn[:, :], AP_(dr["q_norm_rep"])[:, :], qn, True)
    kvn = k.sb([128, KVLORA], F32, "kvnrep")
    k.dma("sp", kvn[:, :], AP_(dr["kv_norm_rep"])[:, :], kvn, True)
    k.op("dve", lambda e: e.scalar_tensor_tensor(out=A[:, :], in0=c.mod[:, D:2 * D], scalar=1.0, in1=A[:, :],
                                                 op0=ALU.add, op1=ALU.mult), [c.mod, A], [A])
    cosT = k.sb([64, TL], F32, "cosT")
    sinT = k.sb([64, TL], F32, "sinT")
    mark = k.scope_begin()
    invf = k.sb([64, 1], F32, "invf")
    k.dma("sp", invf[:, :], AP_(dr["inv_freq"])[:, :], invf, True)
    CH = 1024
    posi = k.sb([64, CH], I32, "posi")
    ang = k.sb([64, CH], F32, "ang")
    tq = k.sb([64, CH], F32, "tq")
    ti = k.sb([64, CH], I32, "ti")
    C1 = 6.28125
    C2 = 2.0 * math.pi - C1
    for ch in range(TL // CH):
        sl = slice(ch * CH, (ch + 1) * CH)
        k.dma("sp", posi[:, :], AP_(dr["pos_rep"])[:, sl], posi, True)
        k.op("dve", lambda e: e.tensor_copy(ang[:, :], posi[:, :]), [posi], [ang])
        k.op("dve", lambda e: e.tensor_scalar(ang[:, :], ang[:, :], invf[:, 0:1], None, ALU.mult), [ang, invf], [ang])
        for (dstb, shift) in ((sinT, 0.0), (cosT, math.pi / 2)):
            dst = dstb[:, sl]
            k.op("dve", lambda e, shift=shift: e.tensor_scalar(tq[:, :], ang[:, :], shift, 1.0 / (2 * math.pi), ALU.add, ALU.mult), [ang], [tq])
            k.op("dve", lambda e: e.tensor_copy(ti[:, :], tq[:, :]), [tq], [ti])
            k.op("dve", lambda e: e.tensor_copy(tq[:, :], ti[:, :]), [ti], [tq])
            k.op("dve", lambda e, dst=dst, shift=shift: e.tensor_scalar(dst, ang[:, :], shift, None, ALU.add), [ang], [dstb])
            k.op("dve", lambda e, dst=dst: e.scalar_tensor_tensor(out=dst, in0=tq[:, :], scalar=-C1, in1=dst, op0=ALU.mult, op1=ALU.add), [tq, dstb], [dstb])
            k.op("dve", lambda e, dst=dst: e.scalar_tensor_tensor(out=dst, in0=tq[:, :], scalar=-C2, in1=dst, op0=ALU.mult, op1=ALU.add), [tq, dstb], [dstb])
            k.op("dve", lambda e, dst=dst: e.tensor_scalar(tq[:, :], dst, math.pi, -2 * math.pi, ALU.is_gt, ALU.mult), [dstb], [tq])
            k.op("dve", lambda e, dst=dst: e.tensor_tensor(dst, dst, tq[:, :], ALU.add), [dstb, tq], [dstb])
            k.op("dve", lambda e, dst=dst: e.tensor_scalar(tq[:, :], dst, -math.pi, 2 * math.pi, ALU.is_lt, ALU.mult), [dstb], [tq])
            k.op("dve", lambda e, dst=dst: e.tensor_tensor(dst, dst, tq[:, :], ALU.add), [dstb, tq], [dstb])
            k.op("dve", lambda e, dst=dst: e.tensor_scalar(dst, dst, math.pi, -math.pi, ALU.min, ALU.max), [dstb], [dstb])
            k.op("act", lambda e, dst=dst: e.activation(dst, dst, AF.Sin), [dstb], [dstb])
    k.scope_end(mark)

    xt = [k.sb([128, D], F32, "xt%d" % i) for i in range(2)]
    h32 = k.sb([128, D], F32, "h32")
    hb = [k.sb([128, D], BF16, "hb%d" % i) for i in range(2)]
    c.junkb = k.sb([128, D], BF16, "junk")
    ss = [k.sb([128, 1], F32, "ss%d" % i) for i in range(2)]
    rstd = [k.sb([128, 1], F32, "rstd%d" % i) for i in range(2)]
    hT = [k.sb([128, 8, 512], BF16, "hT%d" % i) for i in range(2)]
    zs = k.sb([128, 640], F32, "zs")
    ssq = k.sb([128, 1], F32, "ssq")
    rsq = k.sb([128, 1], F32, "rsq")
    ssk = k.sb([128, 1], F32, "ssk")
    rsk = k.sb([128, 1], F32, "rsk")
    cqb = k.sb([128, 640], BF16, "cqb")
    cqT = k.sb([128, 3, 512], BF16, "cqT")
    latT = k.sb([128, 2, 512], BF16, "latT")
    ub = [k.sb([128, 512], BF16, "ub%d" % i) for i in range(2)]
    sg = [k.sb([128, 512], BF16, "sg%d" % i) for i in range(3)]
    kr1 = k.sb([64, 512], F32, "kr1")
    kr2 = k.sb([64, 512], F32, "kr2")
    krb = [k.sb([64, 512], BF16, "krb%d" % i) for i in range(2)]
    qnb = [k.sb([128, 512], BF16, "qnb%d" % i) for i in range(2)]
    qrb = [k.sb([64, 512], BF16, "qrb%d" % i) for i in range(2)]
    x_d = AP_(dr["x"])
    u_d = AP_(dr["u_local"])
    sig_d = AP_(dr["sigT"])
    lat_d = AP_(dr["latT_local"]) if "latT_local" in dr else None
    q_d = AP_(dr["qT"])
    ps = c.ps
    psb = [p[:, :].bitcast(BF16) for p in ps]
    nst = 0
    for g in range(NG):
        hTg = hT[g % 2]
        for tt in range(4):
            t = g * 4 + tt
            xb = xt[t % 2]
            k.dma("sp", xb[:, :], x_d[t * 128:(t + 1) * 128, :], xb, True)
            s_, r_ = ss[t % 2], rstd[t % 2]
            rms_rstd(k, c, xb, xb[:, :], D, c.junkb[:, :], s_, r_)
            k.op("dve", lambda e, xb=xb, r_=r_: e.scalar_tensor_tensor(out=h32[:, :], in0=xb[:, :], scalar=r_[:, 0:1], in1=A[:, :],
                                                                     op0=ALU.mult, op1=ALU.mult), [xb, r_, A], [h32])
            hbb = hb[t % 2]
            k.op("pool", lambda e, hbb=hbb: e.tensor_tensor(hbb[:, :], h32[:, :], c.mod[:, 0:D], ALU.add), [h32, c.mod], [hbb])
            pt = ps[7]
            for kc in range(8):
                tr(k, pt, psb[7][:, kc * 128:(kc + 1) * 128], hbb, hbb[:, kc * 128:(kc + 1) * 128], c.ident_b, c.ident_b[:, :])
            k.op("act", lambda e, hTg=hTg, tt=tt: e.copy(hTg[:, :, tt * 128:(tt + 1) * 128],
                                                        psb[7][:, :].rearrange("p (k t) -> p k t", k=8)), [pt], [hTg])
        for tt in range(4):
            t = g * 4 + tt
            cols = ((0, 512), (512, 1024), (1024, 1216))
            for bi, (c0, c1) in enumerate(cols):
                for kc in range(8):
                    mm(k, ps[bi], ps[bi][:, 0:c1 - c0], hTg, hTg[:, kc, tt * 128:(tt + 1) * 128], win, win[:, kc, c0:c1], kc == 0, kc == 7)
            k.op("act", lambda e: e.copy(zs[:, 0:512], ps[0][:, 0:512]), [ps[0]], [zs])
            k.op("act", lambda e: e.copy(zs[:, 512:640], ps[1][:, 0:128]), [ps[1]], [zs])
            ubb = ub[t % 2]
            k.op("act", lambda e, ubb=ubb: e.copy(ubb[:, 0:320], ps[1][:, 192:512]), [ps[1]], [ubb])
            k.op("act", lambda e, ubb=ubb: e.copy(ubb[:, 320:512], ps[2][:, 0:192]), [ps[2]], [ubb])
            k.dma("pool", u_d[t * 128:(t + 1) * 128, :], ubb[:, :], ubb, False)
            if tt == 3 and "uh_local" in dr:
                k.dma("pool", AP_(dr["uh_local"])[g * 16:(g + 1) * 16, :], ubb[112:128, :], ubb, False)
            rms_rstd(k, c, zs, zs[:, 0:QLORA], QLORA, c.junkb[:, 0:QLORA], ssq, rsq)
            rms_rstd(k, c, zs, zs[:, QLORA:640], KVLORA, c.junkb[:, 0:KVLORA], ssk, rsk)
            k.op("dve", lambda e: e.scalar_tensor_tensor(out=cqb[:, 0:QLORA], in0=zs[:, 0:QLORA], scalar=rsq[:, 0:1], in1=qn[:, :],
                                                         op0=ALU.mult, op1=ALU.mult), [zs, rsq, qn], [cqb])
            k.op("dve", lambda e: e.scalar_tensor_tensor(out=cqb[:, QLORA:640], in0=zs[:, QLORA:640], scalar=rsk[:, 0:1], in1=kvn[:, :],
                                                         op0=ALU.mult, op1=ALU.mult), [zs, rsk, kvn], [cqb])
            pt = ps[7]
            for kc in range(5):
                tr(k, pt, psb[7][:, kc * 128:(kc + 1) * 128], cqb, cqb[:, kc * 128:(kc + 1) * 128], c.ident_b, c.ident_b[:, :])
            k.op("act", lambda e, tt=tt: e.copy(cqT[:, :, tt * 128:(tt + 1) * 128],
                                               psb[7][:, 0:384].rearrange("p (k t) -> p k t", k=3)), [pt], [cqT])
            k.op("act", lambda e, tt=tt: e.copy(latT[:, :, tt * 128:(tt + 1) * 128],
                                               psb[7][:, 384:640].rearrange("p (k t) -> p k t", k=2)), [pt], [latT])
        tok = slice(g * 512, (g + 1) * 512)
        for kc in range(2):
            dst = dr["lat_parts"][kc][:, tok] if "lat_parts" in dr else lat_d[kc * 128:(kc + 1) * 128, tok]
            k.dma("pool", dst, latT[:, kc, :], latT, False)
        for f in range(16):
            pb = ps[3 + (f % 3)]
            for kc in range(8):
                mm(k, pb, pb[:, :], win, win[:, kc, 1216 + f * 128:1216 + (f + 1) * 128], hTg, hTg[:, kc, :], kc == 0, kc == 7)
            sgb = sg[nst % 3]
            nst += 1
            k.op("act", lambda e, sgb=sgb, pb=pb: e.activation(sgb[:, :], pb[:, :], AF.Sigmoid), [pb], [sgb])
            k.dma("pool", sig_d[f * 128:(f + 1) * 128, tok], sgb[:, :], sgb, False)
        for kc in range(8):
            mm(k, ps[6], ps[6][0:64, :], win, win[:, kc, 640:704], hTg, hTg[:, kc, :], kc == 0, kc == 7)
        for kc in range(8):
            mm(k, ps[3], ps[3][0:64, :], winrot, winrot[:, kc, :], hTg, hTg[:, kc, :], kc == 0, kc == 7)
        krbb = krb[g % 2]
        k.op("dve", lambda e, tok=tok: e.tensor_tensor(kr1[:, :], ps[6][0:64, :], cosT[:, tok], ALU.mult), [ps[6], cosT], [kr1])
        k.op("dve", lambda e, tok=tok: e.tensor_tensor(kr2[:, :], ps[3][0:64, :], sinT[:, tok], ALU.mult), [ps[3], sinT], [kr2])
        k.op("pool", lambda e, krbb=krbb: e.tensor_tensor(krbb[:, :], kr1[:, :], kr2[:, :], ALU.add), [kr1, kr2], [krbb])
        k.dma("pool", dr["lat_parts"][2][:, tok] if "lat_parts" in dr else lat_d[256:320, tok], krbb[:, :], krbb, False)
        for h in range(NH):
            pb = ps[4 + (h % 2)]
            for kc in range(3):
                mm(k, pb, pb[:, :], wuq, wuq[:, kc, h * 192:h * 192 + 128], cqT, cqT[:, kc, :], kc == 0, kc == 2)
            qb = qnb[h % 2]
            k.op("act", lambda e, qb=qb, pb=pb: e.copy(qb[:, :], pb[:, :]), [pb], [qb])
            k.dma("pool", q_d[h, 0:128, tok], qb[:, :], qb, False)
            for kc in range(3):
                mm(k, ps[6], ps[6][0:64, :], wuq, wuq[:, kc, h * 192 + 128:h * 192 + 192], cqT, cqT[:, kc, :], kc == 0, kc == 2)
            for kc in range(3):
                mm(k, ps[3], ps[3][0:64, :], wuqrot, wuqrot[:, kc, h, :], cqT, cqT[:, kc, :], kc == 0, kc == 2)
            kb_ = qrb[h % 2]
            k.op("dve", lambda e, tok=tok: e.tensor_tensor(kr1[:, :], ps[6][0:64, :], cosT[:, tok], ALU.mult), [ps[6], cosT], [kr1])
            k.op("dve", lambda e, tok=tok: e.tensor_tensor(kr2[:, :], ps[3][0:64, :], sinT[:, tok], ALU.mult), [ps[3], sinT], [kr2])
            k.op("pool", lambda e, kb_=kb_: e.tensor_tensor(kb_[:, :], kr1[:, :], kr2[:, :], ALU.add), [kr1, kr2], [kb_])
            k.dma("pool", q_d[h, 128:192, tok], kb_[:, :], kb_, False)


def k_q_rope_bufs(k, c):
    if not hasattr(c, "_qrb"):
        c._qrb = [k.sb([64, 512], BF16, "qrb%d" % i) for i in range(2)]
    return c._qrb


def TT(k, eng, ob, o, ab, a, bb, b, op):
    k.op(eng, lambda e: e.tensor_tensor(o, a, b, op), [ab, bb], [ob])


def TS(k, eng, ob, o, ab, a, s1, s2, op0, op1=None, rd=()):
    if op1 is None:
        k.op(eng, lambda e: e.tensor_scalar(o, a, s1, None, op0), [ab] + list(rd), [ob])
    else:
        k.op(eng, lambda e: e.tensor_scalar(o, a, s1, s2, op0, op1), [ab] + list(rd), [ob])


def STT(k, eng, ob, o, ab, a, sc, bb, b, op0, op1, rd=()):
    k.op(eng, lambda e: e.scalar_tensor_tensor(out=o, in0=a, scalar=sc, in1=b, op0=op0, op1=op1), [ab, bb] + list(rd), [ob])


def ACTF(k, ob, o, ib, i, func, scale=1.0, bias=0.0, rd=()):
    k.op("act", lambda e: e.activation(o, i, func, bias=bias, scale=scale), [ib] + list(rd), [ob])


def CP(k, eng, ob, o, ib, i):
    if eng == "act":
        k.op("act", lambda e: e.copy(o, i), [ib], [ob])
    else:
        k.op(eng, lambda e: e.tensor_copy(o, i), [ib], [ob])


def seq_group_owner(G):
    kk, m = G // 8, G % 8
    if m < 4:
        return m, 2 * kk
    return 7 - m, 2 * kk + 1


def phase_attn(k, c, dr):
    ps = c.ps
    psb = [p[:, :].bitcast(BF16) for p in ps]
    mark = k.scope_begin()
    KT = k.sb([128, S], BF16, "KT")
    krT = k.sb([128, S], BF16, "krT")
    V = k.sb([128, 128, 130], BF16, "V")
    masks = k.sb([128, 32, 512], BF16, "masks")
    qn = k.sb([128, TL], BF16, "qn")
    qr = k.sb([128, TL], BF16, "qr")
    latb = [k.sb([128, 2, 512], BF16, "latb%d" % i) for i in range(2)]
    wkv = [k.sb([128, 2, 256], BF16, "wkv%d" % i) for i in range(2)]
    PT = [k.sb([128, 512], BF16, "PT%d" % i) for i in range(8)]
    pacc = [k.sb([128, 512], F32, "pacc%d" % i) for i in range(4)]
    rls = [k.sb([128, 512], F32, "rls%d" % i) for i in range(2)]
    sga = [k.sb([128, 512], BF16, "sga%d" % i) for i in range(4)]
    mxo = [k.sb([128, 512], BF16, "mxo%d" % i) for i in range(2)]
    lat_all = AP_(dr["latT_all"]) if "latT_all" in dr else None
    q_d = AP_(dr["qT"])
    sig_d = AP_(dr["sigT"])
    mix_d = AP_(dr["mixT"])
    wukv = AP_(dr["w_ukv"]).rearrange("(kc p) n -> p kc n", p=128)
    mk_d = AP_(dr["maskbank"])
    for i in range(4):
        k.dma("sp", masks[:, i * 8:(i + 1) * 8, :], mk_d[:, i * 8:(i + 1) * 8, :], masks, True)
    k.op("pool", lambda e: e.memset(krT[64:128, :], 0.0), [], [krT])
    k.op("pool", lambda e: e.memset(qr[64:128, :], 0.0), [], [qr])
    for G in range(32):
        jp, gi = seq_group_owner(G)
        src = dr["lat_all_parts"][2][jp, :, gi * 512:(gi + 1) * 512] if "lat_all_parts" in dr else lat_all[jp, 256:320, gi * 512:(gi + 1) * 512]
        k.dma("sp", krT[0:64, G * 512:(G + 1) * 512], src, krT, True)
    nld = 0
    npt = 0
    nfin = 0
    for h in range(NH):
        wb = wkv[h % 2]
        k.dma("pool", wb[:, :, :], wukv[:, :, h * 256:(h + 1) * 256], wb, True)
        k.dma("sp", qn[:, :], q_d[h, 0:128, :], qn, True)
        k.dma("sp", qr[0:64, :], q_d[h, 128:192, :], qr, True)
        for G in range(32):
            jp, gi = seq_group_owner(G)
            lb = latb[nld % 2]
            nld += 1
            if "lat_all_parts" in dr:
                for kc in range(2):
                    k.dma("sp", lb[:, kc, :], dr["lat_all_parts"][kc][jp, :, gi * 512:(gi + 1) * 512], lb, True)
            else:
                k.dma("sp", lb[:, :, :], lat_all[jp, 0:256, gi * 512:(gi + 1) * 512].rearrange("(kc p) t -> p kc t", p=128), lb, True)
            pk = ps[G % 2]
            for kc in range(2):
                mm(k, pk, pk[:, :], wb, wb[:, kc, 0:128], lb, lb[:, kc, :], kc == 0, kc == 1)
            CP(k, "act" if G % 2 == 0 else "dve", KT, KT[:, G * 512:(G + 1) * 512], pk, pk[:, :])
            pv = ps[2 + G % 2]
            for a in range(4):
                for kc in range(2):
                    mm(k, pv, pv[:, a * 128:(a + 1) * 128], lb, lb[:, kc, a * 128:(a + 1) * 128], wb, wb[:, kc, 128:256], kc == 0, kc == 1)
            CP(k, "dve" if G % 2 == 0 else "act", V, V[:, 4 * G:4 * G + 4, 0:128], pv, pv[:, :].rearrange("p (a d) -> p a d", a=4))
        for SL in ((4, 5, 6, 7), (0, 1, 2, 3)):
            nst = {s_: 32 * (s_ // 2) + 16 + 16 * (s_ % 2) for s_ in SL}
            maxlen = max(nst.values())
            for idx, s_ in enumerate(SL):
                sgb = sga[idx]
                k.dma("sp", sgb[:, :], sig_d[h * 128:(h + 1) * 128, s_ * 512:(s_ + 1) * 512], sgb, True)
            for st in range(maxlen + 1):
                if st < maxlen:
                    act_ = [(idx, s_) for idx, s_ in enumerate(SL) if st < nst[s_]]
                    for idx, s_ in act_:
                        pS = ps[idx]
                        mm(k, pS, pS[:, :], KT, KT[:, st * 128:(st + 1) * 128], qn, qn[:, s_ * 512:(s_ + 1) * 512], True, False)
                    for idx, s_ in act_:
                        pS = ps[idx]
                        mm(k, pS, pS[:, :], krT, krT[:, st * 128:(st + 1) * 128], qr, qr[:, s_ * 512:(s_ + 1) * 512], False, True)
                    for idx, s_ in act_:
                        pS = ps[idx]
                        pt = PT[idx * 2 + st % 2]
                        ACTF(k, pt, pt[:, :], pS, pS[:, :], AF.Exp)
                        sp_ = st - (nst[s_] - 16)
                        if sp_ >= 0:
                            TT(k, "dve", pt, pt[:, :], pt, pt[:, :], masks, masks[:, (s_ % 2) * 16 + sp_, :], ALU.mult)
                if st >= 1:
                    stp = st - 1
                    act_ = [(idx, s_) for idx, s_ in enumerate(SL) if stp < nst[s_]]
                    for idx, s_ in act_:
                        po = ps[4 + idx]
                        pt = PT[idx * 2 + stp % 2]
                        mm(k, po, po[:, :], V, V[:, stp, 0:128], pt, pt[:, :], stp == 0, stp == nst[s_] - 1)
                    for idx, s_ in act_:
                        pt = PT[idx * 2 + stp % 2]
                        pa = pacc[idx]
                        if stp == 0:
                            CP(k, "dve", pa, pa[:, :], pt, pt[:, :])
                        else:
                            TT(k, "dve", pa, pa[:, :], pa, pa[:, :], pt, pt[:, :], ALU.add)
                    for idx, s_ in act_:
                        if stp != nst[s_] - 1:
                            continue
                        po = ps[4 + idx]
                        pa = pacc[idx]
                        sgb = sga[idx]
                        qc = slice(s_ * 512, (s_ + 1) * 512)
                        pl_ = ps[idx]
                        mm(k, pl_, pl_[:, :], c.ones_f, c.ones_f[:, :], pa, pa[:, :], True, True)
                        rl = rls[idx % 2]
                        k.op("dve", lambda e, rl=rl, pl_=pl_: e.reciprocal(rl[:, :], pl_[:, :]), [pl_], [rl])
                        TT(k, "pool", rl, rl[:, :], rl, rl[:, :], sgb, sgb[:, :], ALU.mult)
                        mo = mxo[idx % 2]
                        TT(k, "dve", mo, mo[:, :], po, po[:, :], rl, rl[:, :], ALU.mult)
                        k.dma("pool", mix_d[h * 128:(h + 1) * 128, qc], mo[:, :], mo, False)
    k.scope_end(mark)


def phase_mix(k, c, dr):
    ps = c.ps
    mark = k.scope_begin()
    wpool = k.sb([128, 4, 256], BF16, "wpool")
    k.dma("pool", wpool[:, :, :], AP_(dr["w_pool"]).rearrange("g c d -> c g d"), wpool, True)
    pscale = k.sb([128, 8], F32, "pscale")
    k.dma("sp", pscale[:, :], AP_(dr["pscaleT"])[:, :], pscale, True)
    wout = k.sb([128, 8, D], BF16, "wout")
    woap = AP_(dr["w_out"]).rearrange("(kc p) n -> p kc n", p=128)
    for kc in range(8):
        k.dma("pool", wout[:, kc, :], woap[:, kc, :], wout, True)
    band = k.sb([128, 4, 4, 128], BF16, "band")
    k.dma("sp", band[:, :, :, :], AP_(dr["bands"])[:, :, :, :], band, True)
    sel = k.sb([128, 4, 8, 32], BF16, "sel")
    k.dma("sp", sel[:, :, :, :], AP_(dr["sel"])[:, :, :, :], sel, True)
    ucand = k.sb([128, 4, 512], BF16, "ucand")
    k.dma("sp", ucand[:, :, :], AP_(dr["uhalo_all"]).rearrange("(kt p) c -> p kt c", p=128), ucand, True)
    ug = [k.sb([128, 4, 512], BF16, "ug%d" % i) for i in range(2)]
    halo = k.sb([32, 512], BF16, "halo")
    pTb = [k.sb([128, 512], BF16, "pTb%d" % i) for i in range(2)]
    sgbt = [k.sb([128, 512], BF16, "sgbt%d" % i) for i in range(2)]
    attp = [k.sb([128, 512], BF16, "attp%d" % i) for i in range(2)]
    tmp = [k.sb([128, 512], F32, "ptmp%d" % i) for i in range(2)]
    mixT = [k.sb([128, 8, 512], BF16, "mixT%d" % i) for i in range(2)]
    xt = [k.sb([128, D], F32, "xmt%d" % i) for i in range(2)]
    xo = [k.sb([128, D], F32, "xmo%d" % i) for i in range(2)]
    tmp2 = k.sb([128, D], F32, "tmp2")
    u_d = AP_(dr["u_local"])
    sig_d = AP_(dr["sigT"])
    mix_d = AP_(dr["mixT"])
    x_d = AP_(dr["x"])
    x1_d = AP_(dr["x1"])
    nf = 0
    for gi in range(NG):
        qc = slice(gi * 512, (gi + 1) * 512)
        ugb = ug[gi % 2]
        k.dma("sp", ugb[:, :, :], u_d[qc, :].rearrange("(t p) c -> p t c", p=128), ugb, True)
        ph = ps[0]
        for kt in range(4):
            mm(k, ph, ph[0:32, :], sel, sel[:, kt, gi, :], ucand, ucand[:, kt, :], kt == 0, kt == 3)
        CP(k, "act", halo, halo[:, :], ph, ph[0:32, :])
        mxb = mixT[gi % 2]
        for g4 in range(4):
            pp = ps[1 + g4 % 2]
            for tt in range(4):
                bk = 1 if (gi == 0 and tt == 0) else 0
                mm(k, pp, pp[:, tt * 128:(tt + 1) * 128], ugb, ugb[:, tt, g4 * 128:(g4 + 1) * 128], band, band[:, bk, g4, :], True, False)
                if tt == 0:
                    mm(k, pp, pp[:, 0:128], halo, halo[0:32, g4 * 128:(g4 + 1) * 128], band, band[0:32, 3, g4, :], False, True)
                else:
                    mm(k, pp, pp[:, tt * 128:(tt + 1) * 128], ugb, ugb[64:128, tt - 1, g4 * 128:(g4 + 1) * 128], band, band[64:128, 2, g4, :], False, True)
            pb_ = pTb[g4 % 2]
            CP(k, "act", pb_, pb_[:, :], pp, pp[:, :])
            for half in range(2):
                f = 2 * g4 + half
                po = ps[3 + half]
                mm(k, po, po[:, :], wpool, wpool[:, g4, half * 128:(half + 1) * 128], pb_, pb_[:, :], True, True)
                sb_ = sgbt[nf % 2]
                ap_ = attp[nf % 2]
                tp_ = tmp[nf % 2]
                nf += 1
                k.dma("sp", sb_[:, :], sig_d[1024 + f * 128:1024 + (f + 1) * 128, qc], sb_, True)
                k.dma("sp", ap_[:, :], mix_d[f * 128:(f + 1) * 128, qc], ap_, True)
                STT(k, "dve", tp_, tp_[:, :], po, po[:, :], pscale[:, f:f + 1], sb_, sb_[:, :], ALU.mult, ALU.mult, rd=[pscale])
                TT(k, "pool", mxb, mxb[:, f, :], tp_, tp_[:, :], ap_, ap_[:, :], ALU.add)
        for tt in range(4):
            t = gi * 4 + tt
            xb = xt[t % 2]
            xob = xo[t % 2]
            k.dma("sp", xb[:, :], x_d[t * 128:(t + 1) * 128, :], xb, True)
            for cg in range(2):
                po = ps[5 + cg]
                for f in range(8):
                    mm(k, po, po[:, :], mxb, mxb[:, f, tt * 128:(tt + 1) * 128], wout, wout[:, f, cg * 512:(cg + 1) * 512], f == 0, f == 7)
                TT(k, "dve", tmp2, tmp2[:, cg * 512:(cg + 1) * 512], po, po[:, :], c.mod, c.mod[:, 2 * D + cg * 512:2 * D + (cg + 1) * 512], ALU.mult)
            TT(k, "pool", xob, xob[:, :], tmp2, tmp2[:, :], xb, xb[:, :], ALU.add)
            k.dma("pool", x1_d[t * 128:(t + 1) * 128, :], xob[:, :], xob, False)
    k.scope_end(mark)


def phase_moe(k, c, dr, final):
    ps = c.ps
    mark = k.scope_begin()
    A2 = k.sb([128, D], F32, "A2")
    k.dma("sp", A2[:, :], AP_(dr["norm_ffn_rep"])[:, :], A2, True)
    STT(k, "dve", A2, A2[:, :], c.mod, c.mod[:, 4 * D:5 * D], 1.0, A2, A2[:, :], ALU.add, ALU.mult)
    wr = k.sb([128, 8, NE], F32, "wr")
    k.dma("sp", wr[:, :, :], AP_(dr["w_router"]).rearrange("(kc p) n -> p kc n", p=128), wr, True)
    brep = k.sb([128, NE], F32, "brep")
    k.dma("sp", brep[:, :], AP_(dr["b_router_rep"])[:, :], brep, True)
    bgu = k.sb([128, NE, 16], F32, "bgu")
    k.dma("sp", bgu[:, :, :], AP_(dr["b_guT"])[:, :, :], bgu, True)
    TS(k, "dve", bgu, bgu[:, :, 8:16], bgu, bgu[:, :, 8:16], 1.0, None, ALU.add)
    NT = 8
    NSG = NT // 4
    acc = k.sb([128, NT, D], F32, "acc")
    h2T = k.sb([128, 8, NT * 128], BF16, "h2T")
    Gq = k.sb([128, NT, NE], F32, "Gq")
    wg = k.sb([128, 8, 2 * DFF], BF16, "wgu")
    wd = k.sb([128, 8, D], BF16, "wdn")
    bdf = k.sb([1, D], F32, "bdf")
    bd = k.sb([128, D], BF16, "bdn")
    k.op("pool", lambda e: e.memset(bd[:, :], 0.0), [], [bd])
    actT = [k.sb([128, 8, 512], BF16, "actT%d" % i) for i in range(NSG)]
    g1s = [k.sb([128, 512], F32, "g1_%d" % i) for i in range(2)]
    sgms = [k.sb([128, 512], F32, "sgm_%d" % i) for i in range(2)]
    l1s = [k.sb([128, 512], F32, "l1_%d" % i) for i in range(2)]
    nch = 0
    ss = k.sb([128, 1], F32, "mss")
    rstd = k.sb([128, 1], F32, "mrstd")
    x1_d = AP_(dr["x1"])
    xo_d = AP_(dr["x_out"])
    wgu_d = AP_(dr["w_gu"])
    wdn_d = AP_(dr["w_down"])
    bdn_d = AP_(dr["b_down"])
    NQ = TL // (NT * 128)
    ncast = 0

    def load_wgu(ei, stg):
        nonlocal ncast
        wga = wgu_d[ei].rearrange("(kc p) n -> p kc n", p=128)
        for kc in range(8):
            sb_ = stg[ncast % len(stg)]
            k.dma("sp", sb_[:, :], wga[:, kc, :], sb_, True)
            CP(k, "act" if ncast % 3 != 2 else "dve", wg, wg[:, kc, :], sb_, sb_[:, :])
            ncast += 1

    def load_wdn(ei, stg):
        nonlocal ncast
        wda = wdn_d[ei].rearrange("(kc p) n -> p kc n", p=128)
        for kc in range(0, 8, 2):
            sb_ = stg[ncast % len(stg)]
            k.dma("sp", sb_[:, :].rearrange("p (a n) -> p a n", a=2), wda[:, kc:kc + 2, :], sb_, True)
            CP(k, "act" if ncast % 3 != 2 else "dve", wd, wd[:, kc:kc + 2, :], sb_, sb_[:, :].rearrange("p (a n) -> p a n", a=2))
            ncast += 1
        k.dma("sp", bdf[:, :], bdn_d[ei:ei + 1, :], bdf, True)
        CP(k, "pool", bd, bd[0:1, :], bdf, bdf[:, :])

    for q in range(NQ):
        m2 = k.scope_begin()
        xt = [k.sb([128, D], F32, "xq%d" % i) for i in range(2)]
        h2 = k.sb([128, D], F32, "h2")
        h2Tf = k.sb([128, 8, 128], F32, "h2Tf")
        c.junkb = k.sb([128, D], BF16, "junk2")
        lg = k.sb([128, NE], F32, "lg")
        top8 = k.sb([128, 8], F32, "top8")
        negm = k.sb([128, 1], F32, "negm")
        msk = k.sb([128, NE], F32, "msk")
        ex = k.sb([128, NE], F32, "ex")
        sm = k.sb([128, 1], F32, "sm")
        for T in range(NT):
            t = q * NT + T
            xb = xt[t % 2]
            k.dma("sp", xb[:, :], x1_d[t * 128:(t + 1) * 128, :], xb, True)
            rms_rstd(k, c, xb, xb[:, :], D, c.junkb[:, :], ss, rstd)
            STT(k, "dve", h2, h2[:, :], xb, xb[:, :], rstd[:, 0:1], A2, A2[:, :], ALU.mult, ALU.mult, rd=[rstd])
            TT(k, "pool", h2, h2[:, :], h2, h2[:, :], c.mod, c.mod[:, 3 * D:4 * D], ALU.add)
            for kc in range(8):
                pb = ps[kc // 4]
                tr(k, pb, pb[:, (kc % 4) * 128:(kc % 4 + 1) * 128], h2, h2[:, kc * 128:(kc + 1) * 128], c.ident_f, c.ident_f[:, :])
            for hf in range(2):
                CP(k, "act", h2Tf, h2Tf[:, hf * 4:(hf + 1) * 4, :], ps[hf], ps[hf][:, :].rearrange("p (k t) -> p k t", k=4))
                CP(k, "dve", h2T, h2T[:, hf * 4:(hf + 1) * 4, T * 128:(T + 1) * 128], h2Tf, h2Tf[:, hf * 4:(hf + 1) * 4, :])
            pr = ps[2]
            for kc in range(8):
                mm(k, pr, pr[:, 0:NE], h2Tf, h2Tf[:, kc, :], wr, wr[:, kc, :], kc == 0, kc == 7)
            TT(k, "dve", lg, lg[:, :], pr, pr[:, 0:NE], brep, brep[:, :], ALU.add)
            k.op("dve", lambda e, top8=top8, lg=lg: e.max(top8[:, :], lg[:, :]), [lg], [top8])
            TS(k, "dve", negm, negm[:, :], top8, top8[:, 0:1], -1.0, None, ALU.mult)
            TS(k, "dve", msk, msk[:, :], lg, lg[:, :], top8[:, 3:4], None, ALU.is_ge, rd=[top8])
            ACTF(k, ex, ex[:, :], lg, lg[:, :], AF.Exp, bias=negm[:, 0:1], rd=[negm])
            TT(k, "dve", ex, ex[:, :], ex, ex[:, :], msk, msk[:, :], ALU.mult)
            k.op("dve", lambda e, sm=sm, ex=ex: e.reduce_sum(sm[:, :], ex[:, :], axis=AX.X), [ex], [sm])
            k.op("dve", lambda e, sm=sm: e.reciprocal(sm[:, :], sm[:, :]), [sm], [sm])
            TS(k, "dve", Gq, Gq[:, T, :], ex, ex[:, :], sm[:, 0:1], None, ALU.mult, rd=[sm])
        k.op("pool", lambda e: e.memset(acc[:, :, :], 0.0), [], [acc])
        k.scope_end(m2)
        m2 = k.scope_begin()
        stg = [k.sb([128, 2 * DFF], F32, "stg%d" % i) for i in range(5)]
        ne_run = NE_RUN if MOE_DBG == 0 else 0
        if ne_run:
            load_wgu(0, stg)
            load_wdn(0, stg)
        for ei in range(ne_run):
            for f in range(8):
                base = 4 * (f % 2)
                for half in range(2):
                    for kc in range(8):
                        for sg_ in range(NSG):
                            pb = ps[base + 2 * half + sg_]
                            mm(k, pb, pb[:, :], wg, wg[:, kc, half * DFF + f * 128:half * DFF + (f + 1) * 128],
                               h2T, h2T[:, kc, sg_ * 512:(sg_ + 1) * 512], kc == 0, kc == 7)
                for sg_ in range(NSG):
                    pg = ps[base + sg_]
                    pl = ps[base + 2 + sg_]
                    aT = actT[sg_]
                    g1 = g1s[nch % 2]
                    sgm = sgms[nch % 2]
                    l1 = l1s[nch % 2]
                    nch += 1
                    TS(k, "dve", g1, g1[:, :], pg, pg[:, :], bgu[:, ei, f:f + 1], 7.0, ALU.add, ALU.min, rd=[bgu])
                    ACTF(k, sgm, sgm[:, :], g1, g1[:, :], AF.Sigmoid, scale=1.702)
                    TS(k, "dve", l1, l1[:, :], pl, pl[:, :], bgu[:, ei, 8 + f:9 + f], 8.0, ALU.add, ALU.min, rd=[bgu])
                    TT(k, "pool", sgm, sgm[:, :], g1, g1[:, :], sgm, sgm[:, :], ALU.mult)
                    STT(k, "dve", aT, aT[:, f, :], l1, l1[:, :], -6.0, sgm, sgm[:, :], ALU.max, ALU.mult)
            if ei + 1 < ne_run:
                load_wgu(ei + 1, stg)
            for sg_ in range(NSG):
                aT = actT[sg_]
                for tt in range(4):
                    T = sg_ * 4 + tt
                    pyb = 2 * (T % 2)
                    for cg in range(2):
                        py = ps[pyb + cg]
                        mm(k, py, py[:, :], c.e0, c.e0[:, :], bd, bd[:, cg * 512:(cg + 1) * 512], True, False)
                    for f in range(8):
                        for cg in range(2):
                            py = ps[pyb + cg]
                            mm(k, py, py[:, :], aT, aT[:, f, tt * 128:(tt + 1) * 128], wd, wd[:, f, cg * 512:(cg + 1) * 512], False, f == 7)
                    for cg in range(2):
                        py = ps[pyb + cg]
                        STT(k, "dve", acc, acc[:, T, cg * 512:(cg + 1) * 512], py, py[:, :], Gq[:, T, ei:ei + 1], acc,
                            acc[:, T, cg * 512:(cg + 1) * 512], ALU.mult, ALU.add, rd=[Gq])
            if ei + 1 < ne_run:
                load_wdn(ei + 1, stg)
        k.scope_end(m2)
        m2 = k.scope_begin()
        xt = [k.sb([128, D], F32, "xr%d" % i) for i in range(2)]
        ho = [k.sb([128, D], F32, "ho%d" % i) for i in range(2)]
        c.junkb = k.sb([128, D], BF16, "junk3")
        if final:
            nfr = k.sb([128, D], F32, "nfr")
            k.dma("sp", nfr[:, :], AP_(dr["norm_final_rep"])[:, :], nfr, True)
        for T in range(NT):
            t = q * NT + T
            xb = xt[t % 2]
            h2 = ho[t % 2]
            k.dma("sp", xb[:, :], x1_d[t * 128:(t + 1) * 128, :], xb, True)
            TT(k, "dve", h2, h2[:, :], acc, acc[:, T, :], c.mod, c.mod[:, 5 * D:6 * D], ALU.mult)
            TT(k, "pool", h2, h2[:, :], h2, h2[:, :], xb, xb[:, :], ALU.add)
            if final:
                rms_rstd(k, c, h2, h2[:, :], D, c.junkb[:, :], ss, rstd)
                STT(k, "dve", h2, h2[:, :], h2, h2[:, :], rstd[:, 0:1], nfr, nfr[:, :], ALU.mult, ALU.mult, rd=[rstd])
            k.dma("pool", xo_d[t * 128:(t + 1) * 128, :], h2[:, :], h2, False)
        k.scope_end(m2)
    k.scope_end(mark)


def core_groups(j):
    out = []
    for kk in range(4):
        out.append(8 * kk + j)
        out.append(8 * kk + 7 - j)
    return out


def local_token_index(j):
    idx = np.concatenate([np.arange(G * 512, (G + 1) * 512) for G in core_groups(j)])
    return idx


def rep(v, n=128):
    return np.ascontiguousarray(np.broadcast_to(np.asarray(v, np.float32).reshape(1, -1), (n, v.size)))


def build_pre(layer_has=None):
    nc = bass.Bass("TRN2", target_bir_lowering=False)
    dr = {}
    dr["ident"] = dram_in(nc, "ident", [128, 128], F32)
    dr["cT"] = dram_in(nc, "cT", [128, 8], F32)
    dr["ada_b_rep"] = dram_in(nc, "ada_b_rep", [128, 6 * D], F32)
    dr["ada_w"] = dram_in(nc, "ada_w", [D, 6 * D], F32)
    dr["w_in"] = dram_in(nc, "w_in", [D, INC], F32)
    dr["w_uq"] = dram_in(nc, "w_uq", [QLORA, 1536], F32)
    dr["norm_mix_rep"] = dram_in(nc, "norm_mix_rep", [128, D], F32)
    dr["q_norm_rep"] = dram_in(nc, "q_norm_rep", [128, QLORA], F32)
    dr["kv_norm_rep"] = dram_in(nc, "kv_norm_rep", [128, KVLORA], F32)
    dr["pos_rep"] = dram_in(nc, "pos_rep", [64, TL], I32)
    dr["inv_freq"] = dram_in(nc, "inv_freq", [64, 1], F32)
    dr["x"] = dram_in(nc, "x", [TL, D], F32)
    dr["u_local"] = dram_out(nc, "u_local", [TL, 512], BF16)
    dr["sigT"] = dram_out(nc, "sigT", [2048, TL], BF16)
    dr["latT_local"] = dram_out(nc, "latT_local", [320, TL], BF16)
    dr["qT"] = dram_out(nc, "qT", [NH, 192, TL], BF16)
    k = KB(nc)
    c = Ctx()
    setup_common(k, c, dr)
    compute_mod(k, c, dr)
    phase_pre(k, c, dr)
    k.barrier()
    cnt = k.finish()
    return nc, cnt


def build_post(final, phases=(1, 2, 3), debug=False):
    nc = bass.Bass("TRN2", target_bir_lowering=False)
    dr = {}
    def din(name, shape, dt=F32):
        dr[name] = dram_in(nc, name, shape, dt)
    din("ident", [128, 128])
    din("cT", [128, 8])
    din("ada_b_rep", [128, 6 * D])
    din("ada_w", [D, 6 * D])
    din("latT_all", [4, 320, TL], BF16)
    din("qT", [NH, 192, TL], BF16)
    din("sigT", [2048, TL], BF16)
    din("u_local", [TL, 512], BF16)
    din("uhalo_all", [512, 512], BF16)
    din("maskbank", [128, 32, 512], BF16)
    din("bands", [128, 4, 4, 128], BF16)
    din("sel", [128, 4, 8, 32], BF16)
    din("x", [TL, D])
    din("w_ukv", [KVLORA, 2048])
    din("w_pool", [4, 128, 256])
    din("pscaleT", [128, 8])
    din("w_out", [D, D])
    din("norm_ffn_rep", [128, D])
    din("w_router", [D, NE])
    din("b_router_rep", [128, NE])
    din("b_guT", [128, NE, 16])
    din("w_gu", [NE, D, 2 * DFF])
    din("w_down", [NE, DFF, D])
    din("b_down", [NE, D])
    if final:
        din("norm_final_rep", [128, D])
    mk = dram_out if debug else dram_tmp
    dr["mixT"] = mk(nc, "mixT", [D, TL], BF16)
    dr["x1"] = mk(nc, "x1", [TL, D], F32)
    dr["x_out"] = dram_out(nc, "x_out", [TL, D], F32)
    k = KB(nc)
    c = Ctx()
    setup_common(k, c, dr)
    compute_mod(k, c, dr)
    if 1 in phases:
        phase_attn(k, c, dr)
    if 2 in phases:
        phase_mix(k, c, dr)
    if 3 in phases:
        phase_moe(k, c, dr, final)
    k.barrier()
    cnt = k.finish()
    return nc, cnt


POOL_W = (2, 4, 8, 16)
_bf = ml_dtypes.bfloat16


def host_consts(j):
    bands = np.zeros((128, 4, 4, 128), np.float32)
    tp = np.arange(128)[:, None]
    t = np.arange(128)[None, :]
    for g, w in enumerate(POOL_W):
        inwin = (tp <= t) & (tp > t - w)
        bands[:, 0, g, :] = inwin / float(w) - (tp == t)
        if j == 0:
            cntv = np.minimum(t + 1, w).astype(np.float32)
            bands[:, 1, g, :] = inwin / cntv - (tp == t)
        else:
            bands[:, 1, g, :] = bands[:, 0, g, :]
        tprev = tp - 128
        bands[:, 2, g, :] = ((tprev > t - w) & (tp >= 96)) / float(w)
        th = tp - 32
        bands[:, 3, g, :] = ((th > t - w) & (tp >= 16) & (tp < 32)) / float(w)
    sel = np.zeros((4, 128, 8, 32), np.float32)
    groups = core_groups(j)
    for gi, G in enumerate(groups):
        if G == 0:
            continue
        jp, gip = seq_group_owner(G - 1)
        base = (jp * 8 + gip) * 16
        for r in range(16):
            R = base + r
            sel[R // 128, R % 128, gi, 16 + r] = 1.0
    sel = np.ascontiguousarray(sel.transpose(1, 0, 2, 3))
    mb = np.zeros((128, 32, 512), np.float32)
    p = np.arange(128)[:, None]
    cc = np.arange(512)[None, :]
    for side in range(2):
        for sp in range(16):
            rel = sp - 4 * j if side == 0 else sp - 12 + 4 * j
            mb[:, side * 16 + sp, :] = (rel * 128 + p <= cc)
    return dict(bands=bands.astype(_bf), sel=sel.astype(_bf), maskbank=mb.astype(_bf))


def pre_inputs(inp, L, r, xloc):
    b, j = r // 4, r % 4
    idx = local_token_index(j)
    half = ROPE // 2
    inv_freq = (10000.0 ** (-np.arange(half, dtype=np.float32) / half)).astype(np.float32)
    return dict(
        ident=np.eye(128, dtype=np.float32),
        cT=np.ascontiguousarray(inp["c"][b].reshape(8, 128).T),
        ada_b_rep=rep(inp["ada_b"][L]),
        ada_w=np.ascontiguousarray(inp["ada_w"][L]),
        w_in=np.ascontiguousarray(inp["w_in"][L]),
        w_uq=np.ascontiguousarray(inp["w_uq"][L]),
        norm_mix_rep=rep(inp["norm_mix"][L]),
        q_norm_rep=rep(inp["q_norm"][L]),
        kv_norm_rep=rep(inp["kv_norm"][L]),
        pos_rep=np.ascontiguousarray(np.broadcast_to(inp["positions"][b][idx][None, :], (64, TL))).astype(np.int32),
        inv_freq=np.concatenate([inv_freq, inv_freq]).reshape(64, 1).astype(np.float32),
        x=xloc,
    )


def post_inputs(inp, L, r, xloc, pre_out, final, consts):
    b, j = r // 4, r % 4
    ranks = [b * 4 + i for i in range(4)]
    lat_all = np.ascontiguousarray(np.stack([pre_out[q]["latT_local"] for q in ranks], 0))
    uh = np.concatenate([pre_out[q]["u_local"].reshape(8, 512, 512)[:, 496:512, :].reshape(128, 512) for q in ranks], 0)
    d = dict(
        ident=np.eye(128, dtype=np.float32),
        cT=np.ascontiguousarray(inp["c"][b].reshape(8, 128).T),
        ada_b_rep=rep(inp["ada_b"][L]),
        ada_w=np.ascontiguousarray(inp["ada_w"][L]),
        latT_all=lat_all,
        qT=pre_out[r]["qT"],
        sigT=pre_out[r]["sigT"],
        u_local=pre_out[r]["u_local"],
        uhalo_all=np.ascontiguousarray(uh),
        maskbank=consts[j]["maskbank"],
        bands=consts[j]["bands"],
        sel=consts[j]["sel"],
        x=xloc,
        w_ukv=np.ascontiguousarray(inp["w_ukv"][L]),
        w_pool=np.ascontiguousarray(inp["w_pool"][L]),
        pscaleT=np.ascontiguousarray(inp["pool_scale"][L].reshape(8, 128).T),
        w_out=np.ascontiguousarray(inp["w_out"][L]),
        norm_ffn_rep=rep(inp["norm_ffn"][L]),
        w_router=np.ascontiguousarray(inp["w_router"][L]),
        b_router_rep=rep(inp["b_router"][L]),
        b_guT=np.ascontiguousarray(inp["b_gu"][L].reshape(NE, 16, 128).transpose(2, 0, 1)),
        w_gu=np.ascontiguousarray(inp["w_gu"][L]),
        w_down=np.ascontiguousarray(inp["w_down"][L]),
        b_down=np.ascontiguousarray(inp["b_down"][L]),
    )
    if final:
        d["norm_final_rep"] = rep(inp["norm_final"])
    return d


LAYERED = dict(
    ada_b_rep=[128, 6 * D], ada_w=[D, 6 * D], w_in=[D, INC], w_uq=[QLORA, 1536], norm_mix_rep=[128, D],
    q_norm_rep=[128, QLORA], kv_norm_rep=[128, KVLORA], w_ukv=[KVLORA, 2048], w_pool=[4, 128, 256],
    pscaleT=[128, 8], w_out=[D, D], norm_ffn_rep=[128, D], w_router=[D, NE], b_router_rep=[128, NE],
    b_guT=[128, NE, 16], w_gu=[NE, D, 2 * DFF], w_down=[NE, DFF, D], b_down=[NE, D])
SHARED = dict(ident=([128, 128], F32), cT=([128, 8], F32), pos_rep=([64, TL], I32), inv_freq=([64, 1], F32),
              maskbank=([128, 32, 512], BF16), bands=([128, 4, 4, 128], BF16), sel=([128, 4, 8, 32], BF16),
              norm_final_rep=([128, D], F32), x=([TL, D], F32))
GROUPS = [[0, 1, 2, 3], [4, 5, 6, 7]]
BIG = ("w_gu", "w_down")


def build_fused():
    nc = bass.Bass("TRN2", target_bir_lowering=False)
    hin = {}
    for n, sh in LAYERED.items():
        if n in BIG:
            hin[n] = [dram_in(nc, n + "%d" % L, sh, F32) for L in range(2)]
        else:
            hin[n] = dram_in(nc, n, [2] + sh, F32)
    for n, (sh, dt) in SHARED.items():
        hin[n] = dram_in(nc, n, sh, dt)
    out = dram_out(nc, "out", [TL, D], F32)
    tmp = dict(
        u_local=dram_tmp(nc, "u_local", [TL, 512], BF16), sigT=dram_tmp(nc, "sigT", [2048, TL], BF16),
        qT=dram_tmp(nc, "qT", [NH, 192, TL], BF16),
        uh_local=dram_tmp(nc, "uh_local", [128, 512], BF16),
        uhalo_all=dram_tmp(nc, "uhalo_all", [512, 512], BF16), mixT=dram_tmp(nc, "mixT", [D, TL], BF16),
        x1=dram_tmp(nc, "x1", [TL, D], F32), xbuf=dram_tmp(nc, "xbuf", [TL, D], F32))
    PR = (128, 128, 64)
    lat_loc = [dram_tmp(nc, "lat%d" % i, [PR[i], TL], BF16) for i in range(3)]
    lat_all_t = [dram_tmp(nc, "lata%d" % i, [4 * PR[i], TL], BF16) for i in range(3)]
    k = KB(nc)
    c = Ctx()
    setup_common(k, c, hin)
    for L in range(2):
        d = {n: (hin[n][L].ap() if n in BIG else hin[n].ap()[L]) for n in LAYERED}
        for n in SHARED:
            d[n] = hin[n].ap()
        for n in tmp:
            d[n] = tmp[n].ap()
        d["lat_parts"] = [t_.ap() for t_ in lat_loc]
        d["lat_all_parts"] = [t_.ap().rearrange("(r f) t -> r f t", r=4) for t_ in lat_all_t]
        d["x"] = hin["x"].ap() if L == 0 else tmp["xbuf"].ap()
        d["x_out"] = tmp["xbuf"].ap() if L == 0 else out.ap()
        compute_mod(k, c, d)
        m = k.scope_begin()
        phase_pre(k, c, d)
        k.scope_end(m)
        if FUSE_DBG != 1:
            for t_i, t_o in zip(lat_loc, lat_all_t):
                k.coll("AllGather", t_i.ap(), t_o.ap(), GROUPS)
            k.coll("AllGather", tmp["uh_local"].ap(), tmp["uhalo_all"].ap(), GROUPS)
        k.barrier()
        phase_attn(k, c, d)
        phase_mix(k, c, d)
        phase_moe(k, c, d, L == 1)
    k.barrier()
    cnt = k.finish()
    return nc, cnt


def fused_inputs(inp, r, consts):
    b, j = r // 4, r % 4
    idx = local_token_index(j)
    half = ROPE // 2
    inv_freq = (10000.0 ** (-np.arange(half, dtype=np.float32) / half)).astype(np.float32)
    st = lambda f: np.ascontiguousarray(np.stack([f(L) for L in range(2)], 0))
    d = dict(
        ada_b_rep=st(lambda L: rep(inp["ada_b"][L])), ada_w=np.ascontiguousarray(inp["ada_w"]),
        w_in=np.ascontiguousarray(inp["w_in"]), w_uq=np.ascontiguousarray(inp["w_uq"]),
        norm_mix_rep=st(lambda L: rep(inp["norm_mix"][L])), q_norm_rep=st(lambda L: rep(inp["q_norm"][L])),
        kv_norm_rep=st(lambda L: rep(inp["kv_norm"][L])), w_ukv=np.ascontiguousarray(inp["w_ukv"]),
        w_pool=np.ascontiguousarray(inp["w_pool"]),
        pscaleT=st(lambda L: inp["pool_scale"][L].reshape(8, 128).T),
        w_out=np.ascontiguousarray(inp["w_out"]), norm_ffn_rep=st(lambda L: rep(inp["norm_ffn"][L])),
        w_router=np.ascontiguousarray(inp["w_router"]), b_router_rep=st(lambda L: rep(inp["b_router"][L])),
        b_guT=st(lambda L: inp["b_gu"][L].reshape(NE, 16, 128).transpose(2, 0, 1)),
        w_gu0=np.ascontiguousarray(inp["w_gu"][0]), w_gu1=np.ascontiguousarray(inp["w_gu"][1]),
        w_down0=np.ascontiguousarray(inp["w_down"][0]), w_down1=np.ascontiguousarray(inp["w_down"][1]),
        b_down=np.ascontiguousarray(inp["b_down"]),
        ident=np.eye(128, dtype=np.float32),
        cT=np.ascontiguousarray(inp["c"][b].reshape(8, 128).T),
        pos_rep=np.ascontiguousarray(np.broadcast_to(inp["positions"][b][idx][None, :], (64, TL))).astype(np.int32),
        inv_freq=np.concatenate([inv_freq, inv_freq]).reshape(64, 1).astype(np.float32),
        maskbank=consts[j]["maskbank"], bands=consts[j]["bands"], sel=consts[j]["sel"],
        norm_final_rep=rep(inp["norm_final"]),
        x=np.ascontiguousarray(inp["x"][b][idx]),
    )
    return d


_CACHE = {}


def kernel(**inputs):
    inp = {k_: np.asarray(v) for k_, v in inputs.items()}
    if "fused" not in _CACHE:
        _CACHE["fused"] = build_fused()[0]
    consts = [host_consts(j) for j in range(4)]
    cores = list(range(8))
    maps = [fused_inputs(inp, r, consts) for r in cores]
    res = run_bass_kernel_spmd(_CACHE["fused"], maps, core_ids=cores)
    out = np.empty((2, S, D), np.float32)
    for r in cores:
        out[r // 4][local_token_index(r % 4)] = res.results[r]["out"]
    return out
```
